# Optimizing a Trainium2 kernel written in Bass

```python
import math
import jax
import jax.numpy as jnp
from jax import lax
import numpy as np

D_MODEL = 1024
BATCH = 2
SEQ = 8192
DEPTH = 2

ATT_HEADS = 8
ATT_HEAD_DIM = 128
KV_LATENT = 256
IDX_HEADS = 8
IDX_DIM = 64
TOPK_MAX = 256
Q_BLOCK = 128
ATT_SCALE = ATT_HEAD_DIM ** -0.5
INDEX_SCALE = (IDX_HEADS ** -0.5) * (IDX_DIM ** -0.5)
DN_HEADS = 8
DN_DK = 128
DN_DV = 128
CONV_WIDTH = 4
CHUNK = 64
N_GROUPS = 4
EXPERTS_PER_GROUP = 8
N_EXPERTS = N_GROUPS * EXPERTS_PER_GROUP
EXPERT_TOPK = 2
EXPERT_FF = 512
MOE_BLOCK = 128
LN_EPS = 1e-5
RMS_EPS = 1e-6

ATT_Q_COLS = ATT_HEADS * ATT_HEAD_DIM
IDX_Q_COLS = IDX_HEADS * IDX_DIM
DN_QK_COLS = DN_HEADS * DN_DK
DN_V_COLS = DN_HEADS * DN_DV
CONV_CH = 2 * DN_QK_COLS + DN_V_COLS
COL_SIZES = (ATT_Q_COLS, KV_LATENT, IDX_Q_COLS, IDX_DIM, IDX_HEADS,
             CONV_CH, DN_HEADS, DN_HEADS, DN_V_COLS, 2 * D_MODEL)
N_IN = sum(COL_SIZES)
SPLITS = tuple(int(v) for v in np.cumsum(COL_SIZES)[:-1])

kernel_name = 'hybrid_dsa_gated_deltanet_hier_moe'


def layer_norm(x, g, b):
    xf = x.astype(jnp.float32)
    mu = jnp.mean(xf, axis=-1, keepdims=True)
    var = jnp.mean(jnp.square(xf - mu), axis=-1, keepdims=True)
    return ((xf - mu) * lax.rsqrt(var + LN_EPS)).astype(x.dtype) * g + b


def rms_norm(x, g):
    xf = x.astype(jnp.float32)
    return (xf * lax.rsqrt(jnp.mean(xf * xf, axis=-1, keepdims=True) + RMS_EPS)).astype(x.dtype) * g


def l2_normalize(x):
    return x * lax.rsqrt(jnp.sum(x * x, axis=-1, keepdims=True) + RMS_EPS)


def causal_conv(x, w):
    width = w.shape[0]
    return lax.conv_general_dilated(
        x, w[:, None, :], window_strides=(1,), padding=[(width - 1, 0)],
        dimension_numbers=('NWC', 'WIO', 'NWC'), feature_group_count=x.shape[-1])


def dsa_attention(q_att, c_kv, q_idx, k_idx, w_idx, w_uk, w_uv):
    B, S = c_kv.shape[0], c_kv.shape[1]
    topk = min(TOPK_MAX, S // 4)
    nb = S // Q_BLOCK
    q_lat = jnp.einsum('bshd,hcd->bshc', q_att, w_uk)
    key_pos = jnp.arange(S)
    gather = jax.vmap(lambda table, idx: table[idx])

    def to_blocks(t):
        return jnp.moveaxis(t.reshape((B, nb, Q_BLOCK) + t.shape[2:]), 1, 0)

    def block(args):
        ql, qi, wi, t0 = args
        qpos = t0 + jnp.arange(Q_BLOCK)
        rel = jax.nn.relu(jnp.einsum('bqhd,bsd->bqhs', qi, k_idx))
        iscore = jnp.einsum('bqhs,bqh->bqs', rel, wi).astype(jnp.float32)
        causal = key_pos[None, :] <= qpos[:, None]
        iscore = jnp.where(causal[None], iscore, -jnp.inf)
        _, sel = lax.top_k(iscore, topk)
        kv = gather(c_kv, sel)
        s = jnp.einsum('bqhc,bqkc->bqhk', ql, kv).astype(jnp.float32) * ATT_SCALE
        valid = sel <= qpos[None, :, None]
        s = jnp.where(valid[:, :, None, :], s, -jnp.inf)
        p = jax.nn.softmax(s, axis=-1).astype(kv.dtype)
        return jnp.einsum('bqhk,bqkc->bqhc', p, kv)

    o_lat = lax.map(block, (to_blocks(q_lat), to_blocks(q_idx), to_blocks(w_idx),
                            jnp.arange(nb) * Q_BLOCK))
    o_lat = jnp.moveaxis(o_lat, 0, 1).reshape(B, S, ATT_HEADS, KV_LATENT)
    o = jnp.einsum('bshc,hcd->bshd', o_lat, w_uv)
    return o.reshape(B, S, ATT_HEADS * ATT_HEAD_DIM)


def chunk_gated_delta_rule(q, k, v, beta, g):
    B, S, H, Dk = q.shape
    Dv = v.shape[-1]
    n = S // CHUNK

    def chunks(t):
        return t.reshape(B, n, CHUNK, H, -1).transpose(0, 3, 1, 2, 4)

    q, k, v = chunks(q), chunks(k), chunks(v)
    beta = beta.reshape(B, n, CHUNK, H).transpose(0, 3, 1, 2)
    G = jnp.cumsum(g.reshape(B, n, CHUNK, H).transpose(0, 3, 1, 2), axis=-1)
    tril = jnp.tril(jnp.ones((CHUNK, CHUNK), dtype=bool))
    tril_strict = jnp.tril(jnp.ones((CHUNK, CHUNK), dtype=bool), -1)
    decay = jnp.exp(jnp.where(tril, G[..., :, None] - G[..., None, :], -jnp.inf))
    kb = k * beta[..., None]
    A = jnp.where(tril_strict, jnp.einsum('bhncd,bhnsd->bhncs', kb, k) * decay, 0.0)
    eye = jnp.eye(CHUNK, dtype=A.dtype)
    rhs = jnp.concatenate([v * beta[..., None], kb * jnp.exp(G)[..., None]], axis=-1)
    sol = lax.linalg.triangular_solve(eye + A, rhs, left_side=True, lower=True,
                                      unit_diagonal=True)
    u, w = sol[..., :Dv], sol[..., Dv:]
    qk = jnp.where(tril, jnp.einsum('bhncd,bhnsd->bhncs', q, k) * decay, 0.0)
    q_dec = q * jnp.exp(G)[..., None]
    k_tail = k * jnp.exp(G[..., -1:] - G)[..., None]
    g_last = jnp.exp(G[..., -1])

    def step(state, xs):
        q_n, k_n, u_n, w_n, qk_n, gl_n = xs
        v_new = u_n - jnp.einsum('bhck,bhkv->bhcv', w_n, state)
        o_n = (jnp.einsum('bhck,bhkv->bhcv', q_n, state)
               + jnp.einsum('bhcs,bhsv->bhcv', qk_n, v_new))
        state = state * gl_n[..., None, None] + jnp.einsum('bhck,bhcv->bhkv', k_n, v_new)
        return state, o_n

    xs = tuple(jnp.moveaxis(t, 2, 0) for t in (q_dec, k_tail, u, w, qk, g_last))
    state0 = jnp.zeros((B, H, Dk, Dv), jnp.float32)
    _, o = lax.scan(step, state0, xs)
    return o.transpose(1, 0, 3, 2, 4).reshape(B, S, H, Dv)


def gated_deltanet(qkv, a, b, z, conv_w, a_log, dt_bias, norm_g):
    B, S = qkv.shape[0], qkv.shape[1]
    dtype = qkv.dtype
    qkv = jax.nn.silu(causal_conv(qkv, conv_w)).astype(jnp.float32)
    q = l2_normalize(qkv[..., :DN_QK_COLS].reshape(B, S, DN_HEADS, DN_DK)) * (DN_DK ** -0.5)
    k = l2_normalize(qkv[..., DN_QK_COLS:2 * DN_QK_COLS].reshape(B, S, DN_HEADS, DN_DK))
    v = qkv[..., 2 * DN_QK_COLS:].reshape(B, S, DN_HEADS, DN_DV)
    beta = jax.nn.sigmoid(b.astype(jnp.float32))
    g = -jnp.exp(a_log.astype(jnp.float32)) * jax.nn.softplus(
        a.astype(jnp.float32) + dt_bias.astype(jnp.float32))
    o = chunk_gated_delta_rule(q, k, v, beta, g)
    zf = z.astype(jnp.float32).reshape(B, S, DN_HEADS, DN_DV)
    o = rms_norm(o, norm_g.astype(jnp.float32)) * jax.nn.silu(zf)
    return o.reshape(B, S, DN_V_COLS).astype(dtype)


def token_mixer(h, w_in, kv_norm_g, w_uk, w_uv, conv_w, a_log, dt_bias, dn_norm_g,
                w_br_att, w_br_dn, w_out):
    B, S = h.shape[0], h.shape[1]
    proj = h @ w_in
    (q_att, c_kv, q_idx, k_idx, w_idx, qkv, a, b, z, gates) = jnp.split(proj, SPLITS, axis=-1)
    c_kv = rms_norm(c_kv, kv_norm_g)
    y_att = dsa_attention(q_att.reshape(B, S, ATT_HEADS, ATT_HEAD_DIM), c_kv,
                          q_idx.reshape(B, S, IDX_HEADS, IDX_DIM), k_idx,
                          w_idx * INDEX_SCALE, w_uk, w_uv)
    y_dn = gated_deltanet(qkv, a, b, z, conv_w, a_log, dt_bias, dn_norm_g)
    gates = jax.nn.sigmoid(gates)
    g_att, g_dn = gates[..., :D_MODEL], gates[..., D_MODEL:]
    merged = g_att * (y_att @ w_br_att) + g_dn * (y_dn @ w_br_dn)
    return merged @ w_out


def hier_moe(h, w_route_grp, w_route_exp, w_gate, w_up, w_down):
    B, S, D = h.shape
    n_tok = B * S
    xt = h.reshape(n_tok, D)
    p_grp = jax.nn.softmax((xt @ w_route_grp).astype(jnp.float32), axis=-1)
    top_gp, g_idx = lax.top_k(p_grp, 1)
    exp_logits = (xt @ w_route_exp).astype(jnp.float32).reshape(n_tok, N_GROUPS, EXPERTS_PER_GROUP)
    in_grp = jnp.take_along_axis(exp_logits, g_idx[:, :, None], axis=1)[:, 0]
    top_pe, e_local = lax.top_k(jax.nn.softmax(in_grp, axis=-1), EXPERT_TOPK)
    gates = top_pe / jnp.sum(top_pe, axis=-1, keepdims=True) * top_gp
    e_idx = g_idx * EXPERTS_PER_GROUP + e_local

    m = n_tok * EXPERT_TOPK
    flat_e = e_idx.reshape(m)
    flat_tok = jnp.repeat(jnp.arange(n_tok), EXPERT_TOPK)
    flat_gate = gates.reshape(m)
    order = jnp.argsort(flat_e)
    se = flat_e[order]
    counts = jnp.bincount(flat_e, length=N_EXPERTS)
    pcounts = (counts + MOE_BLOCK - 1) // MOE_BLOCK * MOE_BLOCK
    starts = jnp.cumsum(counts) - counts
    pends = jnp.cumsum(pcounts)
    pstarts = pends - pcounts
    dest = pstarts[se] + jnp.arange(m) - starts[se]
    nb = -(-m // MOE_BLOCK) + N_EXPERTS
    rows = nb * MOE_BLOCK
    tok_buf = jnp.full((rows,), n_tok, jnp.int32).at[dest].set(flat_tok[order])
    gate_buf = jnp.zeros((rows,), jnp.float32).at[dest].set(flat_gate[order])
    blk_e = jnp.minimum(jnp.searchsorted(pends, jnp.arange(nb) * MOE_BLOCK, side='right'),
                        N_EXPERTS - 1)
    x_pad = jnp.concatenate([xt, jnp.zeros((1, D), xt.dtype)], axis=0)
    xs = x_pad[tok_buf].reshape(nb, MOE_BLOCK, D)

    def expert_block(args):
        xb, e = args
        hb = jax.nn.silu(xb @ w_gate[e]) * (xb @ w_up[e])
        return hb @ w_down[e]

    yb = lax.map(expert_block, (xs, blk_e)).reshape(rows, D)
    yb = yb * gate_buf[:, None].astype(yb.dtype)
    out = jnp.zeros((n_tok + 1, D), yb.dtype).at[tok_buf].add(yb)[:n_tok]
    return out.reshape(B, S, D)


def setup_inputs(seed: int = 0) -> dict:
    key = jax.random.key(seed)
    ks = jax.random.split(key, 24)
    f32 = jnp.float32
    L, D = DEPTH, D_MODEL
    beta = (8.0 * DEPTH) ** -0.25

    def nrm(k, shape, scale):
        return jax.random.normal(k, shape, f32) * scale

    dt = jnp.exp(jax.random.uniform(ks[10], (L, DN_HEADS), f32, math.log(1e-3), math.log(1e-1)))
    return {
        'x': nrm(ks[0], (BATCH, SEQ, D), 1.0),
        'c': nrm(ks[1], (BATCH, D), 1.0),
        'w_ada': nrm(ks[2], (L, D, 6 * D), 0.1 * D ** -0.5),
        'b_ada': nrm(ks[3], (L, 6 * D), 0.01),
        'w_in': nrm(ks[4], (L, D, N_IN), D ** -0.5),
        'kv_norm_g': 1.0 + nrm(ks[5], (L, KV_LATENT), 0.02),
        'w_uk': nrm(ks[6], (L, ATT_HEADS, KV_LATENT, ATT_HEAD_DIM), KV_LATENT ** -0.5),
        'w_uv': nrm(ks[7], (L, ATT_HEADS, KV_LATENT, ATT_HEAD_DIM), KV_LATENT ** -0.5),
        'conv_w': nrm(ks[8], (L, CONV_WIDTH, CONV_CH), CONV_WIDTH ** -0.5),
        'a_log': jnp.log(jax.random.uniform(ks[9], (L, DN_HEADS), f32, 1.0, 16.0)),
        'dt_bias': dt + jnp.log(-jnp.expm1(-dt)),
        'dn_norm_g': 1.0 + nrm(ks[11], (L, DN_DV), 0.02),
        'w_br_att': nrm(ks[12], (L, ATT_Q_COLS, D), ATT_Q_COLS ** -0.5),
        'w_br_dn': nrm(ks[13], (L, DN_V_COLS, D), DN_V_COLS ** -0.5),
        'w_out': nrm(ks[14], (L, D, D), beta * D ** -0.5),
        'w_route_grp': nrm(ks[15], (L, D, N_GROUPS), D ** -0.5),
        'w_route_exp': nrm(ks[16], (L, D, N_EXPERTS), D ** -0.5),
        'w_gate': nrm(ks[17], (L, N_EXPERTS, D, EXPERT_FF), D ** -0.5),
        'w_up': nrm(ks[18], (L, N_EXPERTS, D, EXPERT_FF), D ** -0.5),
        'w_down': nrm(ks[19], (L, N_EXPERTS, EXPERT_FF, D), beta * EXPERT_FF ** -0.5),
        'ln_g': 1.0 + nrm(ks[20], (L, 2, D), 0.02),
        'ln_b': nrm(ks[21], (L, 2, D), 0.02),
    }


def reference(x, c, w_ada, b_ada, w_in, kv_norm_g, w_uk, w_uv, conv_w, a_log, dt_bias,
              dn_norm_g, w_br_att, w_br_dn, w_out, w_route_grp, w_route_exp, w_gate,
              w_up, w_down, ln_g, ln_b):
    alpha = (2.0 * DEPTH) ** 0.25
    cond = jax.nn.silu(c)
    for l in range(DEPTH):
        mod = cond @ w_ada[l] + b_ada[l]
        sh1, sc1, gt1, sh2, sc2, gt2 = [m[:, None, :] for m in jnp.split(mod, 6, axis=-1)]
        h = x * (1.0 + sc1) + sh1
        y = token_mixer(h, w_in[l], kv_norm_g[l], w_uk[l], w_uv[l], conv_w[l], a_log[l],
                        dt_bias[l], dn_norm_g[l], w_br_att[l], w_br_dn[l], w_out[l])
        x = layer_norm(alpha * x + (1.0 + gt1) * y, ln_g[l, 0], ln_b[l, 0])
        h = x * (1.0 + sc2) + sh2
        y = hier_moe(h, w_route_grp[l], w_route_exp[l], w_gate[l], w_up[l], w_down[l])
        x = layer_norm(alpha * x + (1.0 + gt2) * y, ln_g[l, 1], ln_b[l, 1])
    return x
```

```python
import numpy as np
from contextlib import ExitStack
import concourse.bass as bass
import concourse.mybir as mybir
from concourse.bass_utils import run_bass_kernel_spmd

F32 = mybir.dt.float32
BF16 = mybir.dt.bfloat16
AF = mybir.ActivationFunctionType
ALU = mybir.AluOpType
AX = mybir.AxisListType

D = 1024
S = 8192
NB = 2
DEPTH = 2
ALPHA = (2.0 * DEPTH) ** 0.25
LN_EPS = 1e-5
RMS_EPS = 1e-6
ATT_SCALE = 128 ** -0.5
TOPK = 256
NEG = -1.0e30
C_QATT, C_CKV, C_QIDX, C_KIDX, C_WIDX = 0, 1024, 1280, 1792, 1856
C_DNQ, C_DNK, C_DNV, C_A, C_B, C_Z, C_GATES = 1864, 2888, 3912, 4936, 4944, 4952, 5976
N_IN = 8024


class Buf:
    __slots__ = ("w", "r", "excl")

    def __init__(self, excl=False):
        self.w = None
        self.r = []
        self.excl = excl


class T:
    def __init__(self, t, b=None):
        self.t = t
        self.b = b if b is not None else Buf()

    def __getitem__(self, idx):
        return self.t[idx]


class K:
    NDS = 24

    def __init__(self, nc, st):
        self.nc = nc
        self.st = st
        self.eng = dict(pe=nc.tensor, act=nc.scalar, dve=nc.vector, pool=nc.gpsimd, sp=nc.sync)
        self.sem = {k: st.enter_context(nc.semaphore("s_" + k)) for k in self.eng}
        self.cnt = {k: 0 for k in self.eng}
        self.waited = {}
        self.dsem = [st.enter_context(nc.semaphore("d%d" % i)) for i in range(self.NDS)]
        self.dcnt = [0] * self.NDS
        self.dnext = 0
        self.uid = 0

    def sb(self, shape, dtype, name=None):
        self.uid += 1
        t = self.st.enter_context(self.nc.sbuf_tensor(name or ("sb%d" % self.uid), list(shape), dtype))
        return T(t)

    def ps(self, shape, dtype, name=None):
        self.uid += 1
        t = self.st.enter_context(self.nc.psum_tensor(name or ("ps%d" % self.uid), list(shape), dtype))
        return T(t, Buf(excl=True))

    def _wait(self, eng, tok):
        kind, who, val = tok
        if kind == "e":
            if who == eng and eng == "pe":
                return
            sem = self.sem[who]
            key = (eng, "e", who)
        else:
            sem = self.dsem[who]
            key = (eng, "d", who)
            val = val * 16
        if self.waited.get(key, 0) >= val:
            return
        self.waited[key] = val
        self.eng[eng].wait_ge(sem, val)

    def _deps(self, eng, r, w):
        for b in r:
            b = b.b if isinstance(b, T) else b
            if b.w is not None:
                self._wait(eng, b.w)
            if b.excl:
                for tok in b.r:
                    if not (tok[0] == "e" and tok[1] == eng):
                        self._wait(eng, tok)
        for b in w:
            b = b.b if isinstance(b, T) else b
            if b.w is not None:
                self._wait(eng, b.w)
            for tok in b.r:
                if tok[0] == "e" and tok[1] == eng:
                    continue
                self._wait(eng, tok)

    def _commit(self, tok, r, w):
        wl = [b.b if isinstance(b, T) else b for b in w]
        for b in wl:
            b.w = tok
            b.r = []
        for b in r:
            b = b.b if isinstance(b, T) else b
            if b not in wl:
                if len(b.r) > 12:
                    last = {}
                    for t_ in b.r:
                        key = (t_[0], t_[1])
                        if key not in last or last[key][2] < t_[2]:
                            last[key] = t_
                    b.r = list(last.values())
                b.r.append(tok)

    def op(self, eng, fn, r=(), w=(), inc=True):
        self._deps(eng, r, w)
        inst = fn()
        if inc:
            self.cnt[eng] += 1
            inst.then_inc(self.sem[eng], 1)
            self._commit(("e", eng, self.cnt[eng]), r, w)
        else:
            self._commit(("e", eng, self.cnt[eng] + 1), r, w)
        return inst

    def dma(self, q, out, in_, r=(), w=(), **kw):
        self._deps(q, r, w)
        i = self.dnext
        self.dnext = (self.dnext + 1) % self.NDS
        if self.dcnt[i] > 0:
            self._wait(q, ("d", i, self.dcnt[i]))
        self.dcnt[i] += 1
        inst = self.eng[q].dma_start(out=out, in_=in_, **kw)
        inst.then_inc(self.dsem[i], 16)
        self._commit(("d", i, self.dcnt[i]), r, w)
        return inst

    def finish(self, bufs):
        for b in bufs:
            b = b.b if isinstance(b, T) else b
            if b.w is not None:
                self._wait("sp", b.w)

    def mm(self, out, lhsT, rhs, start, stop, r=(), w=(), inc=True):
        nc = self.nc
        return self.op("pe", lambda: nc.tensor.matmul(out, lhsT=lhsT, rhs=rhs, start=start, stop=stop), r, w, inc=inc)

    def tr(self, out, in_, ident, r=(), w=()):
        nc = self.nc
        return self.op("pe", lambda: nc.tensor.transpose(out, in_, ident), r, w)

    def act(self, out, in_, func, r=(), w=(), **kw):
        nc = self.nc
        return self.op("act", lambda: nc.scalar.activation(out=out, in_=in_, func=func, **kw), r, w)

    def ts(self, eng, out, in0, s1, s2, op0, op1=None, r=(), w=(), accum_out=None):
        e = self.eng[eng]
        kw = {}
        if op1 is not None:
            kw["op1"] = op1
        if accum_out is not None:
            kw["accum_out"] = accum_out
        return self.op(eng, lambda: e.tensor_scalar(out=out, in0=in0, scalar1=s1, scalar2=s2, op0=op0, **kw), r, w)

    def tt(self, eng, out, in0, in1, op, r=(), w=()):
        e = self.eng[eng]
        return self.op(eng, lambda: e.tensor_tensor(out=out, in0=in0, in1=in1, op=op), r, w)

    def stt(self, eng, out, in0, scalar, in1, op0, op1, r=(), w=()):
        e = self.eng[eng]
        return self.op(eng, lambda: e.scalar_tensor_tensor(out=out, in0=in0, scalar=scalar, in1=in1, op0=op0, op1=op1), r, w)

    def copy(self, eng, out, in_, r=(), w=()):
        if eng == "act":
            nc = self.nc
            return self.op("act", lambda: nc.scalar.copy(out=out, in_=in_), r, w)
        e = self.eng[eng]
        return self.op(eng, lambda: e.tensor_copy(out=out, in_=in_), r, w)

    def memset(self, eng, ap, val, w=()):
        e = self.eng[eng]
        return self.op(eng, lambda: e.memset(ap, val), (), w)


def bcast_rows(handle_ap, nparts):
    ap = handle_ap
    n = ap.shape[-1]
    return bass.AP(ap.tensor, ap.offset, [[0, nparts], [1, n]])


def make_consts(k):
    c = {}
    c["ident_f"] = k.sb([128, 128], F32, "ident_f")
    c["ident_b"] = k.sb([128, 128], BF16, "ident_b")
    c["ones_b"] = k.sb([128, 128], BF16, "ones_b")
    c["ones_f"] = k.sb([128, 128], F32, "ones_f")
    nc = k.nc
    k.memset("pool", c["ident_f"][:], 1.0, w=[c["ident_f"]])
    k.op("pool", lambda: nc.gpsimd.affine_select(out=c["ident_f"][:], in_=c["ident_f"][:], pattern=[[-1, 128]],
                                                  compare_op=ALU.is_equal, fill=0.0, base=0, channel_multiplier=1),
         r=[c["ident_f"]], w=[c["ident_f"]])
    k.copy("pool", c["ident_b"][:], c["ident_f"][:], r=[c["ident_f"]], w=[c["ident_b"]])
    k.memset("pool", c["ones_b"][:], 1.0, w=[c["ones_b"]])
    k.memset("pool", c["ones_f"][:], 1.0, w=[c["ones_f"]])
    c["cmask"] = k.sb([128, 128], F32, "cmask")
    k.memset("pool", c["cmask"][:], 0.0, w=[c["cmask"]])
    k.op("pool", lambda: nc.gpsimd.affine_select(out=c["cmask"][:], in_=c["cmask"][:], pattern=[[-1, 128]],
                                                  compare_op=ALU.is_ge, fill=NEG, base=0, channel_multiplier=1),
         r=[c["cmask"]], w=[c["cmask"]])
    return c


def adaln_cols(k, c, psb, cT_d, w_ada_d, b_adaT_d, which):
    nc = k.nc
    nw = len(which)
    modT = k.sb([128, nw, 8], F32)
    st = ExitStack()
    k_st, k.st = k.st, st
    condT = k.sb([128, 8], F32)
    k.dma("sp", condT[:], cT_d, w=[condT])
    sig = k.sb([128, 8], F32)
    k.act(sig[:], condT[:], AF.Sigmoid, r=[condT], w=[sig])
    k.tt("dve", condT[:], condT[:], sig[:], ALU.mult, r=[condT, sig], w=[condT])
    bT = k.sb([128, 48], F32)
    k.dma("sp", bT[:], b_adaT_d, w=[bT])
    wbufs = [k.sb([128, 8, 512], F32) for _ in range(2)]
    n = 0
    for j, wh in enumerate(which):
        for half in range(2):
            wb = wbufs[n % 2]
            n += 1
            c0 = wh * 1024 + half * 512
            k.dma("sp", wb[:], w_ada_d[:, c0:c0 + 512].rearrange("(kk p) n -> p kk n", p=128), w=[wb])
            for cc in range(4):
                for kk in range(8):
                    k.mm(psb[:, cc:cc + 1], wb[:, kk, cc * 128:(cc + 1) * 128], condT[:, kk:kk + 1],
                         start=(kk == 0), stop=(kk == 7), r=[wb, condT], w=[psb])
            cb = wh * 8 + half * 4
            k.tt("dve", modT[:, j, half * 4:half * 4 + 4], psb[:, 0:4], bT[:, cb:cb + 4], ALU.add,
                 r=[psb, bT], w=[modT])
    barrier(k)
    k.st = k_st
    st.close()
    return modT


def load_w(k, dst, src2d, c0, n, q="pool"):
    return k.dma(q, dst, src2d[:, c0:c0 + n].rearrange("(kk p) n -> p kk n", p=128), w=[])


def transpose_modulate(k, c, xt, hT, pbanks, scp1, sh, ntb, extra=None):
    for kk in range(8):
        pb = pbanks[kk % 2]
        for tb in range(ntb):
            k.tr(pb[:, tb * 128:(tb + 1) * 128], xt[:, tb, kk * 128:(kk + 1) * 128], c["ident_f"][:],
                 r=[xt, c["ident_f"]], w=[pb])
        k.act(hT[:, kk, 0:ntb * 128], pb[:, 0:ntb * 128], AF.Identity, scale=scp1[:, kk:kk + 1], bias=sh[:, kk:kk + 1],
              r=[pb], w=[hT])
        if extra is not None:
            k.act(extra[:, kk, 0:ntb * 128], pb[:, 0:ntb * 128], AF.Identity, scale=scp1[:, kk:kk + 1],
                  bias=sh[:, kk:kk + 1], r=[pb], w=[extra])


def phase_kq(k, c, P, io, jq, res, scr):
    nc = k.nc
    st = ExitStack()
    k_st, k.st = k.st, st
    modT = res["modT"]
    scp1 = k.sb([128, 8], F32)
    k.ts("dve", scp1[:], modT[:, 1, :], 1.0, None, ALU.add, r=[modT], w=[scp1])
    sh1 = modT[:, 0, :]
    w_in = io["w_in"]
    wk = k.sb([128, 8, 320], BF16)
    k.dma("pool", wk[:, :, 0:256], w_in[:, C_CKV:C_CKV + 256].rearrange("(kk p) n -> p kk n", p=128), w=[wk])
    k.dma("pool", wk[:, :, 256:320], w_in[:, C_KIDX:C_KIDX + 64].rearrange("(kk p) n -> p kk n", p=128), w=[wk])
    wq = k.sb([128, 8, 1536], BF16)
    k.dma("pool", wq[:, :, 0:1024], w_in[:, C_QATT:C_QATT + 1024].rearrange("(kk p) n -> p kk n", p=128), w=[wq])
    k.dma("pool", wq[:, :, 1024:1536], w_in[:, C_QIDX:C_QIDX + 512].rearrange("(kk p) n -> p kk n", p=128), w=[wq])
    wwi = k.sb([128, 8, 8], BF16)
    k.dma("pool", wwi[:], w_in[:, C_WIDX:C_WIDX + 8].rearrange("(kk p) n -> p kk n", p=128), w=[wwi])
    wukT = k.sb([128, 8, 256], BF16)
    k.dma("pool", wukT[:], io["w_ukT"].rearrange("h d c -> d h c"), w=[wukT])
    gB = k.sb([128, 256], F32)
    k.dma("sp", gB[:], bcast_rows(io["kv_norm_g"], 128), w=[gB])
    xbufs = [k.sb([128, 4, 1024], F32) for _ in range(2)]
    hbufs = [k.sb([128, 8, 512], BF16) for _ in range(2)]
    qi_sb = [k.sb([128, 4, 128], BF16) for _ in range(2)]
    qa_sb = [k.sb([128, 8, 128], BF16) for _ in range(2)]
    ql_sb = [k.sb([128, 2, 8, 128], BF16) for _ in range(2)]
    ckv_tok, ckvT, kidxT, widx = res["ckv_tok"], res["ckvT"], res["kidxT"], res["widx"]
    import os
    junks = [k.sb([128, 256], F32) for _ in range(4)]
    sss = [k.sb([128, 1], F32) for _ in range(4)]
    rstds = [k.sb([128, 1], F32) for _ in range(4)]
    ckx4 = [k.sb([128, 384], F32) for _ in range(4)]
    pts = [P["m"], P["t"]]
    for t in range(16):
        xt = xbufs[t % 2]
        hT = hbufs[t % 2]
        for tb in range(4):
            k.dma("sp", xt[:, tb, :], io["xrow"](t * 4 + tb), r=[io["xb"]], w=[xt])
        transpose_modulate(k, c, xt, hT, P["tr"], scp1, sh1, 4)

        def kblock(tb):
            blk = t * 4 + tb
            pc = P["c"][tb % 2]
            ckx, junk_, ss_, rstd_ = ckx4[tb], junks[tb], sss[tb], rstds[tb]
            for kk in range(8):
                k.mm(pc[:, 0:320], hT[:, kk, tb * 128:(tb + 1) * 128], wk[:, kk, :], start=(kk == 0), stop=(kk == 7),
                     r=[hT, wk], w=[pc])
            yield
            k.act(junk_[:], pc[:, 0:256], AF.Square, accum_out=ss_[:, 0:1], r=[pc], w=[junk_, ss_])
            yield
            k.ts("dve", rstd_[:], ss_[:], 1.0 / 256, RMS_EPS, ALU.mult, ALU.add, r=[ss_], w=[rstd_])
            k.act(rstd_[:], rstd_[:], AF.Sqrt, r=[rstd_], w=[rstd_])
            yield
            k.op("dve", lambda: nc.vector.reciprocal(out=rstd_[:], in_=rstd_[:]), r=[rstd_], w=[rstd_])
            k.stt("dve", ckx[:, 0:256], pc[:, 0:256], rstd_[:, 0:1], gB[:], ALU.mult, ALU.mult, r=[pc, rstd_, gB], w=[ckx])
            k.copy("dve", ckx[:, 256:320], pc[:, 256:320], r=[pc], w=[ckx])
            k.copy("dve", ckx[:, 320:384], pc[:, 256:320], r=[pc], w=[ckx])
            yield
            k.copy("pool", ckv_tok[:, blk, :], ckx[:, 0:256], r=[ckx], w=[ckv_tok])
            pt = pts[tb % 2]
            for m in range(3):
                k.tr(pt[:, m * 128:(m + 1) * 128], ckx[:, m * 128:(m + 1) * 128], c["ident_f"][:],
                     r=[ckx, c["ident_f"]], w=[pt])
            yield
            k.copy("act", ckvT[:, :, blk * 128:(blk + 1) * 128], pt[:, 0:256].rearrange("p (cc t) -> p cc t", cc=2),
                   r=[pt], w=[ckvT])
            k.copy("act", kidxT[:, blk * 128:(blk + 1) * 128], pt[:, 256:384], r=[pt], w=[kidxT])

        def qside():
            i = t
            ts_ = slice(jq * 128, (jq + 1) * 128)
            k.dma("sp", scr["hT_own"][i], hT[:, :, ts_], r=[hT], w=[scr["hT_own_b"][i]])
            qi = qi_sb[i % 2]
            pq = P["q"][0]
            for m in range(4):
                for kk in range(8):
                    k.mm(pq[:, m * 128:(m + 1) * 128], wq[:, kk, 1024 + m * 128:1024 + (m + 1) * 128], hT[:, kk, ts_],
                         start=(kk == 0), stop=(kk == 7), r=[wq, hT], w=[pq])
            yield
            k.copy("act", qi[:], pq[:, 0:512].rearrange("p (m t) -> p m t", m=4), r=[pq], w=[qi])
            k.dma("sp", scr["qidxT"][i], qi[:], r=[qi], w=[scr["qidxT_b"][i]])
            qa = qa_sb[i % 2]
            for half in range(2):
                pq = P["q"][1 - half]
                for m in range(4):
                    h = half * 4 + m
                    for kk in range(8):
                        k.mm(pq[:, m * 128:(m + 1) * 128], wq[:, kk, h * 128:(h + 1) * 128], hT[:, kk, ts_],
                             start=(kk == 0), stop=(kk == 7), r=[wq, hT], w=[pq])
                yield
                k.copy("dve", qa[:, half * 4:half * 4 + 4, :], pq[:, 0:512].rearrange("p (m t) -> p m t", m=4), r=[pq], w=[qa])
            ql = ql_sb[i % 2]
            for cc in range(2):
                for half in range(2):
                    pq = P["q"][(cc * 2 + half) % 2]
                    for m in range(4):
                        h = half * 4 + m
                        k.mm(pq[:, m * 128:(m + 1) * 128], wukT[:, h, cc * 128:(cc + 1) * 128], qa[:, h, :],
                             start=True, stop=True, r=[wukT, qa], w=[pq])
                    yield
                    k.act(ql[:, cc, half * 4:half * 4 + 4, :], pq[:, 0:512].rearrange("p (m t) -> p m t", m=4), AF.Copy,
                          scale=ATT_SCALE, r=[pq], w=[ql])
            k.dma("sp", scr["qlatT"][i], ql[:], r=[ql], w=[scr["qlatT_b"][i]])
            pw = P["q"][0]
            for kk in range(8):
                k.mm(pw[:, 0:8], hT[:, kk, ts_], wwi[:, kk, :], start=(kk == 0), stop=(kk == 7), r=[hT, wwi], w=[pw])
            yield
            k.copy("dve", widx[:, i, :], pw[:, 0:8], r=[pw], w=[widx])

        qs = qside()
        for grp in ([kblock(0), kblock(1), qs], [kblock(2), kblock(3), qs]):
            alive = [True] * len(grp)
            while any(alive[0:2]):
                for gi_ in range(len(grp)):
                    if alive[gi_]:
                        try:
                            next(grp[gi_])
                        except StopIteration:
                            alive[gi_] = False
        for _ in qs:
            pass
    k.st = k_st
    return st


def barrier(k):
    for e in k.eng:
        for f in k.eng:
            if f != e and k.cnt[f] > 0:
                k._wait(e, ("e", f, k.cnt[f]))
        for i in range(k.NDS):
            if k.dcnt[i] > 0:
                k._wait(e, ("d", i, k.dcnt[i]))


def dram(nc, name, shape, dtype, kind):
    return nc.dram_tensor(name, list(shape), dtype, kind=kind).ap()


def alloc_psum(k):
    P = {}
    P["tr"] = [k.ps([128, 512], F32) for _ in range(2)]
    P["c"] = [k.ps([128, 512], F32) for _ in range(2)]
    P["q"] = [k.ps([128, 512], F32) for _ in range(2)]
    P["t"] = k.ps([128, 512], F32)
    P["m"] = k.ps([128, 512], F32)
    return P


def alloc_scratch_B(nc):
    scr = {}
    scr["hT_own"] = [dram(nc, "s_hT%d" % i, [128, 8, 128], BF16, "Internal") for i in range(16)]
    scr["hT_own_b"] = [Buf() for _ in range(16)]
    scr["qidxT"] = [dram(nc, "s_qi%d" % i, [128, 4, 128], BF16, "Internal") for i in range(16)]
    scr["qidxT_b"] = [Buf() for _ in range(16)]
    scr["qlatT"] = [dram(nc, "s_ql%d" % i, [128, 2, 8, 128], BF16, "Internal") for i in range(16)]
    scr["qlatT_b"] = [Buf() for _ in range(16)]
    scr["yattT"] = [dram(nc, "s_ya%d" % i, [128, 8, 128], BF16, "Internal") for i in range(16)]
    scr["yattT_b"] = [Buf() for _ in range(16)]
    scr["xm"] = [dram(nc, "s_xm%d" % i, [128, D], F32, "Internal") for i in range(16)]
    scr["xm_b"] = [Buf() for _ in range(16)]
    return scr


def phase_dsa(k, c, P, io, jq, res, scr, nblk=16):
    nc = k.nc
    st = ExitStack()
    k_st, k.st = k.st, st
    ckv_tok, ckvT, kidxT, widx = res["ckv_tok"], res["ckvT"], res["kidxT"], res["widx"]
    wuv = k.sb([128, 8, 2, 128], BF16)
    k.dma("pool", wuv[:], io["w_uv"].rearrange("h (cc c) d -> c h cc d", c=128), w=[wuv])
    isc = k.sb([128, S], F32)
    junk = k.sb([128, S], BF16)
    maskT = k.sb([128, 64, 128], BF16)
    rl = [k.sb([128, 512], F32) for _ in range(2)]
    mk = [k.sb([128, 512], F32) for _ in range(2)]
    qis = [k.sb([128, 4, 128], BF16) for _ in range(2)]
    qls = [k.sb([128, 2, 8, 128], BF16) for _ in range(2)]
    es = [k.sb([128, 4, 128], BF16) for _ in range(2)]
    pts = [k.sb([128, 4, 128], BF16) for _ in range(2)]
    rden = k.sb([128, 512], F32)
    olat = k.sb([128, 2, 512], BF16)
    yas = [k.sb([128, 8, 128], BF16) for _ in range(2)]
    tmpd = k.sb([128, 128], F32)
    tmpp = k.sb([128, 384], F32)
    padb = k.sb([128, 384], F32)
    k.dma("sp", padb[:], io["padb"], w=[padb])
    half = k.sb([128, 1], F32)
    k.memset("dve", half[:], 0.5, w=[half])
    sm = {n: k.sb([128, 1], F32) for n in ("lo", "hi", "mid", "cnt", "sel", "d1", "d2", "m1", "m2", "m3")}
    for i in range(nblk):
        g = 4 * i + jq
        nk = (g + 1) * 128
        nkt = (nk + 511) // 512
        qi, ql, ya = qis[i % 2], qls[i % 2], yas[i % 2]
        k.dma("sp", qi[:], scr["qidxT"][i], r=[scr["qidxT_b"][i]], w=[qi])
        k.dma("sp", ql[:], scr["qlatT"][i], r=[scr["qlatT_b"][i]], w=[ql])
        for kt in range(nkt):
            w_ = min(512, nk - kt * 512)
            cols = slice(kt * 512, kt * 512 + w_)
            for h in range(8):
                m, po = h // 2, (h % 2) * 64
                ps = P["c"][h % 2]
                r_ = rl[h % 2]
                k.mm(ps[:, 0:w_], qi[po:po + 64, m, :], kidxT[po:po + 64, cols], start=True, stop=True,
                     r=[qi, kidxT], w=[ps])
                k.act(r_[:, 0:w_], ps[:, 0:w_], AF.Relu, r=[ps], w=[r_])
                if h == 0:
                    k.ts("dve", isc[:, cols], r_[:, 0:w_], widx[:, i, 0:1], None, ALU.mult, r=[r_, widx], w=[isc])
                else:
                    k.stt("dve", isc[:, cols], r_[:, 0:w_], widx[:, i, h:h + 1], isc[:, cols], ALU.mult, ALU.add,
                          r=[r_, widx, isc], w=[isc])
        dg = slice(nk - 128, nk)
        k.tt("dve", tmpp[:], isc[:, 0:384], padb[:], ALU.subtract, r=[isc, padb], w=[tmpp])
        k.tt("dve", isc[:, 0:384], isc[:, 0:384], padb[:], ALU.add, r=[isc, padb], w=[isc])
        k.op("dve", lambda: nc.vector.tensor_reduce(out=sm["m3"][:], in_=tmpp[:], axis=AX.X, op=ALU.min),
             r=[tmpp], w=[sm["m3"]])
        k.tt("dve", tmpd[:], isc[:, dg], c["cmask"][:], ALU.subtract, r=[isc, c["cmask"]], w=[tmpd])
        k.tt("dve", isc[:, dg], isc[:, dg], c["cmask"][:], ALU.add, r=[isc, c["cmask"]], w=[isc])
        k.op("dve", lambda: nc.vector.tensor_reduce(out=sm["hi"][:], in_=isc[:, 0:nk], axis=AX.X, op=ALU.max),
             r=[isc], w=[sm["hi"]])
        k.op("dve", lambda: nc.vector.tensor_reduce(out=sm["m2"][:], in_=tmpd[:], axis=AX.X, op=ALU.min),
             r=[tmpd], w=[sm["m2"]])
        k.tt("dve", sm["m2"][:], sm["m2"][:], sm["m3"][:], ALU.min, r=[sm["m3"], sm["m2"]], w=[sm["m2"]])
        if nk - 128 > 384:
            k.op("dve", lambda: nc.vector.tensor_reduce(out=sm["m1"][:], in_=isc[:, 384:nk - 128], axis=AX.X, op=ALU.min),
                 r=[isc], w=[sm["m1"]])
            k.tt("dve", sm["m2"][:], sm["m2"][:], sm["m1"][:], ALU.min, r=[sm["m1"], sm["m2"]], w=[sm["m2"]])
        k.ts("dve", sm["lo"][:], sm["m2"][:], -1.0, None, ALU.add, r=[sm["m2"]], w=[sm["lo"]])
        k.ts("dve", sm["hi"][:], sm["hi"][:], 1.0, None, ALU.add, r=[sm["hi"]], w=[sm["hi"]])
        lo, hi, mid, cnt, sel, d1, d2 = (sm[n] for n in ("lo", "hi", "mid", "cnt", "sel", "d1", "d2"))
        for it in range(26):
            k.stt("dve", mid[:], lo[:], hi[:, 0:1], half[:], ALU.add, ALU.mult, r=[lo, hi, half], w=[mid])
            k.memset("dve", cnt[:], 0.0, w=[cnt])
            k.ts("dve", junk[:, 0:nk], isc[:, 0:nk], mid[:, 0:1], 0.0, ALU.is_ge, ALU.add, r=[isc, mid, cnt],
                 w=[junk, cnt], accum_out=cnt[:, 0:1])
            k.ts("dve", sel[:], cnt[:], float(TOPK), None, ALU.is_ge, r=[cnt], w=[sel])
            k.tt("dve", d1[:], mid[:], lo[:], ALU.subtract, r=[mid, lo], w=[d1])
            k.tt("dve", d2[:], hi[:], mid[:], ALU.subtract, r=[mid, hi], w=[d2])
            k.stt("dve", lo[:], d1[:], sel[:, 0:1], lo[:], ALU.mult, ALU.add, r=[d1, sel, lo], w=[lo])
            k.stt("dve", hi[:], d2[:], sel[:, 0:1], mid[:], ALU.mult, ALU.add, r=[d2, sel, mid], w=[hi])
        for kt in range(nkt):
            w_ = min(512, nk - kt * 512)
            cols = slice(kt * 512, kt * 512 + w_)
            m_ = mk[kt % 2]
            k.ts("dve", m_[:, 0:w_], isc[:, cols], lo[:, 0:1], None, ALU.is_ge, r=[isc, lo], w=[m_])
            pt = P["t"]
            nb_ = w_ // 128
            for bb in range(nb_):
                k.tr(pt[:, bb * 128:(bb + 1) * 128], m_[:, bb * 128:(bb + 1) * 128], c["ident_f"][:],
                     r=[m_, c["ident_f"]], w=[pt])
            k.copy("act", maskT[:, kt * 4:kt * 4 + nb_, :], pt[:, 0:w_].rearrange("p (b t) -> p b t", b=nb_),
                   r=[pt], w=[maskT])
        for hg in range(2):
            acc = P["q"]
            den = P["m"]
            for kb in range(g + 1):
                psS = P["tr"][kb % 2]
                e, pT = es[kb % 2], pts[kb % 2]
                for cc in range(2):
                    k.mm(psS[:, :], ckvT[:, cc, kb * 128:(kb + 1) * 128],
                         ql[:, cc, hg * 4:hg * 4 + 4, :].rearrange("p h t -> p (h t)"),
                         start=(cc == 0), stop=(cc == 1), r=[ckvT, ql], w=[psS])
                k.act(e[:].rearrange("p h t -> p (h t)"), psS[:, :], AF.Exp, r=[psS], w=[e])
                for hh in range(4):
                    eng = "dve" if hh % 2 == 0 else "pool"
                    k.tt(eng, pT[:, hh, :], e[:, hh, :], maskT[:, kb, :], ALU.mult, r=[e, maskT], w=[pT])
                pf = pT[:].rearrange("p h t -> p (h t)")
                for cc in range(2):
                    k.mm(acc[cc][:, :], ckv_tok[:, kb, cc * 128:(cc + 1) * 128], pf, start=(kb == 0), stop=(kb == g),
                         r=[ckv_tok, pT], w=[acc[cc]])
                k.mm(den[:, :], c["ones_b"][:], pf, start=(kb == 0), stop=(kb == g), r=[c["ones_b"], pT], w=[den])
            k.op("dve", lambda: nc.vector.reciprocal(out=rden[:], in_=den[:, :]), r=[den], w=[rden])
            for cc in range(2):
                k.tt("dve", olat[:, cc, :], acc[cc][:, :], rden[:], ALU.mult, r=[acc[cc], rden], w=[olat])
            for hh in range(4):
                h = hg * 4 + hh
                psY = P["c"][hh % 2]
                for cc in range(2):
                    k.mm(psY[:, 0:128], wuv[:, h, cc, :], olat[:, cc, hh * 128:(hh + 1) * 128], start=(cc == 0),
                         stop=(cc == 1), r=[wuv, olat], w=[psY])
                k.copy("act", ya[:, h, :], psY[:, 0:128], r=[psY], w=[ya])
        k.dma("sp", scr["yattT"][i], ya[:], r=[ya], w=[scr["yattT_b"][i]])
    barrier(k)
    k.st = k_st
    st.close()


def layer_norm_tile(k, nc, x1, out, gB, bB, sm, junk):
    s1, s2, mean, var, rstd, nb = (sm[n] for n in ("s1", "s2", "mean", "var", "rstd", "nb"))
    k.op("dve", lambda: nc.vector.reduce_sum(out=s1[:], in_=x1, axis=AX.X), r=[sm["x1b"]], w=[s1])
    k.op("act", lambda: nc.scalar.memzero(s2[:]), (), [s2])
    k.act(junk[:], x1, AF.Square, accum_out=s2[:, 0:1], r=[sm["x1b"], s2], w=[junk, s2])
    k.ts("dve", mean[:], s1[:], 1.0 / D, None, ALU.mult, r=[s1], w=[mean])
    k.tt("dve", var[:], mean[:], mean[:], ALU.mult, r=[mean], w=[var])
    k.stt("dve", var[:], s2[:], 1.0 / D, var[:], ALU.mult, ALU.subtract, r=[s2, var], w=[var])
    k.ts("dve", var[:], var[:], LN_EPS, None, ALU.add, r=[var], w=[var])
    k.act(rstd[:], var[:], AF.Sqrt, r=[var], w=[rstd])
    k.op("dve", lambda: nc.vector.reciprocal(out=rstd[:], in_=rstd[:]), r=[rstd], w=[rstd])
    k.stt("dve", nb[:], mean[:], -1.0, rstd[:], ALU.mult, ALU.mult, r=[mean, rstd], w=[nb])
    k.act(junk[:], x1, AF.Identity, scale=rstd[:, 0:1], bias=nb[:, 0:1], r=[sm["x1b"], rstd, nb], w=[junk])
    k.tt("dve", junk[:], junk[:], gB[:], ALU.mult, r=[junk, gB], w=[junk])
    k.tt("dve", out, junk[:], bB[:], ALU.add, r=[junk, bB], w=[sm["outb"]])


def layer_norm_gen(k, nc, x1, out, gB, bB, sm, junk):
    s1, s2, mean, var, rstd, nb = (sm[n] for n in ("s1", "s2", "mean", "var", "rstd", "nb"))
    k.op("dve", lambda: nc.vector.reduce_sum(out=s1[:], in_=x1, axis=AX.X), r=[sm["x1b"]], w=[s1])
    k.act(junk[:], x1, AF.Square, accum_out=s2[:, 0:1], r=[sm["x1b"]], w=[junk, s2])
    yield
    k.ts("dve", mean[:], s1[:], 1.0 / D, None, ALU.mult, r=[s1], w=[mean])
    k.tt("dve", var[:], mean[:], mean[:], ALU.mult, r=[mean], w=[var])
    k.stt("dve", var[:], s2[:], 1.0 / D, var[:], ALU.mult, ALU.subtract, r=[s2, var], w=[var])
    k.ts("dve", var[:], var[:], LN_EPS, None, ALU.add, r=[var], w=[var])
    yield
    k.act(rstd[:], var[:], AF.Sqrt, r=[var], w=[rstd])
    yield
    k.op("dve", lambda: nc.vector.reciprocal(out=rstd[:], in_=rstd[:]), r=[rstd], w=[rstd])
    k.stt("dve", nb[:], mean[:], -1.0, rstd[:], ALU.mult, ALU.mult, r=[mean, rstd], w=[nb])
    yield
    k.act(junk[:], x1, AF.Identity, scale=rstd[:, 0:1], bias=nb[:, 0:1], r=[sm["x1b"], rstd, nb], w=[junk])
    yield
    k.tt("dve", junk[:], junk[:], gB[:], ALU.mult, r=[junk, gB], w=[junk])
    k.tt("dve", out, junk[:], bB[:], ALU.add, r=[junk, bB], w=[sm["outb"]])


def gate_rows(k, c, P, io, which, condB, out):
    st = ExitStack()
    k_st, k.st = k.st, st
    wb = [k.sb([128, 8, 512], F32) for _ in range(2)]
    for half in range(2):
        c0 = which * 1024 + half * 512
        k.dma("sp", wb[half][:], io["w_ada"][:, c0:c0 + 512].rearrange("(kk p) n -> p kk n", p=128), w=[wb[half]])
        ps = P["c"][half]
        for kk in range(8):
            k.mm(ps[:, :], condB[:, kk, :], wb[half][:, kk, :], start=(kk == 0), stop=(kk == 7), r=[condB, wb[half]], w=[ps])
        bb = k.sb([128, 512], F32)
        k.dma("sp", bb[:], bcast_rows(io["b_ada"][c0:c0 + 512], 128), w=[bb])
        k.stt("dve", out[:, half * 512:(half + 1) * 512], ps[:, :], 1.0, bb[:], ALU.add, ALU.add, r=[ps, bb], w=[out])
    barrier(k)
    k.st = k_st
    st.close()


def make_condB(k, io):
    condT = k.sb([128, 8], F32)
    k.dma("sp", condT[:], io["cT"], w=[condT])
    sig = k.sb([128, 8], F32)
    k.act(sig[:], condT[:], AF.Sigmoid, r=[condT], w=[sig])
    k.tt("dve", condT[:], condT[:], sig[:], ALU.mult, r=[condT, sig], w=[condT])
    condB = k.sb([128, 8, 128], F32)
    for kk in range(8):
        k.ts("dve", condB[:, kk, :], k.c["ones_f"][:], condT[:, kk:kk + 1], None, ALU.mult, r=[k.c["ones_f"], condT], w=[condB])
    return condB


def phase_merge(k, c, P, io, jq, res, scr, lyr):
    nc = k.nc
    st = ExitStack()
    k_st, k.st = k.st, st
    G1 = res["G1"]
    xo = [k.sb([128, D], F32) for _ in range(2)]
    wA = k.sb([128, 8, 1024], BF16)
    wDn = k.sb([128, 8, 1024], BF16)
    wO = k.sb([128, 8, 1024], BF16)
    wG = k.sb([128, 8, 2048], BF16)
    for dst, src in ((wA, io["w_br_att"]), (wDn, io["w_br_dn"]), (wO, io["w_out"])):
        k.dma("pool", dst[:], src.rearrange("(kk p) n -> p kk n", p=128), w=[dst])
    k.dma("pool", wG[:], io["w_in"][:, C_GATES:C_GATES + 2048].rearrange("(kk p) n -> p kk n", p=128), w=[wG])
    gB = k.sb([128, D], F32)
    bB = k.sb([128, D], F32)
    k.dma("sp", gB[:], bcast_rows(io["ln_g"][0], 128), w=[gB])
    k.dma("sp", bB[:], bcast_rows(io["ln_b"][0], 128), w=[bB])
    yaT = k.sb([128, 8, 512], BF16)
    ydT = k.sb([128, 8, 512], BF16)
    hT = k.sb([128, 8, 512], BF16)
    xt = k.sb([128, 4, D], F32)
    mg = k.sb([128, 8, 512], BF16)
    sg = [k.sb([128, 512], F32) for _ in range(2)]
    m1 = k.sb([128, 512], F32)
    m2 = k.sb([128, 512], F32)
    x1s = [k.sb([128, D], F32) for _ in range(2)]
    junks = [k.sb([128, D], F32) for _ in range(2)]
    sms = [{n: k.sb([128, 1], F32) for n in ("s1", "s2", "mean", "var", "rstd", "nb")} for _ in range(2)]
    for tl in range(4):
        for bb in range(4):
            i = tl * 4 + bb
            g = 4 * i + jq
            ts_ = slice(bb * 128, (bb + 1) * 128)
            k.dma("sp", yaT[:, :, ts_], scr["yattT"][i], r=[scr["yattT_b"][i]], w=[yaT])
            for h_ in range(8):
                k.dma("sp", ydT[:, h_, ts_], io["ycol"](i, h_), r=[io["yb"]], w=[ydT])
            k.dma("sp", hT[:, :, ts_], scr["hT_own"][i], r=[scr["hT_own_b"][i]], w=[hT])
            k.dma("sp", xt[:, bb, :], io["xrow"](g), r=[io["xb"]], w=[xt])
        for n in range(8):
            ns = slice(n * 128, (n + 1) * 128)
            psA, psG = P["tr"][0], P["tr"][1]
            for kk in range(8):
                k.mm(psA[:, :], wA[:, kk, ns], yaT[:, kk, :], start=(kk == 0), stop=(kk == 7), r=[wA, yaT], w=[psA])
            for kk in range(8):
                k.mm(psG[:, :], wG[:, kk, ns], hT[:, kk, :], start=(kk == 0), stop=(kk == 7), r=[wG, hT], w=[psG])
            k.act(sg[0][:], psG[:, :], AF.Sigmoid, r=[psG], w=[sg[0]])
            k.tt("dve", m1[:], psA[:, :], sg[0][:], ALU.mult, r=[psA, sg[0]], w=[m1])
            psD, psG2 = P["q"][0], P["q"][1]
            for kk in range(8):
                k.mm(psD[:, :], wDn[:, kk, ns], ydT[:, kk, :], start=(kk == 0), stop=(kk == 7), r=[wDn, ydT], w=[psD])
            for kk in range(8):
                k.mm(psG2[:, :], wG[:, kk, 1024 + n * 128:1024 + (n + 1) * 128], hT[:, kk, :], start=(kk == 0),
                     stop=(kk == 7), r=[wG, hT], w=[psG2])
            k.act(sg[1][:], psG2[:, :], AF.Sigmoid, r=[psG2], w=[sg[1]])
            k.tt("dve", m2[:], psD[:, :], sg[1][:], ALU.mult, r=[psD, sg[1]], w=[m2])
            k.tt("dve", mg[:, n, :], m1[:], m2[:], ALU.add, r=[m1, m2], w=[mg])
        def y_block(bb, banks, s_):
            i = tl * 4 + bb
            x1, sm = x1s[s_], sms[s_]
            sm["x1b"] = x1.b
            for hf in range(2):
                psY = banks[hf]
                hs = slice(hf * 512, (hf + 1) * 512)
                for n in range(8):
                    k.mm(psY[:, :], mg[:, n, bb * 128:(bb + 1) * 128], wO[:, n, hs], start=(n == 0), stop=(n == 7),
                         r=[mg, wO], w=[psY])
                yield
                k.tt("dve", x1[:, hs], psY[:, :], G1[:, hs], ALU.mult, r=[psY, G1], w=[x1])
                k.stt("dve", x1[:, hs], xt[:, bb, hs], ALPHA, x1[:, hs], ALU.mult, ALU.add, r=[xt, x1], w=[x1])
            yield
            xo_ = xo[i % 2]
            sm["outb"] = xo_.b
            for _ in layer_norm_gen(k, nc, x1[:], xo_[:], gB, bB, sm, junks[s_]):
                yield
            k.dma("sp", scr["xm"][i], xo_[:], r=[xo_], w=[scr["xm_b"][i]])

        for pr_ in range(2):
            gens = [y_block(2 * pr_, P["c"], 0), y_block(2 * pr_ + 1, [P["m"], P["t"]], 1)]
            alive = [True, True]
            while any(alive):
                for gi_ in range(2):
                    if alive[gi_]:
                        try:
                            next(gens[gi_])
                        except StopIteration:
                            alive[gi_] = False
    barrier(k)
    k.st = k_st
    st.close()


def phase_moe(k, c, P, io, jq, res, scr, out_d, on_block=None):
    nc = k.nc
    st = ExitStack()
    k_st, k.st = k.st, st
    G2, modT = res["G2"], res["modT"]
    scp1 = k.sb([128, 8], F32)
    k.ts("dve", scp1[:], modT[:, 4, :], 1.0, None, ALU.add, r=[modT], w=[scp1])
    sh2 = modT[:, 3, :]
    h2T = k.sb([128, 8, 2048], BF16)
    Gm_all = k.sb([128, 16, 32], F32)
    wr = k.sb([128, 8, 36], F32)
    k.dma("sp", wr[:, :, 0:4], io["w_route_grp"].rearrange("(kk p) n -> p kk n", p=128), w=[wr])
    k.dma("sp", wr[:, :, 4:36], io["w_route_exp"].rearrange("(kk p) n -> p kk n", p=128), w=[wr])
    st2 = ExitStack()
    k.st = st2
    h2f = k.sb([128, 8, 512], F32)
    xts = [k.sb([128, 4, D], F32) for _ in range(2)]
    lg = k.sb([128, 36], F32)
    Gm = k.sb([128, 4, 8], F32)
    s = {n: k.sb([128, 1], F32) for n in ("gmax", "ngmax", "sg", "tgp", "m1", "m2", "dm", "g1", "g2")}
    selg = k.sb([128, 4], F32)
    eg = k.sb([128, 4], F32)
    ing = k.sb([128, 8], F32)
    oh1 = k.sb([128, 8], F32)
    oh2 = k.sb([128, 8], F32)
    x2 = k.sb([128, 8], F32)
    ge = k.sb([128, 8], F32)
    for tl in range(4):
        xt_ = xts[tl % 2]
        for bb in range(4):
            k.dma("sp", xt_[:, bb, :], scr["xm"][tl * 4 + bb], r=[scr["xm_b"][tl * 4 + bb]], w=[xt_])
        transpose_modulate(k, c, xt_, T(h2T.t[:, :, tl * 512:(tl + 1) * 512], h2T.b),
                           P["tr"], scp1, sh2, 4, extra=h2f)
        for bb in range(4):
            i = tl * 4 + bb
            pl = P["t"]
            for kk in range(8):
                k.mm(pl[:, 0:36], h2f[:, kk, bb * 128:(bb + 1) * 128], wr[:, kk, :], start=(kk == 0), stop=(kk == 7),
                     r=[h2f, wr], w=[pl])
            k.copy("dve", lg[:], pl[:, 0:36], r=[pl], w=[lg])
            grp = lg[:, 0:4]
            k.op("dve", lambda: nc.vector.tensor_reduce(out=s["gmax"][:], in_=grp, axis=AX.X, op=ALU.max), r=[lg], w=[s["gmax"]])
            k.ts("dve", selg[:], grp, s["gmax"][:, 0:1], None, ALU.is_equal, r=[lg, s["gmax"]], w=[selg])
            k.ts("dve", s["ngmax"][:], s["gmax"][:], -1.0, None, ALU.mult, r=[s["gmax"]], w=[s["ngmax"]])
            k.op("act", lambda: nc.scalar.memzero(s["sg"][:]), (), [s["sg"]])
            k.act(eg[:], grp, AF.Exp, bias=s["ngmax"][:, 0:1], accum_out=s["sg"][:, 0:1], r=[lg, s["ngmax"], s["sg"]],
                  w=[eg, s["sg"]])
            k.op("dve", lambda: nc.vector.reciprocal(out=s["tgp"][:], in_=s["sg"][:]), r=[s["sg"]], w=[s["tgp"]])
            for g_ in range(4):
                le = lg[:, 4 + g_ * 8:12 + g_ * 8]
                if g_ == 0:
                    k.ts("dve", ing[:], le, selg[:, 0:1], None, ALU.mult, r=[lg, selg], w=[ing])
                else:
                    k.stt("dve", ing[:], le, selg[:, g_:g_ + 1], ing[:], ALU.mult, ALU.add, r=[lg, selg, ing], w=[ing])
            k.op("dve", lambda: nc.vector.tensor_reduce(out=s["m1"][:], in_=ing[:], axis=AX.X, op=ALU.max), r=[ing], w=[s["m1"]])
            k.ts("dve", oh1[:], ing[:], s["m1"][:, 0:1], None, ALU.is_equal, r=[ing, s["m1"]], w=[oh1])
            k.stt("dve", x2[:], oh1[:], NEG, ing[:], ALU.mult, ALU.add, r=[oh1, ing], w=[x2])
            k.op("dve", lambda: nc.vector.tensor_reduce(out=s["m2"][:], in_=x2[:], axis=AX.X, op=ALU.max), r=[x2], w=[s["m2"]])
            k.ts("dve", oh2[:], x2[:], s["m2"][:, 0:1], None, ALU.is_equal, r=[x2, s["m2"]], w=[oh2])
            k.tt("dve", s["dm"][:], s["m2"][:], s["m1"][:], ALU.subtract, r=[s["m1"], s["m2"]], w=[s["dm"]])
            k.act(s["g2"][:], s["dm"][:], AF.Sigmoid, r=[s["dm"]], w=[s["g2"]])
            k.tt("dve", s["g2"][:], s["g2"][:], s["tgp"][:], ALU.mult, r=[s["g2"], s["tgp"]], w=[s["g2"]])
            k.tt("dve", s["g1"][:], s["tgp"][:], s["g2"][:], ALU.subtract, r=[s["g2"], s["tgp"]], w=[s["g1"]])
            k.ts("dve", ge[:], oh1[:], s["g1"][:, 0:1], None, ALU.mult, r=[oh1, s["g1"]], w=[ge])
            k.stt("dve", ge[:], oh2[:], s["g2"][:, 0:1], ge[:], ALU.mult, ALU.add, r=[oh2, s["g2"], ge], w=[ge])
            for g_ in range(4):
                k.ts("dve", Gm[:, g_, :], ge[:], selg[:, g_:g_ + 1], None, ALU.mult, r=[ge, selg], w=[Gm])
            k.copy("dve", Gm_all[:, i, :], Gm[:].rearrange("p g e -> p (g e)"), r=[Gm], w=[Gm_all])
    barrier(k)
    k.st = st
    st2.close()
    yacc = k.sb([128, 16, D], F32)
    st3 = ExitStack()
    k.st = st3
    wgs = [k.sb([128, 8, 512], BF16) for _ in range(2)]
    wus = [k.sb([128, 8, 512], BF16) for _ in range(2)]
    wds = [k.sb([128, 4, 1024], BF16) for _ in range(2)]
    sgt = [k.sb([128, 512], F32) for _ in range(2)]
    hp = k.sb([128, 4, 512], BF16)
    hps = [hp, k.sb([128, 4, 512], BF16)]

    def load_w(e):
        wg, wu, wd = wgs[e % 2], wus[e % 2], wds[e % 2]
        k.dma("pool", wg[:], io["w_gate"][e].rearrange("(kk p) n -> p kk n", p=128), w=[wg])
        k.dma("pool", wu[:], io["w_up"][e].rearrange("(kk p) n -> p kk n", p=128), w=[wu])
        k.dma("pool", wd[:], io["w_down"][e].rearrange("(kk p) n -> p kk n", p=128), w=[wd])

    def gate_up(s_):
        e, tl = s_ // 4, s_ % 4
        if tl == 0:
            load_w(e)
        wg, wu = wgs[e % 2], wus[e % 2]
        tsl = slice(tl * 512, (tl + 1) * 512)
        hp_ = hps[s_ % 2]
        for f in range(4):
            fs = slice(f * 128, (f + 1) * 128)
            psg, psu = P["tr"][f % 2], P["q"][f % 2]
            for kk in range(8):
                k.mm(psg[:, :], wg[:, kk, fs], h2T[:, kk, tsl], start=(kk == 0), stop=(kk == 7), r=[wg, h2T], w=[psg], inc=(kk == 7))
            for kk in range(8):
                k.mm(psu[:, :], wu[:, kk, fs], h2T[:, kk, tsl], start=(kk == 0), stop=(kk == 7), r=[wu, h2T], w=[psu], inc=(kk == 7))
            sg_ = sgt[f % 2]
            k.act(sg_[:], psg[:, :], AF.Silu, r=[psg], w=[sg_])
            k.tt("dve", hp_[:, f, :], psu[:, :], sg_[:], ALU.mult, r=[psu, sg_], w=[hp_])

    def down(s_):
        e, tl = s_ // 4, s_ % 4
        wd = wds[e % 2]
        hp_ = hps[s_ % 2]
        for bb in range(4):
            i = tl * 4 + bb
            for hf in range(2):
                psy = P["c"][hf]
                hs = slice(hf * 512, (hf + 1) * 512)
                for f in range(4):
                    k.mm(psy[:, :], hp_[:, f, bb * 128:(bb + 1) * 128], wd[:, f, hs], start=(f == 0), stop=(f == 3),
                         r=[hp_, wd], w=[psy], inc=(f == 3))
                if e == 0:
                    k.ts("dve", yacc[:, i, hs], psy[:, :], Gm_all[:, i, e:e + 1], None, ALU.mult, r=[psy, Gm_all], w=[yacc])
                else:
                    k.stt("dve", yacc[:, i, hs], psy[:, :], Gm_all[:, i, e:e + 1], yacc[:, i, hs], ALU.mult, ALU.add,
                          r=[psy, Gm_all, yacc], w=[yacc])

    gate_up(0)
    for s_ in range(128):
        if s_ + 1 < 128:
            gate_up(s_ + 1)
        down(s_)
    barrier(k)
    k.st = st
    st3.close()
    gB = k.sb([128, D], F32)
    bB = k.sb([128, D], F32)
    k.dma("sp", gB[:], bcast_rows(io["ln_g"][1], 128), w=[gB])
    k.dma("sp", bB[:], bcast_rows(io["ln_b"][1], 128), w=[bB])
    xr = [k.sb([128, D], F32) for _ in range(2)]
    x1s = [k.sb([128, D], F32) for _ in range(2)]
    junks = [k.sb([128, D], F32) for _ in range(2)]
    ob = [k.sb([128, D], F32) for _ in range(2)]
    sms = [{n: k.sb([128, 1], F32) for n in ("s1", "s2", "mean", "var", "rstd", "nb")} for _ in range(2)]
    outb = [Buf() for _ in range(16)]

    def ln_block(i):
        s_ = i % 2
        xr_, o_, x1, sm = xr[s_], ob[s_], x1s[s_], sms[s_]
        sm["x1b"] = x1.b
        sm["outb"] = o_.b
        k.dma("sp", xr_[:], scr["xm"][i], r=[scr["xm_b"][i]], w=[xr_])
        k.tt("dve", x1[:], yacc[:, i, :], G2[:], ALU.mult, r=[yacc, G2], w=[x1])
        k.stt("dve", x1[:], xr_[:], ALPHA, x1[:], ALU.mult, ALU.add, r=[xr_, x1], w=[x1])
        yield
        for _ in layer_norm_gen(k, nc, x1[:], o_[:], gB, bB, sm, junks[s_]):
            yield
        k.dma("sp", out_d[i * 128:(i + 1) * 128, :], o_[:], r=[o_], w=[outb[i]])

    for i2 in range(8):
        gens = [ln_block(2 * i2), ln_block(2 * i2 + 1)]
        alive = [True, True]
        while any(alive):
            for gi_ in range(2):
                if alive[gi_]:
                    try:
                        next(gens[gi_])
                    except StopIteration:
                        alive[gi_] = False
        if on_block is not None:
            on_block(2 * i2 + 1, outb)
    barrier(k)
    k.st = k_st
    st.close()
    return outb


B_INPUTS = [("x", [S, D], F32), ("cT", [128, 8], F32), ("w_ada", [D, 6 * D], F32), ("b_adaT", [128, 48], F32),
            ("b_ada", [6 * D], F32), ("w_in", [D, N_IN], F32), ("w_ukT", [8, 128, 256], F32), ("w_uv", [8, 256, 128], F32),
            ("kv_norm_g", [256], F32), ("y_dnT", [D, S], BF16), ("w_br_att", [D, D], F32), ("w_br_dn", [D, D], F32),
            ("w_out", [D, D], F32), ("ln_g", [2, D], F32), ("ln_b", [2, D], F32), ("w_route_grp", [D, 4], F32),
            ("w_route_exp", [D, 32], F32), ("w_gate", [32, D, 512], F32), ("w_up", [32, D, 512], F32),
            ("w_down", [32, 512, D], F32), ("padb", [128, 384], F32)]


def build_B(jq):
    nc = bass.Bass("TRN2", target_bir_lowering=False)
    io = {n: dram(nc, n, shp, dt, "ExternalInput") for n, shp, dt in B_INPUTS}
    out_d = dram(nc, "out", [2048, D], F32, "ExternalOutput")
    io["xrow"] = lambda p: io["x"][p * 128:(p + 1) * 128, :]
    io["xb"] = Buf()
    io["ycol"] = lambda i, h: io["y_dnT"][h * 128:(h + 1) * 128, (4 * i + jq) * 128:(4 * i + jq + 1) * 128]
    io["yb"] = Buf()
    scr = alloc_scratch_B(nc)
    with ExitStack() as st:
        k = K(nc, st)
        c = make_consts(k)
        k.c = c
        P = alloc_psum(k)
        res = {}
        res["modT"] = adaln_cols(k, c, P["m"], io["cT"], io["w_ada"], io["b_adaT"], [0, 1, 2, 3, 4, 5])
        res["G1"] = k.sb([128, D], F32)
        res["G2"] = k.sb([128, D], F32)
        stc = ExitStack()
        k_st, k.st = k.st, stc
        condB = make_condB(k, io)
        gate_rows(k, c, P, io, 2, condB, res["G1"])
        gate_rows(k, c, P, io, 5, condB, res["G2"])
        barrier(k)
        k.st = k_st
        stc.close()
        sta = ExitStack()
        k.st = sta
        res["ckv_tok"] = k.sb([128, 64, 256], BF16)
        res["ckvT"] = k.sb([128, 2, S], BF16)
        res["kidxT"] = k.sb([128, S], BF16)
        res["widx"] = k.sb([128, 16, 8], F32)
        stp = phase_kq(k, c, P, io, jq, res, scr)
        barrier(k)
        stp.close()
        DSA_IMPL[0](k, c, P, io, jq, res, scr)
        k.st = k_st
        sta.close()
        phase_merge(k, c, P, io, jq, res, scr, 0)
        outb = phase_moe(k, c, P, io, jq, res, scr, out_d)
        k.finish(outb)
        barrier(k)
    return nc


A_INPUTS = [("x", [S, D], F32), ("cT", [128, 8], F32), ("w_ada", [D, 6 * D], F32), ("b_adaT", [128, 48], F32),
            ("w_dn", [D, 1028], F32), ("convw", [128, 3, 2, 4], F32), ("alog", [2, 1], F32), ("dtb", [2, 1], F32),
            ("normg", [128, 1], F32)]


def build_A(hp, ntiles=16):
    nc = bass.Bass("TRN2", target_bir_lowering=False)
    io = {n: dram(nc, n, shp, dt, "ExternalInput") for n, shp, dt in A_INPUTS}
    y_out = dram(nc, "y_dnT", [256, S], BF16, "ExternalOutput")
    io["xrow_true"] = lambda p: io["x"][p * 128:(p + 1) * 128, :]
    io["xb"] = Buf()
    with ExitStack() as st:
        k = K(nc, st)
        c = make_consts(k)
        k.c = c
        P = alloc_psum(k)
        modT = adaln_cols(k, c, P["m"], io["cT"], io["w_ada"], io["b_adaT"], [0, 1])
        outb = DN_IMPL[0](k, c, P, io, modT, hp, y_out, ntiles)
        k.finish([outb])
        barrier(k)
    return nc


def phase_dn(k, c, P, io, modT, hp, y_out, ntiles=16):
    nc = k.nc
    if True:
        st = ExitStack()
        k_st, k.st = k.st, st
        scp1 = k.sb([128, 8], F32)
        k.ts("dve", scp1[:], modT[:, 1, :], 1.0, None, ALU.add, r=[modT], w=[scp1])
        sh1 = modT[:, 0, :]
        w_dn = io["w_dn"]
        wq = k.sb([128, 8, 4, 2, 128], BF16)
        for si in range(4):
            k.dma("pool", wq[:, :, si, :, :].rearrange("p kk hh n -> p kk (hh n)"),
                  w_dn[:, si * 256:si * 256 + 256].rearrange("(kk p) n -> p kk n", p=128), w=[wq])
        wa = k.sb([128, 8, 2], BF16)
        wb = k.sb([128, 8, 2], BF16)
        k.dma("pool", wa[:], w_dn[:, 1024:1026].rearrange("(kk p) n -> p kk n", p=128), w=[wa])
        k.dma("pool", wb[:], w_dn[:, 1026:1028].rearrange("(kk p) n -> p kk n", p=128), w=[wb])
        cw = k.sb([128, 3, 2, 4], F32)
        k.dma("sp", cw[:], io["convw"], w=[cw])
        alog = k.sb([2, 1], F32)
        dtb = k.sb([2, 1], F32)
        normg = k.sb([128, 1], F32)
        k.dma("sp", alog[:], io["alog"], w=[alog])
        k.dma("sp", dtb[:], io["dtb"], w=[dtb])
        k.dma("sp", normg[:], io["normg"], w=[normg])
        nA = k.sb([2, 1], F32)
        k.act(nA[:], alog[:], AF.Exp, r=[alog], w=[nA])
        k.ts("dve", nA[:], nA[:], -1.0, None, ALU.mult, r=[nA], w=[nA])
        SEL2 = k.sb([2, 2, 128], F32)
        k.memset("pool", SEL2[:], 1.0, w=[SEL2])
        k.op("pool", lambda: nc.gpsimd.affine_select(out=SEL2[:], in_=SEL2[:], pattern=[[-1, 2], [0, 128]],
                                                      compare_op=ALU.is_equal, fill=0.0, base=0, channel_multiplier=1),
             r=[SEL2], w=[SEL2])
        xbufs = [k.sb([128, 4, D], F32) for _ in range(2)]
        hT = k.sb([128, 8, 512], BF16)
        pre = [[k.sb([128, 515], F32) for _ in range(2)] for _ in range(3)]
        for s_ in range(3):
            for hh in range(2):
                k.memset("pool", pre[s_][hh][:, 0:3], 0.0, w=[pre[s_][hh]])
        qkv = [[k.sb([128, 512], F32) for _ in range(2)] for _ in range(3)]
        zs = [k.sb([128, 512], F32) for _ in range(2)]
        acc = k.sb([128, 512], F32)
        sq = k.sb([128, 512], F32)
        rn = k.sb([128, 512], F32)
        arow = k.sb([2, 512], F32)
        brow = k.sb([2, 512], F32)
        aB = [k.sb([128, 512], F32) for _ in range(2)]
        nbB = [k.sb([128, 512], F32) for _ in range(2)]
        Sst = [k.sb([128, 128], F32) for _ in range(2)]
        for hh in range(2):
            k.memset("pool", Sst[hh][:], 0.0, w=[Sst[hh]])
        vp = [k.sb([128, 1], F32) for _ in range(2)]
        VL = [k.sb([128, 128], F32) for _ in range(2)]
        oT = k.sb([128, 512], F32)
        yb = [k.sb([128, 512], BF16) for _ in range(2)]
        outb = Buf()
        pc1 = [P["c"][0], P["c"][1]]
        pVB = [P["q"][0], P["q"][1]]
        pso = [P["tr"][0], P["tr"][1]]
        for t in range(ntiles):
            xt = xbufs[t % 2]
            for tb in range(4):
                k.dma("sp", xt[:, tb, :], io["xrow_true"](t * 4 + tb), r=[io.get("xb_true", io["xb"])], w=[xt])
            transpose_modulate(k, c, xt, hT, [P["m"], P["t"]], scp1, sh1, 4)
            pa = P["m"]
            for kk in range(8):
                k.mm(pa[0:2, :], wa[:, kk, :], hT[:, kk, :], start=(kk == 0), stop=(kk == 7), r=[wa, hT], w=[pa])
            k.act(arow[:], pa[0:2, :], AF.Exp, bias=dtb[:, 0:1], r=[pa, dtb], w=[arow])
            k.ts("dve", arow[:], arow[:], 1.0, None, ALU.add, r=[arow], w=[arow])
            k.act(arow[:], arow[:], AF.Ln, r=[arow], w=[arow])
            k.act(arow[:], arow[:], AF.Exp, scale=nA[:, 0:1], r=[arow, nA], w=[arow])
            pb_ = P["t"]
            for kk in range(8):
                k.mm(pb_[0:2, :], wb[:, kk, :], hT[:, kk, :], start=(kk == 0), stop=(kk == 7), r=[wb, hT], w=[pb_])
            k.act(brow[:], pb_[0:2, :], AF.Sigmoid, r=[pb_], w=[brow])
            k.ts("dve", brow[:], brow[:], -1.0, None, ALU.mult, r=[brow], w=[brow])
            for hh in range(2):
                k.mm(pa[:, :], SEL2[:, hh, :], arow[:], start=True, stop=True, r=[SEL2, arow], w=[pa])
                k.copy("act", aB[hh][:], pa[:, :], r=[pa], w=[aB[hh]])
                k.mm(pb_[:, :], SEL2[:, hh, :], brow[:], start=True, stop=True, r=[SEL2, brow], w=[pb_])
                k.copy("act", nbB[hh][:], pb_[:, :], r=[pb_], w=[nbB[hh]])
            for hh in range(2):
                for s_ in range(4):
                    pp = P["m"] if s_ % 2 == 0 else P["t"]
                    for kk in range(8):
                        k.mm(pp[:, :], wq[:, kk, s_, hh, :], hT[:, kk, :], start=(kk == 0), stop=(kk == 7), r=[wq, hT], w=[pp])
                    if s_ == 3:
                        k.act(zs[hh][:], pp[:, :], AF.Silu, r=[pp], w=[zs[hh]])
                        continue
                    pr = pre[s_][hh]
                    k.copy("act", pr[:, 3:515], pp[:, :], r=[pp], w=[pr])
                    k.ts("dve", acc[:], pr[:, 0:512], cw[:, s_, hh, 0:1], None, ALU.mult, r=[pr, cw], w=[acc])
                    for j in range(1, 4):
                        k.stt("dve", acc[:], pr[:, j:j + 512], cw[:, s_, hh, j:j + 1], acc[:], ALU.mult, ALU.add,
                              r=[pr, cw, acc], w=[acc])
                    k.copy("pool", pr[:, 0:3], pr[:, 512:515], r=[pr], w=[pr])
                    dst = qkv[s_][hh]
                    if s_ == 2:
                        k.act(dst[:], acc[:], AF.Silu, r=[acc], w=[dst])
                        continue
                    k.act(acc[:], acc[:], AF.Silu, r=[acc], w=[acc])
                    k.tt("dve", sq[:], acc[:], acc[:], ALU.mult, r=[acc], w=[sq])
                    k.mm(pp[:, :], c["ones_f"][:], sq[:], start=True, stop=True, r=[c["ones_f"], sq], w=[pp])
                    k.ts("dve", rn[:], pp[:, :], RMS_EPS, None, ALU.add, r=[pp], w=[rn])
                    k.act(rn[:], rn[:], AF.Sqrt, r=[rn], w=[rn])
                    k.op("dve", lambda: nc.vector.reciprocal(out=rn[:], in_=rn[:]), r=[rn], w=[rn])
                    if s_ == 0:
                        k.stt("dve", dst[:], acc[:], 128 ** -0.5, rn[:], ALU.mult, ALU.mult, r=[acc, rn], w=[dst])
                    else:
                        k.tt("dve", dst[:], acc[:], rn[:], ALU.mult, r=[acc, rn], w=[dst])
            for tk in range(512):
                for hh in range(2):
                    S_, q_, k_, v_ = Sst[hh], qkv[0][hh], qkv[1][hh], qkv[2][hh]
                    tc_ = slice(tk, tk + 1)
                    k.mm(pc1[hh][:, 0:1], S_[:], k_[:, tc_], start=True, stop=True, r=[S_, k_], w=[pc1[hh]])
                    k.stt("dve", vp[hh][:], pc1[hh][:, 0:1], aB[hh][:, tc_], v_[:, tc_], ALU.mult, ALU.subtract,
                          r=[pc1[hh], aB[hh], v_], w=[vp[hh]])
                    k.ts("dve", vp[hh][:], vp[hh][:], nbB[hh][:, tc_], None, ALU.mult, r=[vp[hh], nbB[hh]], w=[vp[hh]])
                    k.ts("dve", VL[hh][:], c["ones_f"][:], vp[hh][:, 0:1], None, ALU.mult, r=[c["ones_f"], vp[hh]], w=[VL[hh]])
                    k.mm(pVB[hh][:, 0:128], VL[hh][:], c["ident_f"][:], start=True, stop=True, r=[VL[hh], c["ident_f"]],
                         w=[pVB[hh]])
                    k.ts("pool", S_[:], S_[:], aB[hh][:, tc_], None, ALU.mult, r=[S_, aB[hh]], w=[S_])
                    k.stt("dve", S_[:], pVB[hh][:, 0:128], k_[:, tc_], S_[:], ALU.mult, ALU.add, r=[pVB[hh], k_, S_], w=[S_])
                    k.mm(pso[hh][:, tc_], S_[:], q_[:, tc_], start=True, stop=True, r=[S_, q_], w=[pso[hh]])
            for hh in range(2):
                k.copy("act", oT[:], pso[hh][:, :], r=[pso[hh]], w=[oT])
                k.tt("dve", sq[:], oT[:], oT[:], ALU.mult, r=[oT], w=[sq])
                pp = P["m"]
                k.mm(pp[:, :], c["ones_f"][:], sq[:], start=True, stop=True, r=[c["ones_f"], sq], w=[pp])
                k.ts("dve", rn[:], pp[:, :], 1.0 / 128, RMS_EPS, ALU.mult, ALU.add, r=[pp], w=[rn])
                k.act(rn[:], rn[:], AF.Sqrt, r=[rn], w=[rn])
                k.op("dve", lambda: nc.vector.reciprocal(out=rn[:], in_=rn[:]), r=[rn], w=[rn])
                k.stt("dve", oT[:], oT[:], normg[:, 0:1], rn[:], ALU.mult, ALU.mult, r=[oT, normg, rn], w=[oT])
                y_ = yb[hh]
                k.tt("dve", y_[:], oT[:], zs[hh][:], ALU.mult, r=[oT, zs[hh]], w=[y_])
                k.dma("sp", y_out[hh * 128:(hh + 1) * 128, t * 512:(t + 1) * 512], y_[:], r=[y_], w=[outb])
        barrier(k)
        k.st = k_st
        st.close()
    return outb


JQ = 3
_PROGS = {}


def _prog(name):
    if name not in _PROGS:
        _PROGS[name] = build_A(0) if name == "A" else build_B(JQ)
    return _PROGS[name]


def _w_dn(inp, l, hp):
    w = np.asarray(inp["w_in"][l])
    cols = [w[:, c0 + hp * 256:c0 + hp * 256 + 256] for c0 in (C_DNQ, C_DNK, C_DNV, C_Z)]
    cols += [w[:, C_A + 2 * hp:C_A + 2 * hp + 2], w[:, C_B + 2 * hp:C_B + 2 * hp + 2]]
    return np.ascontiguousarray(np.concatenate(cols, axis=1), dtype=np.float32)


def _a_inputs(inp, l, b, hp, x_b):
    w_dn = _w_dn(inp, l, hp)
    cw = np.asarray(inp["conv_w"][l]).reshape(4, 3, 8, 128)[:, :, 2 * hp:2 * hp + 2, :]
    return {"x": np.ascontiguousarray(x_b, dtype=np.float32),
            "cT": np.ascontiguousarray(np.asarray(inp["c"][b]).reshape(8, 128).T),
            "w_ada": np.ascontiguousarray(inp["w_ada"][l]),
            "b_adaT": np.ascontiguousarray(np.asarray(inp["b_ada"][l]).reshape(48, 128).T),
            "w_dn": w_dn, "convw": np.ascontiguousarray(cw.transpose(3, 1, 2, 0)),
            "alog": np.ascontiguousarray(np.asarray(inp["a_log"][l])[2 * hp:2 * hp + 2].reshape(2, 1)),
            "dtb": np.ascontiguousarray(np.asarray(inp["dt_bias"][l])[2 * hp:2 * hp + 2].reshape(2, 1)),
            "normg": np.ascontiguousarray(np.asarray(inp["dn_norm_g"][l]).reshape(128, 1))}


def _b_inputs(inp, l, b, jq, x_b, y_dnT_b):
    s = (JQ - jq) * 128
    x_c = np.zeros((S, D), np.float32)
    x_c[s:] = x_b[:S - s]
    y_c = np.zeros((D, S), y_dnT_b.dtype)
    y_c[:, s:] = y_dnT_b[:, :S - s]
    padb = np.zeros((128, 384), np.float32)
    padb[:, :s] = NEG
    g = lambda n: np.ascontiguousarray(inp[n][l])
    return {"x": x_c, "cT": np.ascontiguousarray(np.asarray(inp["c"][b]).reshape(8, 128).T),
            "w_ada": g("w_ada"), "b_adaT": np.ascontiguousarray(np.asarray(inp["b_ada"][l]).reshape(48, 128).T),
            "b_ada": g("b_ada"), "w_in": g("w_in"),
            "w_ukT": np.ascontiguousarray(np.asarray(inp["w_uk"][l]).transpose(0, 2, 1)), "w_uv": g("w_uv"),
            "kv_norm_g": g("kv_norm_g"), "y_dnT": y_c, "w_br_att": g("w_br_att"), "w_br_dn": g("w_br_dn"),
            "w_out": g("w_out"), "ln_g": g("ln_g"), "ln_b": g("ln_b"), "w_route_grp": g("w_route_grp"),
            "w_route_exp": g("w_route_exp"), "w_gate": g("w_gate"), "w_up": g("w_up"), "w_down": g("w_down"),
            "padb": padb}


def kernel(**inp):
    inp = {k_: np.asarray(v) for k_, v in inp.items()}
    x_cur = np.asarray(inp["x"], dtype=np.float32)
    cores = list(range(8))
    for l in range(DEPTH):
        maps = [_a_inputs(inp, l, cid // 4, cid % 4, x_cur[cid // 4]) for cid in cores]
        resA = run_bass_kernel_spmd(_prog("A"), maps, core_ids=cores).results
        y_dnT = [np.concatenate([resA[b * 4 + hp]["y_dnT"] for hp in range(4)], axis=0) for b in range(NB)]
        maps = [_b_inputs(inp, l, cid // 4, cid % 4, x_cur[cid // 4], y_dnT[cid // 4]) for cid in cores]
        resB = run_bass_kernel_spmd(_prog("B"), maps, core_ids=cores).results
        x_next = np.empty_like(x_cur)
        for cid in cores:
            b, jq = cid // 4, cid % 4
            o = resB[cid]["out"]
            for i in range(16):
                gblk = 4 * i + jq
                x_next[b, gblk * 128:(gblk + 1) * 128] = o[i * 128:(i + 1) * 128]
        x_cur = x_next
    return x_cur.astype(np.float32)


F_SHARED = [("cT", [128, 8], F32), ("w_ada", [2, D, 6 * D], F32), ("b_adaT", [2, 128, 48], F32), ("b_ada", [2, 6 * D], F32),
            ("w_in", [2, D, N_IN], F32), ("w_ukT", [2, 8, 128, 256], F32), ("w_uv", [2, 8, 256, 128], F32),
            ("kv_norm_g", [2, 256], F32), ("w_br_att", [2, D, D], F32), ("w_br_dn", [2, D, D], F32), ("w_out", [2, D, D], F32),
            ("ln_g", [2, 2, D], F32), ("ln_b", [2, 2, D], F32), ("w_route_grp", [2, D, 4], F32),
            ("w_route_exp", [2, D, 32], F32), ("w_gate", [2, 32, D, 512], F32), ("w_up", [2, 32, D, 512], F32),
            ("w_down", [2, 32, 512, D], F32), ("w_dn", [2, D, 1028], F32), ("convw", [2, 128, 3, 2, 4], F32),
            ("alog", [2, 2, 1], F32), ("dtb", [2, 2, 1], F32), ("normg", [2, 128, 1], F32)]
F_OTHER = [("xpad", [67 * 128, D], F32), ("padb", [128, 384], F32)]
GROUPS = [[0, 1, 2, 3], [4, 5, 6, 7]]


def build_fused():
    nc = bass.Bass("TRN2", target_bir_lowering=False)
    io = {n: dram(nc, n, shp, dt, "ExternalInput") for n, shp, dt in F_SHARED + F_OTHER}
    out_d = dram(nc, "out", [2048, D], F32, "ExternalOutput")
    xt1 = dram(nc, "xt1", [67 * 128, D], F32, "Internal")
    ysrc = [dram(nc, "ysrc%d" % l, [256, S], BF16, "Internal") for l in range(2)]
    ygc = [[dram(nc, "ygc%d_%d" % (l, q), [256, S], BF16, "Internal") for q in range(4)] for l in range(2)]
    osrc = dram(nc, "osrc", [2048, D], F32, "Internal")
    ogc = [dram(nc, "ogc%d" % q, [4 * 256, D], F32, "Internal") for q in range(8)]
    xloc = dram(nc, "xloc", [64 * 128, D], F32, "Internal")
    yloc = dram(nc, "yloc", [1024, S - 384], BF16, "Internal")
    scr = alloc_scratch_B(nc)
    from concourse.bass import ds
    with ExitStack() as st:
        jv = nc.sync.snap(nc.sync.partition_id() % 4, min_val=0, max_val=3)
        jva = nc.scalar.snap(nc.scalar.partition_id() % 4, min_val=0, max_val=3)
        k = K(nc, st)
        c = make_consts(k)
        k.c = c
        P = alloc_psum(k)
        xt1b = Buf()
        zt = k.sb([128, D], F32)
        k.memset("pool", zt[:], 0.0, w=[zt])
        for p_ in range(3):
            k.dma("sp", xt1[p_ * 128:(p_ + 1) * 128, :], zt[:], r=[zt], w=[xt1b])
        xsrc = [io["xpad"], xt1]
        xbufs_ = [Buf(), xt1b]
        def _probe(tag):
            import os
            if not os.environ.get("PROBE"):
                return
            try:
                k.dma("sp", zt[:], io["xpad"][ds(jv * 128, 128), :], w=[zt])
                print("probe", tag, "ok", flush=True)
            except Exception as e:
                print("probe", tag, "FAIL", repr(e)[:80], flush=True)
        final = None
        for l in range(DEPTH):
            iol = {n: io[n][l] for n, _, _ in F_SHARED if n != "cT"}
            iol["cT"] = io["cT"]
            iol["padb"] = io["padb"]
            X = xsrc[l]
            iol["xb"] = xbufs_[l]
            iol["xb_true"] = xbufs_[l]
            iol["xrow_true"] = (lambda X_: (lambda p: X_[(3 + p) * 128:(4 + p) * 128, :]))(X)
            xlocb = Buf()
            for q_ in range(4):
                k.dma("act", xloc[q_ * 2048:(q_ + 1) * 2048, :], X[ds(jva * 128 + q_ * 2048, 2048), :],
                      r=[xbufs_[l]], w=[xlocb])
            iol["xrow"] = lambda p: xloc[p * 128:(p + 1) * 128, :]
            iol["xb"] = xlocb
            iol["ycol"] = lambda i, h: yloc[h * 128:(h + 1) * 128, i * 512:i * 512 + 128]
            iol["yb"] = Buf()
            ygb = Buf()
            stl = ExitStack()
            k_st, k.st = k.st, stl
            res = {}
            _probe("layer start")
            res["modT"] = adaln_cols(k, c, P["m"], iol["cT"], iol["w_ada"], iol["b_adaT"], [0, 1, 2, 3, 4, 5])
            _probe("after adaln")
            res["G1"] = k.sb([128, D], F32)
            res["G2"] = k.sb([128, D], F32)
            stc = ExitStack()
            k.st = stc
            condB = make_condB(k, iol)
            gate_rows(k, c, P, iol, 2, condB, res["G1"])
            gate_rows(k, c, P, iol, 5, condB, res["G2"])
            barrier(k)
            k.st = stl
            stc.close()
            _probe("after gates")
            youtb = DN_IMPL[0](k, c, P, iol, res["modT"], 0, ysrc[l])
            _probe("after dn")
            ylv = yloc.rearrange("(r q w) t -> q r w t", r=4, q=4)
            for q_ in range(4):
                k.op("pool", lambda: nc.gpsimd.collective_compute(
                    "AllGather", ALU.bypass, replica_groups=GROUPS,
                    ins=[ysrc[l][q_ * 64:(q_ + 1) * 64, :].opt()], outs=[ygc[l][q_].opt()]), r=[youtb], w=[ygb])
            sta = ExitStack()
            k.st = sta
            res["ckv_tok"] = k.sb([128, 64, 256], BF16)
            res["ckvT"] = k.sb([128, 2, S], BF16)
            res["kidxT"] = k.sb([128, S], BF16)
            res["widx"] = k.sb([128, 16, 8], F32)
            stp = phase_kq(k, c, P, iol, JQ, res, scr)
            barrier(k)
            stp.close()
            DSA_IMPL[0](k, c, P, iol, JQ, res, scr)
            k.st = stl
            sta.close()
            for q_ in range(4):
                k.dma("sp", ylv[q_], ygc[l][q_][:, ds(jv * 128, S - 384)].rearrange("(r w) t -> r w t", r=4),
                      r=[ygb], w=[iol["yb"]])
            phase_merge(k, c, P, iol, JQ, res, scr, l)
            dest = osrc if l == 0 else out_d
            if l == 0:
                xv = xt1[384:, :].rearrange("(i j p) d -> i j p d", j=4, p=128)

                def on_block(i_, outb_):
                    if i_ % 2 == 0:
                        return
                    q_ = i_ // 2
                    ogb = Buf()
                    k.op("pool", lambda: nc.gpsimd.collective_compute(
                        "AllGather", ALU.bypass, replica_groups=GROUPS,
                        ins=[osrc[q_ * 256:(q_ + 1) * 256, :].opt()], outs=[ogc[q_].opt()]),
                         r=[outb_[i_ - 1], outb_[i_]], w=[ogb])
                    sv = ogc[q_].rearrange("(j i p) d -> i j p d", j=4, i=2, p=128)
                    for i2 in range(2):
                        k.dma("act", xv[2 * q_ + i2], sv[i2], r=[ogb], w=[xt1b])

                phase_moe(k, c, P, iol, JQ, res, scr, dest, on_block=on_block)
            else:
                final = phase_moe(k, c, P, iol, JQ, res, scr, dest)
            barrier(k)
            k.st = k_st
            stl.close()
        k.finish(final)
        barrier(k)
    return nc


def _fused_inputs(inp, cid):
    b, j = cid // 4, cid % 4
    m = {}
    for n, _, _ in F_SHARED:
        if n in ("cT", "b_adaT", "w_ukT", "w_dn", "convw", "alog", "dtb", "normg"):
            continue
        m[n] = np.ascontiguousarray(inp[n], dtype=np.float32)
    m["cT"] = np.ascontiguousarray(inp["c"][b].reshape(8, 128).T)
    m["b_adaT"] = np.ascontiguousarray(inp["b_ada"].reshape(2, 48, 128).transpose(0, 2, 1))
    m["w_ukT"] = np.ascontiguousarray(inp["w_uk"].transpose(0, 1, 3, 2))
    m["w_dn"] = np.stack([_w_dn(inp, l, j) for l in range(2)])
    cw = inp["conv_w"].reshape(2, 4, 3, 8, 128)[:, :, :, 2 * j:2 * j + 2, :]
    m["convw"] = np.ascontiguousarray(cw.transpose(0, 4, 2, 3, 1))
    m["alog"] = np.ascontiguousarray(inp["a_log"][:, 2 * j:2 * j + 2].reshape(2, 2, 1))
    m["dtb"] = np.ascontiguousarray(inp["dt_bias"][:, 2 * j:2 * j + 2].reshape(2, 2, 1))
    m["normg"] = np.ascontiguousarray(inp["dn_norm_g"].reshape(2, 128, 1))
    xpad = np.zeros((67 * 128, D), np.float32)
    xpad[384:] = inp["x"][b]
    m["xpad"] = xpad
    padb = np.zeros((128, 384), np.float32)
    padb[:, :(JQ - j) * 128] = NEG
    m["padb"] = padb
    return m


def kernel_unfused(**inp):
    return _kernel_unfused(**inp)


_kernel_unfused = kernel


def kernel(**inp):
    inp = {k_: np.asarray(v) for k_, v in inp.items()}
    if "F" not in _PROGS:
        _PROGS["F"] = build_fused()
    cores = list(range(8))
    maps = [_fused_inputs(inp, cid) for cid in cores]
    res = run_bass_kernel_spmd(_PROGS["F"], maps, core_ids=cores).results
    out = np.empty((NB, S, D), np.float32)
    for cid in cores:
        b, j = cid // 4, cid % 4
        o = res[cid]["out"]
        for i in range(16):
            g = 4 * i + j
            out[b, g * 128:(g + 1) * 128] = o[i * 128:(i + 1) * 128]
    return out


def phase_dn2(k, c, P, io, modT, hp, y_out, ntiles=16):
    nc = k.nc
    st = ExitStack()
    k_st, k.st = k.st, st
    scp1 = k.sb([128, 8], F32)
    k.ts("dve", scp1[:], modT[:, 1, :], 1.0, None, ALU.add, r=[modT], w=[scp1])
    sh1 = modT[:, 0, :]
    w_dn = io["w_dn"]
    wq = k.sb([128, 8, 4, 2, 128], BF16)
    for si in range(4):
        k.dma("pool", wq[:, :, si, :, :].rearrange("p kk hh n -> p kk (hh n)"),
              w_dn[:, si * 256:si * 256 + 256].rearrange("(kk p) n -> p kk n", p=128), w=[wq])
    wab = k.sb([128, 8, 4], BF16)
    k.dma("pool", wab[:], w_dn[:, 1024:1028].rearrange("(kk p) n -> p kk n", p=128), w=[wab])
    cw = k.sb([128, 3, 2, 4], F32)
    k.dma("sp", cw[:], io["convw"], w=[cw])
    I64 = c["ident_f"][0:64, 0:64]
    ones64 = c["ones_f"][0:64, 0:64]
    ones64_128 = c["ones_f"][0:64, :]
    dtbB = k.sb([64, 2], F32)
    nAB = k.sb([64, 2], F32)
    k.dma("sp", dtbB[:], bass.AP(io["dtb"].tensor, io["dtb"].offset, [[0, 64], [1, 2]]), w=[dtbB])
    k.dma("sp", nAB[:], bass.AP(io["alog"].tensor, io["alog"].offset, [[0, 64], [1, 2]]), w=[nAB])
    k.act(nAB[:], nAB[:], AF.Exp, r=[nAB], w=[nAB])
    k.ts("dve", nAB[:], nAB[:], -1.0, None, ALU.mult, r=[nAB], w=[nAB])
    ngB = k.sb([64, 128], F32)
    k.dma("sp", ngB[:], bass.AP(io["normg"].tensor, io["normg"].offset, [[0, 64], [1, 128]]), w=[ngB])
    UT = k.sb([64, 64], F32)
    k.memset("pool", UT[:], 1.0, w=[UT])
    k.op("pool", lambda: nc.gpsimd.affine_select(out=UT[:], in_=UT[:], pattern=[[1, 64]], compare_op=ALU.is_ge,
                                                  fill=0.0, base=0, channel_multiplier=-1), r=[UT], w=[UT])
    TRIU = k.sb([64, 8, 64], F32)
    TRILS = k.sb([64, 8, 64], F32)
    k.memset("pool", TRIU[:], 1.0, w=[TRIU])
    k.op("pool", lambda: nc.gpsimd.affine_select(out=TRIU[:], in_=TRIU[:], pattern=[[0, 8], [1, 64]], compare_op=ALU.is_ge,
                                                  fill=0.0, base=0, channel_multiplier=-1), r=[TRIU], w=[TRIU])
    k.memset("pool", TRILS[:], 1.0, w=[TRILS])
    k.op("pool", lambda: nc.gpsimd.affine_select(out=TRILS[:], in_=TRILS[:], pattern=[[0, 8], [-1, 64]], compare_op=ALU.is_gt,
                                                  fill=0.0, base=0, channel_multiplier=1), r=[TRILS], w=[TRILS])
    xbufs = [k.sb([128, 4, D], F32) for _ in range(2)]
    hT = k.sb([128, 8, 512], BF16)
    pre = [[k.sb([128, 515], F32) for _ in range(2)] for _ in range(3)]
    for s_ in range(3):
        for hh in range(2):
            k.memset("pool", pre[s_][hh][:, 0:3], 0.0, w=[pre[s_][hh]])
    qkv = [[k.sb([128, 512], F32) for _ in range(2)] for _ in range(3)]
    zs = [k.sb([128, 512], F32) for _ in range(2)]
    acc = k.sb([128, 512], F32)
    sq = k.sb([128, 512], F32)
    rn = k.sb([128, 512], F32)
    Sst = [k.sb([128, 128], F32) for _ in range(2)]
    for hh in range(2):
        k.memset("pool", Sst[hh][:], 0.0, w=[Sst[hh]])
    gtok = k.sb([64, 2, 8], F32)
    betok = k.sb([64, 2, 8], F32)
    Gs = k.sb([64, 16], F32)
    eG = k.sb([64, 16], F32)
    eGlG = k.sb([64, 16], F32)
    eGl128 = k.sb([128, 16], F32)
    rhsG = k.sb([64, 8, 64], F32)
    eGB = k.sb([128, 512], F32)
    qd = k.sb([128, 512], F32)
    dd = k.sb([64, 8, 64], F32)
    e1 = k.sb([64, 8, 64], F32)
    e2 = k.sb([64, 8, 64], F32)
    Am = [k.sb([64, 8, 64], F32), k.sb([64, 4, 64], F32)]
    An = [k.sb([64, 8, 64], F32), k.sb([64, 4, 64], F32)]
    Mq = [k.sb([64, 4, 64], F32) for _ in range(2)]
    Nq = [k.sb([64, 4, 64], F32) for _ in range(2)]
    qkT = k.sb([64, 8, 64], F32)
    ktok = k.sb([64, 8, 128], F32)
    vtok = k.sb([64, 8, 128], F32)
    ztok = k.sb([64, 8, 128], F32)
    ktail = k.sb([64, 8, 128], F32)
    xs = k.sb([64, 4, 256], F32)
    WT = k.sb([128, 4, 64], F32)
    vnew = [k.sb([64, 128], F32) for _ in range(2)]
    otok = [k.sb([64, 128], F32) for _ in range(2)]
    ytok = [k.sb([64, 128], F32) for _ in range(2)]
    junk = k.sb([64, 128], F32)
    ss = k.sb([64, 1], F32)
    yb = [k.sb([128, 512], BF16) for _ in range(2)]
    outb = Buf()
    B_GB, B_KK, B_QK, B_TR, B_XA, B_XB, B_MN, B_SC = P["c"][0], P["c"][1], P["q"][0], P["tr"][0], P["m"], P["t"], P["q"][1], P["tr"][1]
    for t in range(ntiles):
        xt = xbufs[t % 2]
        for tb in range(4):
            k.dma("sp", xt[:, tb, :], io["xrow_true"](t * 4 + tb), r=[io.get("xb_true", io["xb"])], w=[xt])
        transpose_modulate(k, c, xt, hT, [P["m"], P["t"]], scp1, sh1, 4)
        pab = B_XA
        for cc in range(8):
            for kk in range(8):
                k.mm(pab[0:64, cc * 4:cc * 4 + 4], hT[:, kk, cc * 64:(cc + 1) * 64], wab[:, kk, :], start=(kk == 0),
                     stop=(kk == 7), r=[hT, wab], w=[pab])
        pabv = pab[0:64, 0:32].rearrange("p (cc f) -> p cc f", f=4)
        for hh in range(2):
            k.act(gtok[:, hh, :], pabv[:, :, hh], AF.Exp, bias=dtbB[:, hh:hh + 1], r=[pab, dtbB], w=[gtok])
            k.act(betok[:, hh, :], pabv[:, :, 2 + hh], AF.Sigmoid, r=[pab], w=[betok])
        k.ts("dve", gtok[:], gtok[:], 1.0, None, ALU.add, r=[gtok], w=[gtok])
        k.act(gtok[:], gtok[:], AF.Ln, r=[gtok], w=[gtok])
        for hh in range(2):
            k.ts("dve", gtok[:, hh, :], gtok[:, hh, :], nAB[:, hh:hh + 1], None, ALU.mult, r=[gtok, nAB], w=[gtok])
        gflat = gtok[:].rearrange("p h cc -> p (h cc)")
        pG = B_XB
        k.mm(pG[0:64, 0:16], UT[:], gflat, start=True, stop=True, r=[UT, gtok], w=[pG])
        k.mm(pG[0:64, 16:32], ones64, gflat, start=True, stop=True, r=[c["ones_f"], gtok], w=[pG])
        k.mm(pG[:, 32:48], ones64_128, gflat, start=True, stop=True, r=[c["ones_f"], gtok], w=[pG])
        k.copy("dve", Gs[:], pG[0:64, 0:16], r=[pG], w=[Gs])
        k.tt("dve", eGlG[:], pG[0:64, 16:32], Gs[:], ALU.subtract, r=[pG, Gs], w=[eGlG])
        k.copy("dve", eGl128[:], pG[:, 32:48], r=[pG], w=[eGl128])
        k.act(eG[:], Gs[:], AF.Exp, r=[Gs], w=[eG])
        k.act(eGlG[:], eGlG[:], AF.Exp, r=[eGlG], w=[eGlG])
        k.act(eGl128[:], eGl128[:], AF.Exp, r=[eGl128], w=[eGl128])
        for hh in range(2):
            for s_ in range(4):
                pp = P["m"] if s_ % 2 == 0 else P["t"]
                for kk in range(8):
                    k.mm(pp[:, :], wq[:, kk, s_, hh, :], hT[:, kk, :], start=(kk == 0), stop=(kk == 7), r=[wq, hT], w=[pp])
                if s_ == 3:
                    k.act(zs[hh][:], pp[:, :], AF.Silu, r=[pp], w=[zs[hh]])
                    continue
                pr = pre[s_][hh]
                k.copy("act", pr[:, 3:515], pp[:, :], r=[pp], w=[pr])
                k.ts("dve", acc[:], pr[:, 0:512], cw[:, s_, hh, 0:1], None, ALU.mult, r=[pr, cw], w=[acc])
                for j in range(1, 4):
                    k.stt("dve", acc[:], pr[:, j:j + 512], cw[:, s_, hh, j:j + 1], acc[:], ALU.mult, ALU.add,
                          r=[pr, cw, acc], w=[acc])
                k.copy("pool", pr[:, 0:3], pr[:, 512:515], r=[pr], w=[pr])
                dst = qkv[s_][hh]
                if s_ == 2:
                    k.act(dst[:], acc[:], AF.Silu, r=[acc], w=[dst])
                    continue
                k.act(acc[:], acc[:], AF.Silu, r=[acc], w=[acc])
                k.tt("dve", sq[:], acc[:], acc[:], ALU.mult, r=[acc], w=[sq])
                k.mm(pp[:, :], c["ones_f"][:], sq[:], start=True, stop=True, r=[c["ones_f"], sq], w=[pp])
                k.ts("dve", rn[:], pp[:, :], RMS_EPS, None, ALU.add, r=[pp], w=[rn])
                k.act(rn[:], rn[:], AF.Sqrt, r=[rn], w=[rn])
                k.op("dve", lambda: nc.vector.reciprocal(out=rn[:], in_=rn[:]), r=[rn], w=[rn])
                if s_ == 0:
                    k.stt("dve", dst[:], acc[:], 128 ** -0.5, rn[:], ALU.mult, ALU.mult, r=[acc, rn], w=[dst])
                else:
                    k.tt("dve", dst[:], acc[:], rn[:], ALU.mult, r=[acc, rn], w=[dst])
        for hh in range(2):
            q_, k_, v_, S_ = qkv[0][hh], qkv[1][hh], qkv[2][hh], Sst[hh]
            gi = lambda cc: hh * 8 + cc
            for cc in range(8):
                k.ts("pool", rhsG[:, cc, :], UT[:], gtok[:, hh, cc:cc + 1], None, ALU.mult, r=[UT, gtok], w=[rhsG])
            k.mm(B_GB[:, :], ones64_128, rhsG[:].rearrange("p cc i -> p (cc i)"), start=True, stop=True,
                 r=[c["ones_f"], rhsG], w=[B_GB])
            k.act(eGB[:], B_GB[:, :], AF.Exp, r=[B_GB], w=[eGB])
            k.tt("pool", qd[:], q_[:], eGB[:], ALU.mult, r=[q_, eGB], w=[qd])
            for cc in range(8):
                k.ts("dve", dd[:, cc, :], B_GB[0:64, cc * 64:(cc + 1) * 64], Gs[:, gi(cc):gi(cc) + 1], None, ALU.subtract,
                     r=[B_GB, Gs], w=[dd])
            k.ts("dve", e1[:], dd[:], 0.0, None, ALU.min, r=[dd], w=[e1])
            k.ts("dve", e2[:], dd[:], 0.0, None, ALU.max, r=[dd], w=[e2])
            k.act(e1[:], e1[:], AF.Exp, r=[e1], w=[e1])
            k.act(e2[:], e2[:], AF.Exp, scale=-1.0, r=[e2], w=[e2])
            k.tt("pool", e1[:], e1[:], TRIU[:], ALU.mult, r=[e1, TRIU], w=[e1])
            k.tt("pool", e2[:], e2[:], TRILS[:], ALU.mult, r=[e2, TRILS], w=[e2])
            for cc in range(8):
                cs = slice(cc * 64, (cc + 1) * 64)
                k.mm(B_KK[0:64, cs], k_[:, cs], k_[:, cs], start=True, stop=True, r=[k_], w=[B_KK])
                k.mm(B_QK[0:64, cs], k_[:, cs], q_[:, cs], start=True, stop=True, r=[k_, q_], w=[B_QK])
            A0, AT0 = Am[0], An[0]
            for cc in range(8):
                k.stt("dve", A0[:, cc, :], B_KK[0:64, cc * 64:(cc + 1) * 64], betok[:, hh, cc:cc + 1], e2[:, cc, :],
                      ALU.mult, ALU.mult, r=[B_KK, betok, e2], w=[A0])
            k.tt("dve", qkT[:].rearrange("p cc i -> p (cc i)"), B_QK[0:64, :], e1[:].rearrange("p cc i -> p (cc i)"), ALU.mult,
                 r=[B_QK, e1], w=[qkT])
            for cc in range(8):
                k.tr(B_TR[0:64, cc * 64:(cc + 1) * 64], A0[:, cc, :], I64, r=[A0, c["ident_f"]], w=[B_TR])
            k.copy("act", AT0[:].rearrange("p cc i -> p (cc i)"), B_TR[0:64, :], r=[B_TR], w=[AT0])
            for src, dst in ((k_, ktok), (v_, vtok), (zs[hh], ztok)):
                for half in range(2):
                    for c4 in range(4):
                        cc = half * 4 + c4
                        k.tr(B_TR[0:64, c4 * 128:(c4 + 1) * 128], src[:, cc * 64:(cc + 1) * 64], c["ident_f"][:],
                             r=[src, c["ident_f"]], w=[B_TR])
                    k.copy("act", dst[:, half * 4:half * 4 + 4, :].rearrange("p cc d -> p (cc d)"), B_TR[0:64, :],
                           r=[B_TR], w=[dst])
            for cc in range(8):
                k.ts("pool", ktail[:, cc, :], ktok[:, cc, :], eGlG[:, gi(cc):gi(cc) + 1], None, ALU.mult, r=[ktok, eGlG], w=[ktail])
            pyT = B_TR
            for grp in range(2):
                c0 = grp * 4
                for c4 in range(4):
                    cc = c0 + c4
                    k.ts("dve", xs[:, c4, 0:128], vtok[:, cc, :], betok[:, hh, cc:cc + 1], None, ALU.mult, r=[vtok, betok], w=[xs])
                    k.ts("dve", xs[:, c4, 128:256], ktok[:, cc, :], betok[:, hh, cc:cc + 1], eG[:, gi(cc):gi(cc) + 1], ALU.mult,
                         ALU.mult, r=[ktok, betok, eG], w=[xs])
                Mcur = T(A0.t[:, c0:c0 + 4, :], A0.b)
                Ncur = T(AT0.t[:, c0:c0 + 4, :], AT0.b)
                for lev in range(6):
                    if lev > 0:
                        Mn, Nn = Mq[lev % 2], Nq[lev % 2]
                        for c4 in range(4):
                            k.mm(B_MN[0:64, c4 * 64:(c4 + 1) * 64], Ncur[:, c4, :], Mcur[:, c4, :], start=True, stop=True,
                                 r=[Ncur, Mcur], w=[B_MN])
                            k.mm(B_MN[0:64, 256 + c4 * 64:256 + (c4 + 1) * 64], Mcur[:, c4, :], Ncur[:, c4, :], start=True,
                                 stop=True, r=[Ncur, Mcur], w=[B_MN])
                        k.copy("act", Mn[:].rearrange("p cc i -> p (cc i)"), B_MN[0:64, 0:256], r=[B_MN], w=[Mn])
                        k.copy("act", Nn[:].rearrange("p cc i -> p (cc i)"), B_MN[0:64, 256:512], r=[B_MN], w=[Nn])
                        Mcur, Ncur = Mn, Nn
                    for c4 in range(4):
                        pb_ = B_XA if c4 < 2 else B_XB
                        k.mm(pb_[0:64, (c4 % 2) * 256:(c4 % 2) * 256 + 256], Ncur[:, c4, :], xs[:, c4, :], start=True, stop=True,
                             r=[Ncur, xs], w=[pb_])
                    op_ = ALU.subtract if lev == 0 else ALU.add
                    k.tt("dve", xs[:, 0:2, :].rearrange("p cc n -> p (cc n)"), xs[:, 0:2, :].rearrange("p cc n -> p (cc n)"),
                         B_XA[0:64, :], op_, r=[xs, B_XA], w=[xs])
                    k.tt("dve", xs[:, 2:4, :].rearrange("p cc n -> p (cc n)"), xs[:, 2:4, :].rearrange("p cc n -> p (cc n)"),
                         B_XB[0:64, :], op_, r=[xs, B_XB], w=[xs])
                for c4 in range(4):
                    k.tr(B_MN[:, c4 * 64:(c4 + 1) * 64], xs[:, c4, 128:256], I64, r=[xs, c["ident_f"]], w=[B_MN])
                k.copy("act", WT[:].rearrange("p cc i -> p (cc i)"), B_MN[:, 0:256], r=[B_MN], w=[WT])
                for c4 in range(4):
                    cc = c0 + c4
                    cs = slice(cc * 64, (cc + 1) * 64)
                    vn, ot, yt = vnew[cc % 2], otok[cc % 2], ytok[cc % 2]
                    k.mm(B_SC[0:64, 0:128], WT[:, c4, :], S_[:], start=True, stop=True, r=[WT, S_], w=[B_SC])
                    k.tt("dve", vn[:], xs[:, c4, 0:128], B_SC[0:64, 0:128], ALU.subtract, r=[xs, B_SC], w=[vn])
                    k.mm(B_SC[0:64, 128:256], qd[:, cs], S_[:], start=True, stop=False, r=[qd, S_], w=[B_SC])
                    k.mm(B_SC[0:64, 128:256], qkT[:, cc, :], vn[:], start=False, stop=True, r=[qkT, vn], w=[B_SC])
                    k.mm(B_SC[:, 256:384], ktail[:, cc, :], vn[:], start=True, stop=True, r=[ktail, vn], w=[B_SC])
                    k.stt("dve", S_[:], S_[:], eGl128[:, gi(cc):gi(cc) + 1], B_SC[:, 256:384], ALU.mult, ALU.add,
                          r=[S_, eGl128, B_SC], w=[S_])
                    k.copy("dve", ot[:], B_SC[0:64, 128:256], r=[B_SC], w=[ot])
                    k.op("act", lambda: nc.scalar.memzero(ss[:]), (), [ss])
                    k.act(junk[:], ot[:], AF.Square, accum_out=ss[:, 0:1], r=[ot, ss], w=[junk, ss])
                    k.ts("dve", ss[:], ss[:], 1.0 / 128, RMS_EPS, ALU.mult, ALU.add, r=[ss], w=[ss])
                    k.act(ss[:], ss[:], AF.Sqrt, r=[ss], w=[ss])
                    k.op("dve", lambda: nc.vector.reciprocal(out=ss[:], in_=ss[:]), r=[ss], w=[ss])
                    k.stt("dve", yt[:], ot[:], ss[:, 0:1], ngB[:], ALU.mult, ALU.mult, r=[ot, ss, ngB], w=[yt])
                    k.tt("pool", yt[:], yt[:], ztok[:, cc, :], ALU.mult, r=[yt, ztok], w=[yt])
                    k.tr(pyT[:, 256 + c4 * 64:256 + (c4 + 1) * 64], yt[:], I64, r=[yt, c["ident_f"]], w=[pyT])
                y_ = yb[hh]
                k.copy("act", y_[:, c0 * 64:(c0 + 4) * 64], pyT[:, 256:512], r=[pyT], w=[y_])
            k.dma("sp", y_out[hh * 128:(hh + 1) * 128, t * 512:(t + 1) * 512], yb[hh][:], r=[yb[hh]], w=[outb])
    barrier(k)
    k.st = k_st
    st.close()
    return outb


def phase_dn3(k, c, P, io, modT, hp, y_out, ntiles=16):
    nc = k.nc
    st = ExitStack()
    k_st, k.st = k.st, st
    scp1 = k.sb([128, 8], F32)
    k.ts("dve", scp1[:], modT[:, 1, :], 1.0, None, ALU.add, r=[modT], w=[scp1])
    sh1 = modT[:, 0, :]
    w_dn = io["w_dn"]
    wq = k.sb([128, 8, 4, 2, 128], BF16)
    for si in range(4):
        k.dma("pool", wq[:, :, si, :, :].rearrange("p kk hh n -> p kk (hh n)"),
              w_dn[:, si * 256:si * 256 + 256].rearrange("(kk p) n -> p kk n", p=128), w=[wq])
    wab = k.sb([128, 8, 4], BF16)
    k.dma("pool", wab[:], w_dn[:, 1024:1028].rearrange("(kk p) n -> p kk n", p=128), w=[wab])
    cw = k.sb([128, 3, 2, 4], F32)
    k.dma("sp", cw[:], io["convw"], w=[cw])
    I64 = c["ident_f"][0:64, 0:64]
    ones64 = c["ones_f"][0:64, 0:64]
    ones64_128 = c["ones_f"][0:64, :]
    dtbB = k.sb([64, 2], F32)
    nAB = k.sb([64, 2], F32)
    k.dma("sp", dtbB[:], bass.AP(io["dtb"].tensor, io["dtb"].offset, [[0, 64], [1, 2]]), w=[dtbB])
    k.dma("sp", nAB[:], bass.AP(io["alog"].tensor, io["alog"].offset, [[0, 64], [1, 2]]), w=[nAB])
    k.act(nAB[:], nAB[:], AF.Exp, r=[nAB], w=[nAB])
    k.ts("dve", nAB[:], nAB[:], -1.0, None, ALU.mult, r=[nAB], w=[nAB])
    ngB = k.sb([64, 128], F32)
    k.dma("sp", ngB[:], bass.AP(io["normg"].tensor, io["normg"].offset, [[0, 64], [1, 128]]), w=[ngB])
    UT = k.sb([64, 64], F32)
    k.memset("pool", UT[:], 1.0, w=[UT])
    k.op("pool", lambda: nc.gpsimd.affine_select(out=UT[:], in_=UT[:], pattern=[[1, 64]], compare_op=ALU.is_ge,
                                                  fill=0.0, base=0, channel_multiplier=-1), r=[UT], w=[UT])
    TRIU = k.sb([64, 8, 64], F32)
    TRILS = k.sb([64, 8, 64], F32)
    k.memset("pool", TRIU[:], 1.0, w=[TRIU])
    k.op("pool", lambda: nc.gpsimd.affine_select(out=TRIU[:], in_=TRIU[:], pattern=[[0, 8], [1, 64]], compare_op=ALU.is_ge,
                                                  fill=0.0, base=0, channel_multiplier=-1), r=[TRIU], w=[TRIU])
    k.memset("pool", TRILS[:], 1.0, w=[TRILS])
    k.op("pool", lambda: nc.gpsimd.affine_select(out=TRILS[:], in_=TRILS[:], pattern=[[0, 8], [-1, 64]], compare_op=ALU.is_gt,
                                                  fill=0.0, base=0, channel_multiplier=1), r=[TRILS], w=[TRILS])
    xbufs = [k.sb([128, 4, D], F32) for _ in range(2)]
    hT = k.sb([128, 8, 512], BF16)
    pre = [[k.sb([128, 515], F32) for _ in range(2)] for _ in range(3)]
    for s_ in range(3):
        for hh in range(2):
            k.memset("pool", pre[s_][hh][:, 0:3], 0.0, w=[pre[s_][hh]])
    qkv = [[k.sb([128, 512], F32) for _ in range(2)] for _ in range(3)]
    zs = [k.sb([128, 512], F32) for _ in range(2)]
    acc = k.sb([128, 512], F32)
    sq = k.sb([128, 512], F32)
    rn = k.sb([128, 512], F32)
    Sst = [k.sb([128, 128], F32) for _ in range(2)]
    for hh in range(2):
        k.memset("pool", Sst[hh][:], 0.0, w=[Sst[hh]])
    gtok = k.sb([64, 2, 8], F32)
    betok = k.sb([64, 2, 8], F32)
    Gs = k.sb([64, 16], F32)
    eG = k.sb([64, 16], F32)
    eGlG = k.sb([64, 16], F32)
    eGl128 = k.sb([128, 16], F32)
    rhsG = k.sb([64, 8, 64], F32)
    eGB = k.sb([128, 512], F32)
    dd = k.sb([64, 8, 64], F32)
    e1 = k.sb([64, 8, 64], F32)
    e2 = k.sb([64, 8, 64], F32)
    A0s = [k.sb([64, 8, 64], F32) for _ in range(2)]
    A0bs = [k.sb([64, 8, 64], BF16) for _ in range(2)]
    AT0s = [k.sb([64, 8, 64], BF16) for _ in range(2)]
    Mqs = [[k.sb([64, 2, 64], BF16) for _ in range(2)] for _ in range(2)]
    Nqs = [[k.sb([64, 2, 64], BF16) for _ in range(2)] for _ in range(2)]
    xsbs = [[k.sb([64, 2, 256], BF16) for _ in range(2)] for _ in range(2)]
    qkTs = [k.sb([64, 8, 64], F32) for _ in range(2)]
    ktoks = [k.sb([64, 8, 128], F32) for _ in range(2)]
    vtoks = [k.sb([64, 8, 128], F32) for _ in range(2)]
    ztoks = [k.sb([64, 8, 128], F32) for _ in range(2)]
    ktails = [k.sb([64, 8, 128], F32) for _ in range(2)]
    qds = [k.sb([128, 512], F32) for _ in range(2)]
    xss = [[k.sb([64, 2, 256], F32) for _ in range(2)] for _ in range(2)]
    WTs = [[k.sb([128, 2, 64], F32) for _ in range(2)] for _ in range(2)]
    vnews = [[k.sb([64, 128], F32) for _ in range(2)] for _ in range(2)]
    otoks = [[k.sb([64, 128], F32) for _ in range(2)] for _ in range(2)]
    ytoks = [[k.sb([64, 128], F32) for _ in range(2)] for _ in range(2)]
    junks = [k.sb([64, 128], F32) for _ in range(2)]
    sss = [k.sb([64, 1], F32) for _ in range(2)]
    yb = [k.sb([128, 512], BF16) for _ in range(2)]
    outb = Buf()
    B_GB, B_KK, B_QK, B_TR, B_XA, B_XB = P["c"][0], P["c"][1], P["q"][0], P["q"][1], P["m"], P["t"]
    BX, BMN, BSC, BY = [P["c"][0], P["c"][1]], [P["q"][0], P["q"][1]], [P["tr"][0], P["tr"][1]], [P["m"], P["t"]]
    for t in range(ntiles):
        xt = xbufs[t % 2]
        for tb in range(4):
            k.dma("sp", xt[:, tb, :], io["xrow_true"](t * 4 + tb), r=[io.get("xb_true", io["xb"])], w=[xt])
        transpose_modulate(k, c, xt, hT, [P["m"], P["t"]], scp1, sh1, 4)
        pab = B_XA
        for cc in range(8):
            for kk in range(8):
                k.mm(pab[0:64, cc * 4:cc * 4 + 4], hT[:, kk, cc * 64:(cc + 1) * 64], wab[:, kk, :], start=(kk == 0),
                     stop=(kk == 7), r=[hT, wab], w=[pab])
        pabv = pab[0:64, 0:32].rearrange("p (cc f) -> p cc f", f=4)
        for hh in range(2):
            k.act(gtok[:, hh, :], pabv[:, :, hh], AF.Exp, bias=dtbB[:, hh:hh + 1], r=[pab, dtbB], w=[gtok])
            k.act(betok[:, hh, :], pabv[:, :, 2 + hh], AF.Sigmoid, r=[pab], w=[betok])
        k.ts("dve", gtok[:], gtok[:], 1.0, None, ALU.add, r=[gtok], w=[gtok])
        k.act(gtok[:], gtok[:], AF.Ln, r=[gtok], w=[gtok])
        for hh in range(2):
            k.ts("dve", gtok[:, hh, :], gtok[:, hh, :], nAB[:, hh:hh + 1], None, ALU.mult, r=[gtok, nAB], w=[gtok])
        gflat = gtok[:].rearrange("p h cc -> p (h cc)")
        pG = B_XB
        k.mm(pG[0:64, 0:16], UT[:], gflat, start=True, stop=True, r=[UT, gtok], w=[pG])
        k.mm(pG[0:64, 16:32], ones64, gflat, start=True, stop=True, r=[c["ones_f"], gtok], w=[pG])
        k.mm(pG[:, 32:48], ones64_128, gflat, start=True, stop=True, r=[c["ones_f"], gtok], w=[pG])
        k.copy("dve", Gs[:], pG[0:64, 0:16], r=[pG], w=[Gs])
        k.tt("dve", eGlG[:], pG[0:64, 16:32], Gs[:], ALU.subtract, r=[pG, Gs], w=[eGlG])
        k.copy("dve", eGl128[:], pG[:, 32:48], r=[pG], w=[eGl128])
        k.act(eG[:], Gs[:], AF.Exp, r=[Gs], w=[eG])
        k.act(eGlG[:], eGlG[:], AF.Exp, r=[eGlG], w=[eGlG])
        k.act(eGl128[:], eGl128[:], AF.Exp, r=[eGl128], w=[eGl128])
        for hh in range(2):
            for s_ in range(4):
                pp = P["m"] if s_ % 2 == 0 else P["t"]
                for kk in range(8):
                    k.mm(pp[:, :], wq[:, kk, s_, hh, :], hT[:, kk, :], start=(kk == 0), stop=(kk == 7), r=[wq, hT], w=[pp])
                if s_ == 3:
                    k.act(zs[hh][:], pp[:, :], AF.Silu, r=[pp], w=[zs[hh]])
                    continue
                pr = pre[s_][hh]
                k.copy("act", pr[:, 3:515], pp[:, :], r=[pp], w=[pr])
                k.ts("dve", acc[:], pr[:, 0:512], cw[:, s_, hh, 0:1], None, ALU.mult, r=[pr, cw], w=[acc])
                for j in range(1, 4):
                    k.stt("dve", acc[:], pr[:, j:j + 512], cw[:, s_, hh, j:j + 1], acc[:], ALU.mult, ALU.add,
                          r=[pr, cw, acc], w=[acc])
                k.copy("pool", pr[:, 0:3], pr[:, 512:515], r=[pr], w=[pr])
                dst = qkv[s_][hh]
                if s_ == 2:
                    k.act(dst[:], acc[:], AF.Silu, r=[acc], w=[dst])
                    continue
                k.act(acc[:], acc[:], AF.Silu, r=[acc], w=[acc])
                k.tt("dve", sq[:], acc[:], acc[:], ALU.mult, r=[acc], w=[sq])
                k.mm(pp[:, :], c["ones_f"][:], sq[:], start=True, stop=True, r=[c["ones_f"], sq], w=[pp])
                k.ts("dve", rn[:], pp[:, :], RMS_EPS, None, ALU.add, r=[pp], w=[rn])
                k.act(rn[:], rn[:], AF.Sqrt, r=[rn], w=[rn])
                k.op("dve", lambda: nc.vector.reciprocal(out=rn[:], in_=rn[:]), r=[rn], w=[rn])
                if s_ == 0:
                    k.stt("dve", dst[:], acc[:], 128 ** -0.5, rn[:], ALU.mult, ALU.mult, r=[acc, rn], w=[dst])
                else:
                    k.tt("dve", dst[:], acc[:], rn[:], ALU.mult, r=[acc, rn], w=[dst])
        for hh in range(2):
            q_, k_, v_, S_ = qkv[0][hh], qkv[1][hh], qkv[2][hh], Sst[hh]
            qd, qkT, ktok, vtok, ztok, ktail = qds[hh], qkTs[hh], ktoks[hh], vtoks[hh], ztoks[hh], ktails[hh]
            gi = lambda cc: hh * 8 + cc
            for cc in range(8):
                k.ts("pool", rhsG[:, cc, :], UT[:], gtok[:, hh, cc:cc + 1], None, ALU.mult, r=[UT, gtok], w=[rhsG])
            k.mm(B_GB[:, :], ones64_128, rhsG[:].rearrange("p cc i -> p (cc i)"), start=True, stop=True,
                 r=[c["ones_f"], rhsG], w=[B_GB])
            k.act(eGB[:], B_GB[:, :], AF.Exp, r=[B_GB], w=[eGB])
            k.tt("pool", qd[:], q_[:], eGB[:], ALU.mult, r=[q_, eGB], w=[qd])
            for cc in range(8):
                k.ts("dve", dd[:, cc, :], B_GB[0:64, cc * 64:(cc + 1) * 64], Gs[:, gi(cc):gi(cc) + 1], None, ALU.subtract,
                     r=[B_GB, Gs], w=[dd])
            k.ts("dve", e1[:], dd[:], 0.0, None, ALU.min, r=[dd], w=[e1])
            k.ts("dve", e2[:], dd[:], 0.0, None, ALU.max, r=[dd], w=[e2])
            k.act(e1[:], e1[:], AF.Exp, r=[e1], w=[e1])
            k.act(e2[:], e2[:], AF.Exp, scale=-1.0, r=[e2], w=[e2])
            k.tt("pool", e1[:], e1[:], TRIU[:], ALU.mult, r=[e1, TRIU], w=[e1])
            k.tt("pool", e2[:], e2[:], TRILS[:], ALU.mult, r=[e2, TRILS], w=[e2])
            for cc in range(8):
                cs = slice(cc * 64, (cc + 1) * 64)
                k.mm(B_KK[0:64, cs], k_[:, cs], k_[:, cs], start=True, stop=True, r=[k_], w=[B_KK])
                k.mm(B_QK[0:64, cs], k_[:, cs], q_[:, cs], start=True, stop=True, r=[k_, q_], w=[B_QK])
            A0, AT0 = A0s[hh], AT0s[hh]
            for cc in range(8):
                k.stt("dve", A0[:, cc, :], B_KK[0:64, cc * 64:(cc + 1) * 64], betok[:, hh, cc:cc + 1], e2[:, cc, :],
                      ALU.mult, ALU.mult, r=[B_KK, betok, e2], w=[A0])
            k.tt("dve", qkT[:].rearrange("p cc i -> p (cc i)"), B_QK[0:64, :], e1[:].rearrange("p cc i -> p (cc i)"), ALU.mult,
                 r=[B_QK, e1], w=[qkT])
            for cc in range(8):
                k.tr(B_TR[0:64, cc * 64:(cc + 1) * 64], A0[:, cc, :], I64, r=[A0, c["ident_f"]], w=[B_TR])
            k.copy("act", AT0[:].rearrange("p cc i -> p (cc i)"), B_TR[0:64, :], r=[B_TR], w=[AT0])
            k.copy("pool", A0bs[hh][:], A0[:], r=[A0], w=[A0bs[hh]])
            for src, dst in ((k_, ktok), (v_, vtok), (zs[hh], ztok)):
                for half in range(2):
                    for c4 in range(4):
                        cc = half * 4 + c4
                        k.tr(B_TR[0:64, c4 * 128:(c4 + 1) * 128], src[:, cc * 64:(cc + 1) * 64], c["ident_f"][:],
                             r=[src, c["ident_f"]], w=[B_TR])
                    k.copy("act", dst[:, half * 4:half * 4 + 4, :].rearrange("p cc d -> p (cc d)"), B_TR[0:64, :],
                           r=[B_TR], w=[dst])
            for cc in range(8):
                k.ts("pool", ktail[:, cc, :], ktok[:, cc, :], eGlG[:, gi(cc):gi(cc) + 1], None, ALU.mult, r=[ktok, eGlG], w=[ktail])

        def solve(hh, g2):
            A0, AT0, ktok, vtok = A0bs[hh], AT0s[hh], ktoks[hh], vtoks[hh]
            xs, WT = xss[hh][g2 % 2], WTs[hh][g2 % 2]
            xb = xsbs[hh][g2 % 2]
            bx, bmn = BX[hh], BMN[hh]
            c0 = g2 * 2
            gi = lambda cc: hh * 8 + cc
            for c2 in range(2):
                cc = c0 + c2
                k.ts("dve", xs[:, c2, 0:128], vtok[:, cc, :], betok[:, hh, cc:cc + 1], None, ALU.mult, r=[vtok, betok], w=[xs])
                k.ts("dve", xs[:, c2, 128:256], ktok[:, cc, :], betok[:, hh, cc:cc + 1], eG[:, gi(cc):gi(cc) + 1], ALU.mult,
                     ALU.mult, r=[ktok, betok, eG], w=[xs])
            Mcur = T(A0.t[:, c0:c0 + 2, :], A0.b)
            Ncur = T(AT0.t[:, c0:c0 + 2, :], AT0.b)
            xf = xs[:].rearrange("p cc n -> p (cc n)")
            k.copy("pool", xb[:], xs[:], r=[xs], w=[xb])
            for lev in range(6):
                if lev > 0:
                    Mn, Nn = Mqs[hh][lev % 2], Nqs[hh][lev % 2]
                    for c2 in range(2):
                        k.mm(bmn[0:64, c2 * 64:(c2 + 1) * 64], Ncur[:, c2, :], Mcur[:, c2, :], start=True, stop=True,
                             r=[Ncur, Mcur], w=[bmn])
                        k.mm(bmn[0:64, 128 + c2 * 64:128 + (c2 + 1) * 64], Mcur[:, c2, :], Ncur[:, c2, :], start=True,
                             stop=True, r=[Ncur, Mcur], w=[bmn])
                    k.copy("act", Mn[:].rearrange("p cc i -> p (cc i)"), bmn[0:64, 0:128], r=[bmn], w=[Mn])
                    k.copy("act", Nn[:].rearrange("p cc i -> p (cc i)"), bmn[0:64, 128:256], r=[bmn], w=[Nn])
                    Mcur, Ncur = Mn, Nn
                for c2 in range(2):
                    k.mm(bx[0:64, c2 * 256:(c2 + 1) * 256], Ncur[:, c2, :], xb[:, c2, :], start=True, stop=True,
                         r=[Ncur, xb], w=[bx])
                k.tt("dve", xf, xf, bx[0:64, :], ALU.subtract if lev == 0 else ALU.add, r=[xs, bx], w=[xs])
                if lev < 5:
                    k.copy("pool", xb[:], xs[:], r=[xs], w=[xb])
                yield
            for c2 in range(2):
                k.tr(bmn[:, 256 + c2 * 64:256 + (c2 + 1) * 64], xs[:, c2, 128:256], I64, r=[xs, c["ident_f"]], w=[bmn])
            k.copy("act", WT[:].rearrange("p cc i -> p (cc i)"), bmn[:, 256:384], r=[bmn], w=[WT])
            yield

        def scan(hh, g2):
            S_, qd, qkT, ktail, ztok = Sst[hh], qds[hh], qkTs[hh], ktails[hh], ztoks[hh]
            xs, WT = xss[hh][g2 % 2], WTs[hh][g2 % 2]
            bsc, by = BSC[hh], BY[hh]
            ss, junk = sss[hh], junks[hh]
            gi = lambda cc: hh * 8 + cc
            for c2 in range(2):
                cc = g2 * 2 + c2
                cs = slice(cc * 64, (cc + 1) * 64)
                vn, ot, yt = vnews[hh][cc % 2], otoks[hh][cc % 2], ytoks[hh][cc % 2]
                k.mm(bsc[0:64, 0:128], WT[:, c2, :], S_[:], start=True, stop=True, r=[WT, S_], w=[bsc])
                k.tt("dve", vn[:], xs[:, c2, 0:128], bsc[0:64, 0:128], ALU.subtract, r=[xs, bsc], w=[vn])
                k.mm(bsc[0:64, 128:256], qd[:, cs], S_[:], start=True, stop=False, r=[qd, S_], w=[bsc])
                k.mm(bsc[0:64, 128:256], qkT[:, cc, :], vn[:], start=False, stop=True, r=[qkT, vn], w=[bsc])
                k.mm(bsc[:, 256:384], ktail[:, cc, :], vn[:], start=True, stop=True, r=[ktail, vn], w=[bsc])
                k.stt("dve", S_[:], S_[:], eGl128[:, gi(cc):gi(cc) + 1], bsc[:, 256:384], ALU.mult, ALU.add,
                      r=[S_, eGl128, bsc], w=[S_])
                k.copy("dve", ot[:], bsc[0:64, 128:256], r=[bsc], w=[ot])
                k.act(junk[:], ot[:], AF.Square, accum_out=ss[:, 0:1], r=[ot], w=[junk, ss])
                k.ts("dve", ss[:], ss[:], 1.0 / 128, RMS_EPS, ALU.mult, ALU.add, r=[ss], w=[ss])
                k.act(ss[:], ss[:], AF.Sqrt, r=[ss], w=[ss])
                k.op("dve", lambda: nc.vector.reciprocal(out=ss[:], in_=ss[:]), r=[ss], w=[ss])
                k.stt("dve", yt[:], ot[:], ss[:, 0:1], ngB[:], ALU.mult, ALU.mult, r=[ot, ss, ngB], w=[yt])
                k.tt("pool", yt[:], yt[:], ztok[:, cc, :], ALU.mult, r=[yt, ztok], w=[yt])
                k.tr(by[:, cc * 64:(cc + 1) * 64], yt[:], I64, r=[yt, c["ident_f"]], w=[by])
                yield
            if g2 == 3:
                k.copy("act", yb[hh][:], by[:, :], r=[by], w=[yb[hh]])
                k.dma("sp", y_out[hh * 128:(hh + 1) * 128, t * 512:(t + 1) * 512], yb[hh][:], r=[yb[hh]], w=[outb])

        def head_flow(hh):
            for _ in solve(hh, 0):
                yield
            for g2 in range(4):
                ga = scan(hh, g2)
                gb = solve(hh, g2 + 1) if g2 + 1 < 4 else iter(())
                alive = [True, True]
                gens = [gb, ga]
                while any(alive):
                    for gi_ in range(2):
                        if alive[gi_]:
                            try:
                                next(gens[gi_])
                            except StopIteration:
                                alive[gi_] = False
                    yield

        flows = [head_flow(0), head_flow(1)]
        alive = [True, True]
        while any(alive):
            for hh in range(2):
                if alive[hh]:
                    try:
                        next(flows[hh])
                    except StopIteration:
                        alive[hh] = False
    barrier(k)
    k.st = k_st
    st.close()
    return outb


def phase_dn4(k, c, P, io, modT, hp, y_out, ntiles=16):
    nc = k.nc
    st = ExitStack()
    k_st, k.st = k.st, st
    scp1 = k.sb([128, 8], F32)
    k.ts("dve", scp1[:], modT[:, 1, :], 1.0, None, ALU.add, r=[modT], w=[scp1])
    sh1 = modT[:, 0, :]
    w_dn = io["w_dn"]
    wq = k.sb([128, 8, 4, 2, 128], BF16)
    for si in range(4):
        k.dma("pool", wq[:, :, si, :, :].rearrange("p kk hh n -> p kk (hh n)"),
              w_dn[:, si * 256:si * 256 + 256].rearrange("(kk p) n -> p kk n", p=128), w=[wq])
    wab = k.sb([128, 8, 4], BF16)
    k.dma("pool", wab[:], w_dn[:, 1024:1028].rearrange("(kk p) n -> p kk n", p=128), w=[wab])
    cw = k.sb([128, 3, 2, 4], F32)
    k.dma("sp", cw[:], io["convw"], w=[cw])
    I64 = c["ident_f"][0:64, 0:64]
    ones64 = c["ones_f"][0:64, 0:64]
    ones64_128 = c["ones_f"][0:64, :]
    dtbB = k.sb([128, 2], F32)
    nAB = k.sb([128, 2], F32)
    k.dma("sp", dtbB[:], bass.AP(io["dtb"].tensor, io["dtb"].offset, [[0, 128], [1, 2]]), w=[dtbB])
    k.dma("sp", nAB[:], bass.AP(io["alog"].tensor, io["alog"].offset, [[0, 128], [1, 2]]), w=[nAB])
    k.act(nAB[:], nAB[:], AF.Exp, r=[nAB], w=[nAB])
    k.ts("dve", nAB[:], nAB[:], -1.0, None, ALU.mult, r=[nAB], w=[nAB])
    ngB = k.sb([128, 128], F32)
    k.dma("sp", ngB[:], bass.AP(io["normg"].tensor, io["normg"].offset, [[0, 128], [1, 128]]), w=[ngB])
    I128 = c["ident_f"][:]
    UTbd = k.sb([128, 128], F32)
    k.memset("pool", UTbd[:], 1.0, w=[UTbd])
    k.op("pool", lambda: nc.gpsimd.affine_select(out=UTbd[:], in_=UTbd[:], pattern=[[1, 128]], compare_op=ALU.is_ge,
                                                  fill=0.0, base=0, channel_multiplier=-1), r=[UTbd], w=[UTbd])
    k.memset("pool", UTbd[0:64, 64:128], 0.0, w=[UTbd])
    ONESbd = k.sb([128, 128], F32)
    k.memset("pool", ONESbd[:], 0.0, w=[ONESbd])
    k.memset("pool", ONESbd[0:64, 0:64], 1.0, w=[ONESbd])
    k.memset("pool", ONESbd[64:128, 64:128], 1.0, w=[ONESbd])
    L0 = k.sb([128, 128], F32)
    L1 = k.sb([128, 128], F32)
    k.memset("pool", L0[:], 0.0, w=[L0])
    k.memset("pool", L0[0:64, :], 1.0, w=[L0])
    k.memset("pool", L1[:], 0.0, w=[L1])
    k.memset("pool", L1[64:128, :], 1.0, w=[L1])
    TRIU = k.sb([128, 4, 128], F32)
    for p_ in range(4):
        k.copy("pool", TRIU[:, p_, :], UTbd[:], r=[UTbd], w=[TRIU])
    TRILS = k.sb([128, 4, 128], F32)
    k.memset("pool", TRILS[:], 1.0, w=[TRILS])
    k.op("pool", lambda: nc.gpsimd.affine_select(out=TRILS[:], in_=TRILS[:], pattern=[[0, 4], [-1, 128]], compare_op=ALU.is_gt,
                                                  fill=0.0, base=0, channel_multiplier=1), r=[TRILS], w=[TRILS])
    k.memset("pool", TRILS[64:128, :, 0:64], 0.0, w=[TRILS])
    xbufs = [k.sb([128, 4, D], F32) for _ in range(2)]
    hT = k.sb([128, 8, 512], BF16)
    pre = [[k.sb([128, 515], F32) for _ in range(2)] for _ in range(3)]
    for s_ in range(3):
        for hh in range(2):
            k.memset("pool", pre[s_][hh][:, 0:3], 0.0, w=[pre[s_][hh]])
    qkv = [[k.sb([128, 512], F32) for _ in range(2)] for _ in range(3)]
    zs = [k.sb([128, 512], F32) for _ in range(2)]
    accs6 = [k.sb([128, 512], F32) for _ in range(6)]
    rns6 = [k.sb([128, 512], F32) for _ in range(6)]
    Sst = [k.sb([128, 128], F32) for _ in range(2)]
    for hh in range(2):
        k.memset("pool", Sst[hh][:], 0.0, w=[Sst[hh]])
    gtok = k.sb([128, 2, 4], F32)
    betok = k.sb([128, 2, 4], F32)
    Gs = k.sb([128, 8], F32)
    eG = k.sb([128, 8], F32)
    eGlG = k.sb([128, 8], F32)
    eGlA = k.sb([128, 8], F32)
    eGlB = k.sb([128, 8], F32)
    rhsG = k.sb([128, 4, 128], F32)
    eGB = k.sb([128, 512], F32)
    dd = k.sb([128, 4, 128], F32)
    e1 = k.sb([128, 4, 128], F32)
    e2 = k.sb([128, 4, 128], F32)
    A0s = [k.sb([128, 4, 128], F32) for _ in range(2)]
    A0bs = [k.sb([128, 4, 128], BF16) for _ in range(2)]
    AT0s = [k.sb([128, 4, 128], BF16) for _ in range(2)]
    Mqs = [[[k.sb([128, 128], BF16) for _ in range(2)] for _ in range(2)] for _ in range(2)]
    Nqs = [[[k.sb([128, 128], BF16) for _ in range(2)] for _ in range(2)] for _ in range(2)]
    qkTs = [k.sb([128, 4, 128], F32) for _ in range(2)]
    ktoks = [k.sb([128, 4, 128], F32) for _ in range(2)]
    vtoks = [k.sb([128, 4, 128], F32) for _ in range(2)]
    ztoks = [k.sb([128, 4, 128], F32) for _ in range(2)]
    ktails = [k.sb([128, 4, 128], F32) for _ in range(2)]
    qds = [k.sb([128, 512], F32) for _ in range(2)]
    xss = [[k.sb([128, 256], F32) for _ in range(4)] for _ in range(2)]
    xsbs = [[k.sb([128, 256], BF16) for _ in range(4)] for _ in range(2)]
    WTs = [[k.sb([128, 128], F32) for _ in range(4)] for _ in range(2)]
    vns = [k.sb([128, 128], F32) for _ in range(2)]
    for hh in range(2):
        k.memset("pool", vns[hh][:], 0.0, w=[vns[hh]])
    ots = [[k.sb([128, 128], F32) for _ in range(2)] for _ in range(2)]
    yts = [[k.sb([128, 128], F32) for _ in range(2)] for _ in range(2)]
    junks = [k.sb([128, 128], F32) for _ in range(2)]
    sss = [k.sb([128, 1], F32) for _ in range(2)]
    yb = [k.sb([128, 512], BF16) for _ in range(2)]
    outb = Buf()
    SB = [[P["c"][0], P["c"][1]], [P["q"][0], P["q"][1]]]
    BSC, BY = [P["tr"][0], P["tr"][1]], [P["m"], P["t"]]
    for t in range(ntiles):
        xt = xbufs[t % 2]
        for tb in range(4):
            k.dma("sp", xt[:, tb, :], io["xrow_true"](t * 4 + tb), r=[io.get("xb_true", io["xb"])], w=[xt])
        transpose_modulate(k, c, xt, hT, [P["m"], P["t"]], scp1, sh1, 4)
        pab = P["m"]
        for pr_ in range(4):
            for kk in range(8):
                k.mm(pab[:, pr_ * 4:pr_ * 4 + 4], hT[:, kk, pr_ * 128:(pr_ + 1) * 128], wab[:, kk, :], start=(kk == 0),
                     stop=(kk == 7), r=[hT, wab], w=[pab])
        pabv = pab[:, 0:16].rearrange("p (cc f) -> p cc f", f=4)
        for hh in range(2):
            k.act(gtok[:, hh, :], pabv[:, :, hh], AF.Exp, bias=dtbB[:, hh:hh + 1], r=[pab, dtbB], w=[gtok])
            k.act(betok[:, hh, :], pabv[:, :, 2 + hh], AF.Sigmoid, r=[pab], w=[betok])
        k.ts("dve", gtok[:], gtok[:], 1.0, None, ALU.add, r=[gtok], w=[gtok])
        k.act(gtok[:], gtok[:], AF.Ln, r=[gtok], w=[gtok])
        for hh in range(2):
            k.ts("dve", gtok[:, hh, :], gtok[:, hh, :], nAB[:, hh:hh + 1], None, ALU.mult, r=[gtok, nAB], w=[gtok])
        gflat = gtok[:].rearrange("p h cc -> p (h cc)")
        pG = P["t"]
        k.mm(pG[:, 0:8], UTbd[:], gflat, start=True, stop=True, r=[UTbd, gtok], w=[pG])
        k.mm(pG[:, 8:16], ONESbd[:], gflat, start=True, stop=True, r=[ONESbd, gtok], w=[pG])
        k.mm(pG[:, 16:24], L0[:], gflat, start=True, stop=True, r=[L0, gtok], w=[pG])
        k.mm(pG[:, 24:32], L1[:], gflat, start=True, stop=True, r=[L1, gtok], w=[pG])
        k.copy("dve", Gs[:], pG[:, 0:8], r=[pG], w=[Gs])
        k.tt("dve", eGlG[:], pG[:, 8:16], Gs[:], ALU.subtract, r=[pG, Gs], w=[eGlG])
        k.copy("dve", eGlA[:], pG[:, 16:24], r=[pG], w=[eGlA])
        k.copy("dve", eGlB[:], pG[:, 24:32], r=[pG], w=[eGlB])
        k.act(eG[:], Gs[:], AF.Exp, r=[Gs], w=[eG])
        k.act(eGlG[:], eGlG[:], AF.Exp, r=[eGlG], w=[eGlG])
        k.act(eGlA[:], eGlA[:], AF.Exp, r=[eGlA], w=[eGlA])
        k.act(eGlB[:], eGlB[:], AF.Exp, r=[eGlB], w=[eGlB])
        def section(hh, s_, pp):
            for kk in range(8):
                k.mm(pp[:, :], wq[:, kk, s_, hh, :], hT[:, kk, :], start=(kk == 0), stop=(kk == 7), r=[wq, hT], w=[pp])
            yield
            if s_ == 3:
                k.act(zs[hh][:], pp[:, :], AF.Silu, r=[pp], w=[zs[hh]])
                return
            pr = pre[s_][hh]
            acc, rn = accs6[hh * 3 + s_], rns6[hh * 3 + s_]
            dst = qkv[s_][hh]
            k.copy("act", pr[:, 3:515], pp[:, :], r=[pp], w=[pr])
            yield
            k.ts("dve", acc[:], pr[:, 0:512], cw[:, s_, hh, 0:1], None, ALU.mult, r=[pr, cw], w=[acc])
            for j in range(1, 4):
                k.stt("dve", acc[:], pr[:, j:j + 512], cw[:, s_, hh, j:j + 1], acc[:], ALU.mult, ALU.add,
                      r=[pr, cw, acc], w=[acc])
                if j == 2:
                    yield
            k.copy("pool", pr[:, 0:3], pr[:, 512:515], r=[pr], w=[pr])
            yield
            if s_ == 2:
                k.act(dst[:], acc[:], AF.Silu, r=[acc], w=[dst])
                return
            k.act(acc[:], acc[:], AF.Silu, r=[acc], w=[acc])
            yield
            k.tt("dve", dst[:], acc[:], acc[:], ALU.mult, r=[acc], w=[dst])
            k.mm(pp[:, :], c["ones_f"][:], dst[:], start=True, stop=True, r=[c["ones_f"], dst], w=[pp])
            yield
            k.ts("dve", rn[:], pp[:, :], RMS_EPS, None, ALU.add, r=[pp], w=[rn])
            k.act(rn[:], rn[:], AF.Sqrt, r=[rn], w=[rn])
            yield
            k.op("dve", lambda: nc.vector.reciprocal(out=rn[:], in_=rn[:]), r=[rn], w=[rn])
            if s_ == 0:
                k.stt("dve", dst[:], acc[:], 128 ** -0.5, rn[:], ALU.mult, ALU.mult, r=[acc, rn], w=[dst])
            else:
                k.tt("dve", dst[:], acc[:], rn[:], ALU.mult, r=[acc, rn], w=[dst])

        pbanks = [SB[0][0], SB[0][1], SB[1][0], SB[1][1], BSC[0], BSC[1], BY[0], BY[1]]
        sgens = [section(hh, s_, pbanks[hh * 4 + s_]) for s_ in range(4) for hh in range(2)]
        salive = [True] * 8
        while any(salive):
            for gi_ in range(8):
                if salive[gi_]:
                    try:
                        next(sgens[gi_])
                    except StopIteration:
                        salive[gi_] = False
        def front(hh):
            q_, k_, v_ = qkv[0][hh], qkv[1][hh], qkv[2][hh]
            qd, qkT, ktok, vtok, ztok, ktail = qds[hh], qkTs[hh], ktoks[hh], vtoks[hh], ztoks[hh], ktails[hh]
            A0, AT0 = A0s[hh], AT0s[hh]
            B_GB, B_KK, B_QK, B_TR = SB[hh][0], SB[hh][1], BSC[hh], BY[hh]
            gi = lambda p_: hh * 4 + p_
            for p_ in range(4):
                k.ts("dve", rhsG[:, p_, :], UTbd[:], gtok[:, hh, p_:p_ + 1], None, ALU.mult, r=[UTbd, gtok], w=[rhsG])
            k.mm(B_GB[:, :], c["ones_f"][:], rhsG[:].rearrange("p cc i -> p (cc i)"), start=True, stop=True,
                 r=[c["ones_f"], rhsG], w=[B_GB])
            k.act(eGB[:], B_GB[:, :], AF.Exp, r=[B_GB], w=[eGB])
            k.tt("dve", qd[:], q_[:], eGB[:], ALU.mult, r=[q_, eGB], w=[qd])
            yield
            for p_ in range(4):
                k.ts("dve", dd[:, p_, :], B_GB[:, p_ * 128:(p_ + 1) * 128], Gs[:, gi(p_):gi(p_) + 1], None, ALU.subtract,
                     r=[B_GB, Gs], w=[dd])
            k.ts("dve", e1[:], dd[:], 0.0, None, ALU.min, r=[dd], w=[e1])
            k.ts("dve", e2[:], dd[:], 0.0, None, ALU.max, r=[dd], w=[e2])
            k.act(e1[:], e1[:], AF.Exp, r=[e1], w=[e1])
            k.act(e2[:], e2[:], AF.Exp, scale=-1.0, r=[e2], w=[e2])
            k.tt("dve", e1[:], e1[:], TRIU[:], ALU.mult, r=[e1, TRIU], w=[e1])
            k.tt("pool", e2[:], e2[:], TRILS[:], ALU.mult, r=[e2, TRILS], w=[e2])
            yield
            for p_ in range(4):
                cs = slice(p_ * 128, (p_ + 1) * 128)
                k.mm(B_KK[:, cs], k_[:, cs], k_[:, cs], start=True, stop=True, r=[k_], w=[B_KK])
                k.mm(B_QK[:, cs], k_[:, cs], q_[:, cs], start=True, stop=True, r=[k_, q_], w=[B_QK])
            for p_ in range(4):
                k.stt("dve", A0[:, p_, :], B_KK[:, p_ * 128:(p_ + 1) * 128], betok[:, hh, p_:p_ + 1], e2[:, p_, :],
                      ALU.mult, ALU.mult, r=[B_KK, betok, e2], w=[A0])
            k.tt("dve", qkT[:].rearrange("p cc i -> p (cc i)"), B_QK[:, :], e1[:].rearrange("p cc i -> p (cc i)"), ALU.mult,
                 r=[B_QK, e1], w=[qkT])
            for p_ in range(4):
                k.tr(B_TR[:, p_ * 128:(p_ + 1) * 128], A0[:, p_, :], I128, r=[A0, c["ident_f"]], w=[B_TR])
            k.copy("act", AT0[:].rearrange("p cc i -> p (cc i)"), B_TR[:, :], r=[B_TR], w=[AT0])
            k.copy("act", A0bs[hh][:], A0[:], r=[A0], w=[A0bs[hh]])
            yield
            for src_, dst_ in ((k_, ktok), (v_, vtok), (zs[hh], ztok)):
                for p_ in range(4):
                    k.tr(B_TR[:, p_ * 128:(p_ + 1) * 128], src_[:, p_ * 128:(p_ + 1) * 128], I128, r=[src_, c["ident_f"]], w=[B_TR])
                k.copy("act", dst_[:].rearrange("p cc d -> p (cc d)"), B_TR[:, :], r=[B_TR], w=[dst_])
                yield
            for p_ in range(4):
                k.act(ktail[:, p_, :], ktok[:, p_, :], AF.Copy, scale=eGlG[:, gi(p_):gi(p_) + 1], r=[ktok, eGlG], w=[ktail])
            yield

        def solve(hh, p_):
            A0b, AT0, ktok, vtok = A0bs[hh], AT0s[hh], ktoks[hh], vtoks[hh]
            xs, xb, WT = xss[hh][p_], xsbs[hh][p_], WTs[hh][p_]
            bsc_ = BSC[hh]
            g_ = hh * 4 + p_
            k.ts("dve", xs[:, 0:128], vtok[:, p_, :], betok[:, hh, p_:p_ + 1], None, ALU.mult, r=[vtok, betok], w=[xs])
            k.ts("dve", xs[:, 128:256], ktok[:, p_, :], betok[:, hh, p_:p_ + 1], eG[:, g_:g_ + 1], ALU.mult, ALU.mult,
                 r=[ktok, betok, eG], w=[xs])
            k.copy("act", xb[:], xs[:], r=[xs], w=[xb])
            Mcur = T(A0b.t[:, p_, :], A0b.b)
            Ncur = T(AT0.t[:, p_, :], AT0.b)
            bankX, bankY = SB[hh][0], SB[hh][1]
            cx = slice((p_ % 2) * 256, (p_ % 2) * 256 + 256)
            cm = slice((p_ % 2) * 256, (p_ % 2) * 256 + 128)
            cn = slice((p_ % 2) * 256 + 128, (p_ % 2) * 256 + 256)

            def square(lev, Mc, Nc):
                Mn, Nn = Mqs[hh][p_ % 2][lev % 2], Nqs[hh][p_ % 2][lev % 2]
                k.mm(bankY[:, cm], Nc[:], Mc[:], start=True, stop=True, r=[Nc, Mc], w=[bankY])
                k.mm(bankY[:, cn], Mc[:], Nc[:], start=True, stop=True, r=[Nc, Mc], w=[bankY])
                k.copy("act", Mn[:], bankY[:, cm], r=[bankY], w=[Mn])
                k.copy("act", Nn[:], bankY[:, cn], r=[bankY], w=[Nn])
                return Mn, Nn

            nxt = None
            for lev in range(6):
                if lev < 5:
                    nxt = square(lev + 1, Mcur, Ncur)
                k.mm(bankX[:, cx], Ncur[:], xb[:], start=True, stop=True, r=[Ncur, xb], w=[bankX])
                k.tt("dve", xs[:], xs[:], bankX[:, cx], ALU.subtract if lev == 0 else ALU.add, r=[xs, bankX], w=[xs])
                if lev < 5:
                    k.copy("act", xb[:], xs[:], r=[xs], w=[xb])
                    Mcur, Ncur = nxt
                yield
            k.tr(bsc_[:, 384:512], xs[:, 128:256], I128, r=[xs, c["ident_f"]], w=[bsc_])
            k.copy("dve", WT[:], bsc_[:, 384:512], r=[bsc_], w=[WT])
            yield

        def scan(hh, p_):
            S_, qd, qkT, ktail, ztok = Sst[hh], qds[hh], qkTs[hh], ktails[hh], ztoks[hh]
            xs, WT = xss[hh][p_], WTs[hh][p_]
            bsc, by = BSC[hh], BY[hh]
            ss, junk, vn = sss[hh], junks[hh], vns[hh]
            ot, yt = ots[hh][p_ % 2], yts[hh][p_ % 2]
            g_ = hh * 4 + p_
            cs = slice(p_ * 128, (p_ + 1) * 128)
            for c2 in range(2):
                rows = slice(c2 * 64, c2 * 64 + 64)
                egl = (eGlA if c2 == 0 else eGlB)
                k.mm(bsc[:, 0:128], WT[:], S_[:], start=True, stop=True, r=[WT, S_], w=[bsc])
                k.tt("dve", vn[rows, :], xs[rows, 0:128], bsc[rows, 0:128], ALU.subtract, r=[xs, bsc], w=[vn])
                k.mm(bsc[:, 128:256], qd[:, cs], S_[:], start=True, stop=False, r=[qd, S_], w=[bsc])
                k.mm(bsc[:, 128:256], qkT[:, p_, :], vn[:], start=False, stop=True, r=[qkT, vn], w=[bsc])
                k.mm(bsc[:, 256:384], ktail[rows, p_, :], vn[rows, :], start=True, stop=True, r=[ktail, vn], w=[bsc])
                k.stt("dve", S_[:], S_[:], egl[:, g_:g_ + 1], bsc[:, 256:384], ALU.mult, ALU.add, r=[S_, egl, bsc], w=[S_])
                k.copy("dve", ot[rows, :], bsc[rows, 128:256], r=[bsc], w=[ot])
                yield
            k.act(junk[:], ot[:], AF.Square, accum_out=ss[:, 0:1], r=[ot], w=[junk, ss])
            k.ts("dve", ss[:], ss[:], 1.0 / 128, RMS_EPS, ALU.mult, ALU.add, r=[ss], w=[ss])
            k.act(ss[:], ss[:], AF.Sqrt, r=[ss], w=[ss])
            k.op("dve", lambda: nc.vector.reciprocal(out=ss[:], in_=ss[:]), r=[ss], w=[ss])
            k.stt("dve", yt[:], ot[:], ss[:, 0:1], ngB[:], ALU.mult, ALU.mult, r=[ot, ss, ngB], w=[yt])
            k.tt("pool", yt[:], yt[:], ztok[:, p_, :], ALU.mult, r=[yt, ztok], w=[yt])
            k.tr(by[:, cs], yt[:], I128, r=[yt, c["ident_f"]], w=[by])
            if p_ == 3:
                k.copy("act", yb[hh][:], by[:, :], r=[by], w=[yb[hh]])
                k.dma("sp", y_out[hh * 128:(hh + 1) * 128, t * 512:(t + 1) * 512], yb[hh][:], r=[yb[hh]], w=[outb])
            yield

        def rr(gens):
            gens = list(gens)
            alive = [True] * len(gens)
            while any(alive):
                for gi_ in range(len(gens)):
                    if alive[gi_]:
                        try:
                            next(gens[gi_])
                        except StopIteration:
                            alive[gi_] = False
                yield

        def seq(gens):
            for g_ in gens:
                for _ in g_:
                    yield

        def head_flow(hh):
            for _ in rr([solve(hh, 0), solve(hh, 1)]):
                yield
            for _ in rr([solve(hh, 2), solve(hh, 3), seq([scan(hh, 0), scan(hh, 1)])]):
                yield
            for _ in seq([scan(hh, 2), scan(hh, 3)]):
                yield

        for hh in range(2):
            for _ in front(hh):
                pass
        flows = [head_flow(0), head_flow(1)]
        alive = [True, True]
        while any(alive):
            for hh in range(2):
                if alive[hh]:
                    try:
                        next(flows[hh])
                    except StopIteration:
                        alive[hh] = False
    barrier(k)
    k.st = k_st
    st.close()
    return outb


def phase_dn5(k, c, P, io, modT, hp, y_out, ntiles=16):
    nc = k.nc
    st = ExitStack()
    k_st, k.st = k.st, st
    scp1 = k.sb([128, 8], F32)
    k.ts("dve", scp1[:], modT[:, 1, :], 1.0, None, ALU.add, r=[modT], w=[scp1])
    sh1 = modT[:, 0, :]
    w_dn = io["w_dn"]
    wq = k.sb([128, 8, 4, 2, 128], BF16)
    for si in range(4):
        k.dma("pool", wq[:, :, si, :, :].rearrange("p kk hh n -> p kk (hh n)"),
              w_dn[:, si * 256:si * 256 + 256].rearrange("(kk p) n -> p kk n", p=128), w=[wq])
    wab = k.sb([128, 8, 4], BF16)
    k.dma("pool", wab[:], w_dn[:, 1024:1028].rearrange("(kk p) n -> p kk n", p=128), w=[wab])
    cw = k.sb([128, 3, 2, 4], F32)
    k.dma("sp", cw[:], io["convw"], w=[cw])
    I64 = c["ident_f"][0:64, 0:64]
    ones64 = c["ones_f"][0:64, 0:64]
    ones64_128 = c["ones_f"][0:64, :]
    dtbB = k.sb([128, 2], F32)
    nAB = k.sb([128, 2], F32)
    k.dma("sp", dtbB[:], bass.AP(io["dtb"].tensor, io["dtb"].offset, [[0, 128], [1, 2]]), w=[dtbB])
    k.dma("sp", nAB[:], bass.AP(io["alog"].tensor, io["alog"].offset, [[0, 128], [1, 2]]), w=[nAB])
    k.act(nAB[:], nAB[:], AF.Exp, r=[nAB], w=[nAB])
    k.ts("dve", nAB[:], nAB[:], -1.0, None, ALU.mult, r=[nAB], w=[nAB])
    ngB = k.sb([128, 128], F32)
    k.dma("sp", ngB[:], bass.AP(io["normg"].tensor, io["normg"].offset, [[0, 128], [1, 128]]), w=[ngB])
    I128 = c["ident_f"][:]
    UTbd = k.sb([128, 128], F32)
    k.memset("pool", UTbd[:], 1.0, w=[UTbd])
    k.op("pool", lambda: nc.gpsimd.affine_select(out=UTbd[:], in_=UTbd[:], pattern=[[1, 128]], compare_op=ALU.is_ge,
                                                  fill=0.0, base=0, channel_multiplier=-1), r=[UTbd], w=[UTbd])
    k.memset("pool", UTbd[0:64, 64:128], 0.0, w=[UTbd])
    ONESbd = k.sb([128, 128], F32)
    k.memset("pool", ONESbd[:], 0.0, w=[ONESbd])
    k.memset("pool", ONESbd[0:64, 0:64], 1.0, w=[ONESbd])
    k.memset("pool", ONESbd[64:128, 64:128], 1.0, w=[ONESbd])
    L0 = k.sb([128, 128], F32)
    L1 = k.sb([128, 128], F32)
    k.memset("pool", L0[:], 0.0, w=[L0])
    k.memset("pool", L0[0:64, :], 1.0, w=[L0])
    k.memset("pool", L1[:], 0.0, w=[L1])
    k.memset("pool", L1[64:128, :], 1.0, w=[L1])
    TRIU = k.sb([128, 4, 128], F32)
    for p_ in range(4):
        k.copy("pool", TRIU[:, p_, :], UTbd[:], r=[UTbd], w=[TRIU])
    TRILS = k.sb([128, 4, 128], F32)
    k.memset("pool", TRILS[:], 1.0, w=[TRILS])
    k.op("pool", lambda: nc.gpsimd.affine_select(out=TRILS[:], in_=TRILS[:], pattern=[[0, 4], [-1, 128]], compare_op=ALU.is_gt,
                                                  fill=0.0, base=0, channel_multiplier=1), r=[TRILS], w=[TRILS])
    k.memset("pool", TRILS[64:128, :, 0:64], 0.0, w=[TRILS])
    xbufs = [k.sb([128, 4, D], F32) for _ in range(2)]
    hT = k.sb([128, 8, 512], BF16)
    pre = [[k.sb([128, 515], F32) for _ in range(2)] for _ in range(3)]
    for s_ in range(3):
        for hh in range(2):
            k.memset("pool", pre[s_][hh][:, 0:3], 0.0, w=[pre[s_][hh]])
    qkv = [[k.sb([128, 512], F32) for _ in range(2)] for _ in range(3)]
    zs = [k.sb([128, 512], F32) for _ in range(2)]
    accs = [k.sb([128, 512], F32) for _ in range(2)]
    sqs = [k.sb([128, 512], F32) for _ in range(2)]
    rns = [k.sb([128, 512], F32) for _ in range(2)]
    Sst = [k.sb([128, 128], F32) for _ in range(2)]
    for hh in range(2):
        k.memset("pool", Sst[hh][:], 0.0, w=[Sst[hh]])
    gtoks = [k.sb([128, 2, 4], F32) for _ in range(2)]
    betoks = [k.sb([128, 2, 4], F32) for _ in range(2)]
    Gss = [k.sb([128, 8], F32) for _ in range(2)]
    eGs = [k.sb([128, 8], F32) for _ in range(2)]
    eGlGs = [k.sb([128, 8], F32) for _ in range(2)]
    eGlAs = [k.sb([128, 8], F32) for _ in range(2)]
    eGlBs = [k.sb([128, 8], F32) for _ in range(2)]
    rhsG = k.sb([128, 4, 128], F32)
    eGB = k.sb([128, 512], F32)
    dd = k.sb([128, 4, 128], F32)
    e1 = k.sb([128, 4, 128], F32)
    e2 = k.sb([128, 4, 128], F32)
    A0s = [k.sb([128, 4, 128], F32) for _ in range(2)]
    A0bs = [[k.sb([128, 4, 128], BF16) for _ in range(2)] for _ in range(2)]
    AT0s = [[k.sb([128, 4, 128], BF16) for _ in range(2)] for _ in range(2)]
    Mqs = [[[k.sb([128, 128], BF16) for _ in range(2)] for _ in range(2)] for _ in range(2)]
    Nqs = [[[k.sb([128, 128], BF16) for _ in range(2)] for _ in range(2)] for _ in range(2)]
    qkTs = [[k.sb([128, 4, 128], F32) for _ in range(2)] for _ in range(2)]
    ktoks = [[k.sb([128, 4, 128], F32) for _ in range(2)] for _ in range(2)]
    vtoks = [[k.sb([128, 4, 128], F32) for _ in range(2)] for _ in range(2)]
    ztoks = [[k.sb([128, 4, 128], F32) for _ in range(2)] for _ in range(2)]
    ktails = [[k.sb([128, 4, 128], F32) for _ in range(2)] for _ in range(2)]
    qds = [[k.sb([128, 512], F32) for _ in range(2)] for _ in range(2)]
    xss = [[k.sb([128, 256], F32) for _ in range(4)] for _ in range(2)]
    xsbs = [[k.sb([128, 256], BF16) for _ in range(4)] for _ in range(2)]
    WTs = [[k.sb([128, 128], F32) for _ in range(4)] for _ in range(2)]
    vns = [k.sb([128, 128], F32) for _ in range(2)]
    for hh in range(2):
        k.memset("pool", vns[hh][:], 0.0, w=[vns[hh]])
    ots = [[k.sb([128, 128], F32) for _ in range(2)] for _ in range(2)]
    yts = [[k.sb([128, 128], F32) for _ in range(2)] for _ in range(2)]
    junks = [k.sb([128, 128], F32) for _ in range(2)]
    sss = [k.sb([128, 1], F32) for _ in range(2)]
    yb = [[k.sb([128, 512], BF16) for _ in range(2)] for _ in range(2)]
    outb = Buf()
    SBK = [P["c"][0], P["c"][1]]
    BSC = [P["tr"][0], P["tr"][1]]
    FB = [P["q"][0], P["q"][1]]
    def pstage(t):
        par = t % 2
        gtok_, betok_, Gs_, eG_, eGlG_, eGlA_, eGlB_ = gtoks[par], betoks[par], Gss[par], eGs[par], eGlGs[par], eGlAs[par], eGlBs[par]
        xt = xbufs[t % 2]
        for tb in range(4):
            k.dma("sp", xt[:, tb, :], io["xrow_true"](t * 4 + tb), r=[io.get("xb_true", io["xb"])], w=[xt])
        transpose_modulate(k, c, xt, hT, [P["m"], P["t"]], scp1, sh1, 4)
        pab = P["m"]
        for pr_ in range(4):
            for kk in range(8):
                k.mm(pab[:, pr_ * 4:pr_ * 4 + 4], hT[:, kk, pr_ * 128:(pr_ + 1) * 128], wab[:, kk, :], start=(kk == 0),
                     stop=(kk == 7), r=[hT, wab], w=[pab])
        pabv = pab[:, 0:16].rearrange("p (cc f) -> p cc f", f=4)
        for hh in range(2):
            k.act(gtok_[:, hh, :], pabv[:, :, hh], AF.Exp, bias=dtbB[:, hh:hh + 1], r=[pab, dtbB], w=[gtok_])
            k.act(betok_[:, hh, :], pabv[:, :, 2 + hh], AF.Sigmoid, r=[pab], w=[betok_])
        k.ts("dve", gtok_[:], gtok_[:], 1.0, None, ALU.add, r=[gtok_], w=[gtok_])
        k.act(gtok_[:], gtok_[:], AF.Ln, r=[gtok_], w=[gtok_])
        for hh in range(2):
            k.ts("dve", gtok_[:, hh, :], gtok_[:, hh, :], nAB[:, hh:hh + 1], None, ALU.mult, r=[gtok_, nAB], w=[gtok_])
        gflat = gtok_[:].rearrange("p h cc -> p (h cc)")
        pG = P["t"]
        k.mm(pG[:, 0:8], UTbd[:], gflat, start=True, stop=True, r=[UTbd, gtok_], w=[pG])
        k.mm(pG[:, 8:16], ONESbd[:], gflat, start=True, stop=True, r=[ONESbd, gtok_], w=[pG])
        k.mm(pG[:, 16:24], L0[:], gflat, start=True, stop=True, r=[L0, gtok_], w=[pG])
        k.mm(pG[:, 24:32], L1[:], gflat, start=True, stop=True, r=[L1, gtok_], w=[pG])
        k.copy("dve", Gs_[:], pG[:, 0:8], r=[pG], w=[Gs_])
        k.tt("dve", eGlG_[:], pG[:, 8:16], Gs_[:], ALU.subtract, r=[pG, Gs_], w=[eGlG_])
        k.copy("dve", eGlA_[:], pG[:, 16:24], r=[pG], w=[eGlA_])
        k.copy("dve", eGlB_[:], pG[:, 24:32], r=[pG], w=[eGlB_])
        k.act(eG_[:], Gs_[:], AF.Exp, r=[Gs_], w=[eG_])
        k.act(eGlG_[:], eGlG_[:], AF.Exp, r=[eGlG_], w=[eGlG_])
        k.act(eGlA_[:], eGlA_[:], AF.Exp, r=[eGlA_], w=[eGlA_])
        k.act(eGlB_[:], eGlB_[:], AF.Exp, r=[eGlB_], w=[eGlB_])
        yield
        for hh in range(2):
            for s_ in range(4):
                pp = P["m"] if s_ % 2 == 0 else P["t"]
                for kk in range(8):
                    k.mm(pp[:, :], wq[:, kk, s_, hh, :], hT[:, kk, :], start=(kk == 0), stop=(kk == 7), r=[wq, hT], w=[pp])
                yield
                if s_ == 3:
                    k.act(zs[hh][:], pp[:, :], AF.Silu, r=[pp], w=[zs[hh]])
                    continue
                pr = pre[s_][hh]
                acc, sq, rn = accs[s_ % 2], sqs[s_ % 2], rns[s_ % 2]
                k.copy("act", pr[:, 3:515], pp[:, :], r=[pp], w=[pr])
                k.ts("dve", acc[:], pr[:, 0:512], cw[:, s_, hh, 0:1], None, ALU.mult, r=[pr, cw], w=[acc])
                for j in range(1, 4):
                    k.stt("dve", acc[:], pr[:, j:j + 512], cw[:, s_, hh, j:j + 1], acc[:], ALU.mult, ALU.add,
                          r=[pr, cw, acc], w=[acc])
                k.copy("pool", pr[:, 0:3], pr[:, 512:515], r=[pr], w=[pr])
                dst = qkv[s_][hh]
                if s_ == 2:
                    k.act(dst[:], acc[:], AF.Silu, r=[acc], w=[dst])
                    continue
                k.act(acc[:], acc[:], AF.Silu, r=[acc], w=[acc])
                k.tt("dve", sq[:], acc[:], acc[:], ALU.mult, r=[acc], w=[sq])
                k.mm(pp[:, :], c["ones_f"][:], sq[:], start=True, stop=True, r=[c["ones_f"], sq], w=[pp])
                k.ts("dve", rn[:], pp[:, :], RMS_EPS, None, ALU.add, r=[pp], w=[rn])
                k.act(rn[:], rn[:], AF.Sqrt, r=[rn], w=[rn])
                k.op("dve", lambda: nc.vector.reciprocal(out=rn[:], in_=rn[:]), r=[rn], w=[rn])
                if s_ == 0:
                    k.stt("dve", dst[:], acc[:], 128 ** -0.5, rn[:], ALU.mult, ALU.mult, r=[acc, rn], w=[dst])
                else:
                    k.tt("dve", dst[:], acc[:], rn[:], ALU.mult, r=[acc, rn], w=[dst])

    def front(t, hh):
        par = t % 2
        gtok, betok, Gs, eGlG = gtoks[par], betoks[par], Gss[par], eGlGs[par]
        q_, k_, v_ = qkv[0][hh], qkv[1][hh], qkv[2][hh]
        qd, qkT, ktok, vtok, ztok, ktail = qds[hh][par], qkTs[hh][par], ktoks[hh][par], vtoks[hh][par], ztoks[hh][par], ktails[hh][par]
        A0, AT0 = A0s[hh], AT0s[hh][par]
        B_GB, B_KK, B_QK, B_TR = FB[0], FB[1], FB[0], FB[1]
        gi = lambda p_: hh * 4 + p_
        for p_ in range(4):
            k.ts("dve", rhsG[:, p_, :], UTbd[:], gtok[:, hh, p_:p_ + 1], None, ALU.mult, r=[UTbd, gtok], w=[rhsG])
        k.mm(B_GB[:, :], c["ones_f"][:], rhsG[:].rearrange("p cc i -> p (cc i)"), start=True, stop=True,
             r=[c["ones_f"], rhsG], w=[B_GB])
        k.act(eGB[:], B_GB[:, :], AF.Exp, r=[B_GB], w=[eGB])
        k.tt("dve", qd[:], q_[:], eGB[:], ALU.mult, r=[q_, eGB], w=[qd])
        yield
        for p_ in range(4):
            k.ts("dve", dd[:, p_, :], B_GB[:, p_ * 128:(p_ + 1) * 128], Gs[:, gi(p_):gi(p_) + 1], None, ALU.subtract,
                 r=[B_GB, Gs], w=[dd])
        k.ts("dve", e1[:], dd[:], 0.0, None, ALU.min, r=[dd], w=[e1])
        k.ts("dve", e2[:], dd[:], 0.0, None, ALU.max, r=[dd], w=[e2])
        k.act(e1[:], e1[:], AF.Exp, r=[e1], w=[e1])
        k.act(e2[:], e2[:], AF.Exp, scale=-1.0, r=[e2], w=[e2])
        k.tt("dve", e1[:], e1[:], TRIU[:], ALU.mult, r=[e1, TRIU], w=[e1])
        k.tt("pool", e2[:], e2[:], TRILS[:], ALU.mult, r=[e2, TRILS], w=[e2])
        yield
        for p_ in range(4):
            cs = slice(p_ * 128, (p_ + 1) * 128)
            k.mm(B_KK[:, cs], k_[:, cs], k_[:, cs], start=True, stop=True, r=[k_], w=[B_KK])
            k.mm(B_QK[:, cs], k_[:, cs], q_[:, cs], start=True, stop=True, r=[k_, q_], w=[B_QK])
        for p_ in range(4):
            k.stt("dve", A0[:, p_, :], B_KK[:, p_ * 128:(p_ + 1) * 128], betok[:, hh, p_:p_ + 1], e2[:, p_, :],
                  ALU.mult, ALU.mult, r=[B_KK, betok, e2], w=[A0])
        k.tt("dve", qkT[:].rearrange("p cc i -> p (cc i)"), B_QK[:, :], e1[:].rearrange("p cc i -> p (cc i)"), ALU.mult,
             r=[B_QK, e1], w=[qkT])
        for p_ in range(4):
            k.tr(B_TR[:, p_ * 128:(p_ + 1) * 128], A0[:, p_, :], I128, r=[A0, c["ident_f"]], w=[B_TR])
        k.copy("act", AT0[:].rearrange("p cc i -> p (cc i)"), B_TR[:, :], r=[B_TR], w=[AT0])
        k.copy("act", A0bs[hh][par][:], A0[:], r=[A0], w=[A0bs[hh][par]])
        yield
        for src_, dst_ in ((k_, ktok), (v_, vtok), (zs[hh], ztok)):
            for p_ in range(4):
                k.tr(B_TR[:, p_ * 128:(p_ + 1) * 128], src_[:, p_ * 128:(p_ + 1) * 128], I128, r=[src_, c["ident_f"]], w=[B_TR])
            k.copy("act", dst_[:].rearrange("p cc d -> p (cc d)"), B_TR[:, :], r=[B_TR], w=[dst_])
            yield
        for p_ in range(4):
            k.act(ktail[:, p_, :], ktok[:, p_, :], AF.Copy, scale=eGlG[:, gi(p_):gi(p_) + 1], r=[ktok, eGlG], w=[ktail])
        yield


    def solve(t, hh, p_):
        par = t % 2
        betok, eG = betoks[par], eGs[par]
        A0b, AT0, ktok, vtok = A0bs[hh][par], AT0s[hh][par], ktoks[hh][par], vtoks[hh][par]
        xs, xb, WT = xss[hh][p_], xsbs[hh][p_], WTs[hh][p_]
        bank = SBK[hh]
        bsc_ = BSC[hh]
        g_ = hh * 4 + p_
        k.ts("dve", xs[:, 0:128], vtok[:, p_, :], betok[:, hh, p_:p_ + 1], None, ALU.mult, r=[vtok, betok], w=[xs])
        k.ts("dve", xs[:, 128:256], ktok[:, p_, :], betok[:, hh, p_:p_ + 1], eG[:, g_:g_ + 1], ALU.mult, ALU.mult,
             r=[ktok, betok, eG], w=[xs])
        k.copy("act", xb[:], xs[:], r=[xs], w=[xb])
        Mcur = T(A0b.t[:, p_, :], A0b.b)
        Ncur = T(AT0.t[:, p_, :], AT0.b)
        for lev in range(6):
            if lev > 0:
                Mn, Nn = Mqs[hh][p_ % 2][lev % 2], Nqs[hh][p_ % 2][lev % 2]
                k.mm(bank[:, 256:384], Ncur[:], Mcur[:], start=True, stop=True, r=[Ncur, Mcur], w=[bank])
                k.mm(bank[:, 384:512], Mcur[:], Ncur[:], start=True, stop=True, r=[Ncur, Mcur], w=[bank])
                k.copy("act", Mn[:], bank[:, 256:384], r=[bank], w=[Mn])
                k.copy("act", Nn[:], bank[:, 384:512], r=[bank], w=[Nn])
                Mcur, Ncur = Mn, Nn
            k.mm(bank[:, 0:256], Ncur[:], xb[:], start=True, stop=True, r=[Ncur, xb], w=[bank])
            k.tt("dve", xs[:], xs[:], bank[:, 0:256], ALU.subtract if lev == 0 else ALU.add, r=[xs, bank], w=[xs])
            if lev < 5:
                k.copy("act", xb[:], xs[:], r=[xs], w=[xb])
            yield
        k.tr(bsc_[:, 384:512], xs[:, 128:256], I128, r=[xs, c["ident_f"]], w=[bsc_])
        k.copy("dve", WT[:], bsc_[:, 384:512], r=[bsc_], w=[WT])
        yield

    def scan(t, hh, p_):
        par = t % 2
        eGlA, eGlB = eGlAs[par], eGlBs[par]
        S_, qd, qkT, ktail, ztok = Sst[hh], qds[hh][par], qkTs[hh][par], ktails[hh][par], ztoks[hh][par]
        xs, WT = xss[hh][p_], WTs[hh][p_]
        bsc = BSC[hh]
        ss, junk, vn = sss[hh], junks[hh], vns[hh]
        ot, yt = ots[hh][p_ % 2], yts[hh][p_ % 2]
        g_ = hh * 4 + p_
        cs = slice(p_ * 128, (p_ + 1) * 128)
        for c2 in range(2):
            rows = slice(c2 * 64, c2 * 64 + 64)
            egl = (eGlA if c2 == 0 else eGlB)
            k.mm(bsc[:, 0:128], WT[:], S_[:], start=True, stop=True, r=[WT, S_], w=[bsc])
            k.tt("dve", vn[rows, :], xs[rows, 0:128], bsc[rows, 0:128], ALU.subtract, r=[xs, bsc], w=[vn])
            k.mm(bsc[:, 128:256], qd[:, cs], S_[:], start=True, stop=False, r=[qd, S_], w=[bsc])
            k.mm(bsc[:, 128:256], qkT[:, p_, :], vn[:], start=False, stop=True, r=[qkT, vn], w=[bsc])
            k.mm(bsc[:, 256:384], ktail[rows, p_, :], vn[rows, :], start=True, stop=True, r=[ktail, vn], w=[bsc])
            k.stt("dve", S_[:], S_[:], egl[:, g_:g_ + 1], bsc[:, 256:384], ALU.mult, ALU.add, r=[S_, egl, bsc], w=[S_])
            k.copy("dve", ot[rows, :], bsc[rows, 128:256], r=[bsc], w=[ot])
            yield
        k.act(junk[:], ot[:], AF.Square, accum_out=ss[:, 0:1], r=[ot], w=[junk, ss])
        k.ts("dve", ss[:], ss[:], 1.0 / 128, RMS_EPS, ALU.mult, ALU.add, r=[ss], w=[ss])
        k.act(ss[:], ss[:], AF.Sqrt, r=[ss], w=[ss])
        k.op("dve", lambda: nc.vector.reciprocal(out=ss[:], in_=ss[:]), r=[ss], w=[ss])
        k.stt("dve", yt[:], ot[:], ss[:, 0:1], ngB[:], ALU.mult, ALU.mult, r=[ot, ss, ngB], w=[yt])
        k.tt("pool", yt[:], yt[:], ztok[:, p_, :], ALU.mult, r=[yt, ztok], w=[yt])
        k.tr(bsc[:, 384:512], yt[:], I128, r=[yt, c["ident_f"]], w=[bsc])
        k.copy("act", yb[hh][par][:, cs], bsc[:, 384:512], r=[bsc], w=[yb[hh][par]])
        if p_ == 3:
            k.dma("sp", y_out[hh * 128:(hh + 1) * 128, t * 512:(t + 1) * 512], yb[hh][par][:], r=[yb[hh][par]], w=[outb])
        yield


    def rr(gens):
        gens = list(gens)
        alive = [True] * len(gens)
        while any(alive):
            for gi_ in range(len(gens)):
                if alive[gi_]:
                    try:
                        next(gens[gi_])
                    except StopIteration:
                        alive[gi_] = False
            yield

    def seq(gens):
        for g_ in gens:
            for _ in g_:
                yield

    def head_flow(t, hh):
        for _ in solve(t, hh, 0):
            yield
        for p_ in range(4):
            nxt = [solve(t, hh, p_ + 1)] if p_ + 1 < 4 else []
            for _ in rr(nxt + [scan(t, hh, p_)]):
                yield

    def prep(t):
        return seq([pstage(t), front(t, 0), front(t, 1)])

    for _ in prep(0):
        pass
    for t in range(ntiles):
        gens = [rr([head_flow(t, 0), head_flow(t, 1)])]
        if t + 1 < ntiles:
            gens.append(prep(t + 1))
        for _ in rr(gens):
            pass
    barrier(k)
    k.st = k_st
    st.close()
    return outb


DN_IMPL = [phase_dn4]


NIT = 16


def phase_dsa2(k, c, P, io, jq, res, scr, nblk=16):
    nc = k.nc
    st = ExitStack()
    k_st, k.st = k.st, st
    ckv_tok, ckvT, kidxT, widx = res["ckv_tok"], res["ckvT"], res["kidxT"], res["widx"]
    wuv = k.sb([128, 8, 2, 128], BF16)
    k.dma("pool", wuv[:], io["w_uv"].rearrange("h (cc c) d -> c h cc d", c=128), w=[wuv])
    isc = k.sb([128, S], F32)
    maskTs = [k.sb([128, 64, 128], BF16) for _ in range(2)]
    rl = [k.sb([128, 512], F32) for _ in range(2)]
    mk = [k.sb([128, 512], F32) for _ in range(2)]
    qis = [k.sb([128, 4, 128], BF16) for _ in range(2)]
    qls = [k.sb([128, 2, 8, 128], BF16) for _ in range(2)]
    es = [k.sb([128, 4, 128], BF16) for _ in range(3)]
    pts = [k.sb([128, 4, 128], BF16) for _ in range(3)]
    rden = k.sb([128, 512], F32)
    olat = k.sb([128, 2, 512], BF16)
    yas = [k.sb([128, 8, 128], BF16) for _ in range(2)]
    tmpd = k.sb([128, 128], F32)
    tmpp = k.sb([128, 384], F32)
    padb = k.sb([128, 384], F32)
    k.dma("sp", padb[:], io["padb"], w=[padb])
    pw2 = k.sb([128, NIT + 1], F32)
    for it in range(NIT + 1):
        k.memset("pool", pw2[:, it:it + 1], 2.0 ** -(it + 1), w=[pw2])
    steps = k.sb([128, NIT + 1], F32)
    nbias = k.sb([128, 1], F32)
    k.memset("pool", nbias[:], -30000.0, w=[nbias])
    sm = {n: k.sb([128, 1], F32) for n in ("lo", "hi", "mid", "cnt", "u", "R", "m1", "m2", "m3")}
    thr = [k.sb([128, 1], F32) for _ in range(2)]

    def front(i):
        g = 4 * i + jq
        nk = (g + 1) * 128
        nkt = (nk + 511) // 512
        qi, ql = qis[i % 2], qls[i % 2]
        maskT = maskTs[i % 2]
        k.dma("sp", qi[:], scr["qidxT"][i], r=[scr["qidxT_b"][i]], w=[qi])
        k.dma("sp", ql[:], scr["qlatT"][i], r=[scr["qlatT_b"][i]], w=[ql])
        for kt in range(nkt):
            w_ = min(512, nk - kt * 512)
            cols = slice(kt * 512, kt * 512 + w_)
            for h in range(8):
                m, po = h // 2, (h % 2) * 64
                ps = P["c"][h % 2]
                r_ = rl[h % 2]
                k.mm(ps[:, 0:w_], qi[po:po + 64, m, :], kidxT[po:po + 64, cols], start=True, stop=True,
                     r=[qi, kidxT], w=[ps])
                k.act(r_[:, 0:w_], ps[:, 0:w_], AF.Relu, r=[ps], w=[r_])
                if h == 0:
                    k.ts("dve", isc[:, cols], r_[:, 0:w_], widx[:, i, 0:1], None, ALU.mult, r=[r_, widx], w=[isc])
                else:
                    k.stt("dve", isc[:, cols], r_[:, 0:w_], widx[:, i, h:h + 1], isc[:, cols], ALU.mult, ALU.add,
                          r=[r_, widx, isc], w=[isc])
                yield
        dg = slice(nk - 128, nk)
        k.tt("dve", tmpp[:], isc[:, 0:384], padb[:], ALU.subtract, r=[isc, padb], w=[tmpp])
        k.tt("dve", isc[:, 0:384], isc[:, 0:384], padb[:], ALU.add, r=[isc, padb], w=[isc])
        k.op("dve", lambda: nc.vector.tensor_reduce(out=sm["m3"][:], in_=tmpp[:], axis=AX.X, op=ALU.min), r=[tmpp], w=[sm["m3"]])
        k.tt("dve", tmpd[:], isc[:, dg], c["cmask"][:], ALU.subtract, r=[isc, c["cmask"]], w=[tmpd])
        k.tt("dve", isc[:, dg], isc[:, dg], c["cmask"][:], ALU.add, r=[isc, c["cmask"]], w=[isc])
        k.op("dve", lambda: nc.vector.tensor_reduce(out=sm["hi"][:], in_=isc[:, 0:nk], axis=AX.X, op=ALU.max), r=[isc], w=[sm["hi"]])
        k.op("dve", lambda: nc.vector.tensor_reduce(out=sm["m2"][:], in_=tmpd[:], axis=AX.X, op=ALU.min), r=[tmpd], w=[sm["m2"]])
        k.tt("dve", sm["m2"][:], sm["m2"][:], sm["m3"][:], ALU.min, r=[sm["m3"], sm["m2"]], w=[sm["m2"]])
        if nk - 128 > 384:
            k.op("dve", lambda: nc.vector.tensor_reduce(out=sm["m1"][:], in_=isc[:, 384:nk - 128], axis=AX.X, op=ALU.min),
                 r=[isc], w=[sm["m1"]])
            k.tt("dve", sm["m2"][:], sm["m2"][:], sm["m1"][:], ALU.min, r=[sm["m1"], sm["m2"]], w=[sm["m2"]])
        lo, hi, mid, cnt, u, R = (sm[n] for n in ("lo", "hi", "mid", "cnt", "u", "R"))
        k.ts("dve", lo[:], sm["m2"][:], -1.0, None, ALU.add, r=[sm["m2"]], w=[lo])
        k.stt("dve", R[:], hi[:], 1.0, lo[:], ALU.add, ALU.subtract, r=[hi, lo], w=[R])
        k.ts("dve", steps[:], pw2[:], R[:, 0:1], None, ALU.mult, r=[pw2, R], w=[steps])
        k.tt("dve", mid[:], lo[:], steps[:, 0:1], ALU.add, r=[lo, steps], w=[mid])
        yield
        for it in range(NIT):
            k.ts("dve", maskT[:].rearrange("p b t -> p (b t)")[:, 0:nk], isc[:, 0:nk], mid[:, 0:1], 0.0, ALU.is_ge, ALU.add,
                 r=[isc, mid], w=[maskT, cnt], accum_out=cnt[:, 0:1])
            k.ts("dve", u[:], cnt[:], float(TOPK), -0.5, ALU.is_ge, ALU.add, r=[cnt], w=[u])
            k.stt("dve", mid[:], u[:], steps[:, it:it + 1], mid[:], ALU.mult, ALU.add, r=[u, steps, mid], w=[mid])
            yield
        th = thr[i % 2]
        k.tt("dve", th[:], mid[:], steps[:, NIT:NIT + 1], ALU.subtract, r=[mid, steps], w=[th])
        for kt in range(nkt):
            w_ = min(512, nk - kt * 512)
            cols = slice(kt * 512, kt * 512 + w_)
            m_ = mk[kt % 2]
            k.ts("dve", m_[:, 0:w_], isc[:, cols], th[:, 0:1], None, ALU.is_ge, r=[isc, th], w=[m_])
            pt = P["t"]
            nb_ = w_ // 128
            for bb in range(nb_):
                k.tr(pt[:, bb * 128:(bb + 1) * 128], m_[:, bb * 128:(bb + 1) * 128], c["ident_f"][:],
                     r=[m_, c["ident_f"]], w=[pt])
            k.act(maskT[:, kt * 4:kt * 4 + nb_, :], pt[:, 0:w_].rearrange("p (b t) -> p b t", b=nb_), AF.Identity,
                  scale=30000.0, bias=nbias[:, 0:1], r=[pt, nbias], w=[maskT])
            yield

    def n_front(i):
        g = 4 * i + jq
        nkt = ((g + 1) * 128 + 511) // 512
        return nkt * 8 + 1 + NIT + nkt

    def att(i):
        g = 4 * i + jq
        ql, ya = qls[i % 2], yas[i % 2]
        maskT = maskTs[i % 2]
        n = 0
        for hg in range(2):
            acc = P["q"]
            den = P["m"]
            qlf = [ql[:, cc, hg * 4:hg * 4 + 4, :].rearrange("p h t -> p (h t)") for cc in range(2)]

            def scores(kb, n_):
                psS = P["tr"][kb % 2]
                pT = pts[n_ % 3]
                for cc in range(2):
                    k.mm(psS[:, :], ckvT[:, cc, kb * 128:(kb + 1) * 128], qlf[cc], start=(cc == 0), stop=False,
                         r=[ckvT, ql], w=[psS])
                mrow = maskT[:, kb, :]
                apl = [list(x_) for x_ in mrow.ap]
                mb = bass.AP(mrow.tensor, mrow.offset, [apl[0], [0, 4], apl[1]])
                k.mm(psS[:, :], c["ident_b"][:], mb, start=False, stop=True, r=[c["ident_b"], maskT], w=[psS])
                k.act(pT[:].rearrange("p h t -> p (h t)"), psS[:, :], AF.Exp, r=[psS], w=[pT])

            scores(0, n)
            for kb in range(g + 1):
                pT = pts[n % 3]
                if kb + 1 <= g:
                    scores(kb + 1, n + 1)
                pf = pT[:].rearrange("p h t -> p (h t)")
                for cc in range(2):
                    k.mm(acc[cc][:, :], ckv_tok[:, kb, cc * 128:(cc + 1) * 128], pf, start=(kb == 0), stop=(kb == g),
                         r=[ckv_tok, pT], w=[acc[cc]])
                k.mm(den[:, :], c["ones_b"][:], pf, start=(kb == 0), stop=(kb == g), r=[c["ones_b"], pT], w=[den])
                n += 1
                yield
            k.op("dve", lambda: nc.vector.reciprocal(out=rden[:], in_=den[:, :]), r=[den], w=[rden])
            for cc in range(2):
                k.tt("dve", olat[:, cc, :], acc[cc][:, :], rden[:], ALU.mult, r=[acc[cc], rden], w=[olat])
            for hh in range(4):
                h = hg * 4 + hh
                psY = P["tr"][hh % 2]
                for cc in range(2):
                    k.mm(psY[:, 0:128], wuv[:, h, cc, :], olat[:, cc, hh * 128:(hh + 1) * 128], start=(cc == 0),
                         stop=(cc == 1), r=[wuv, olat], w=[psY])
                k.copy("act", ya[:, h, :], psY[:, 0:128], r=[psY], w=[ya])
            yield
        k.dma("sp", scr["yattT"][i], ya[:], r=[ya], w=[scr["yattT_b"][i]])

    def n_att(i):
        return 2 * (4 * i + jq + 2)

    for _ in front(0):
        pass
    for i in range(nblk):
        ga = att(i)
        gb = front(i + 1) if i + 1 < nblk else iter(())
        na = n_att(i)
        nb = n_front(i + 1) if i + 1 < nblk else 0
        done_a = done_b = 0
        a_alive = b_alive = True
        while a_alive or b_alive:
            fa = done_a / na
            fb = done_b / nb if nb else 2.0
            if a_alive and (fa <= fb or not b_alive):
                try:
                    next(ga)
                    done_a += 1
                except StopIteration:
                    a_alive = False
            elif b_alive:
                try:
                    next(gb)
                    done_b += 1
                except StopIteration:
                    b_alive = False
            else:
                break
    barrier(k)
    k.st = k_st
    st.close()


DSA_IMPL = [phase_dsa2]
```

```python
import numpy as np
from contextlib import ExitStack
import concourse.bass as bass
import concourse.mybir as mybir
from concourse.bass_utils import run_bass_kernel_spmd

F32 = mybir.dt.float32
BF16 = mybir.dt.bfloat16
AF = mybir.ActivationFunctionType
ALU = mybir.AluOpType
AX = mybir.AxisListType

D = 1024
S = 8192
NB = 2
DEPTH = 2
ALPHA = (2.0 * DEPTH) ** 0.25
LN_EPS = 1e-5
RMS_EPS = 1e-6
ATT_SCALE = 128 ** -0.5
TOPK = 256
NEG = -1.0e30
C_QATT, C_CKV, C_QIDX, C_KIDX, C_WIDX = 0, 1024, 1280, 1792, 1856
C_DNQ, C_DNK, C_DNV, C_A, C_B, C_Z, C_GATES = 1864, 2888, 3912, 4936, 4944, 4952, 5976
N_IN = 8024


class Buf:
    __slots__ = ("w", "r", "excl")

    def __init__(self, excl=False):
        self.w = None
        self.r = []
        self.excl = excl


class T:
    def __init__(self, t, b=None):
        self.t = t
        self.b = b if b is not None else Buf()

    def __getitem__(self, idx):
        return self.t[idx]


class K:
    NDS = 24

    def __init__(self, nc, st):
        self.nc = nc
        self.st = st
        self.eng = dict(pe=nc.tensor, act=nc.scalar, dve=nc.vector, pool=nc.gpsimd, sp=nc.sync)
        self.sem = {k: st.enter_context(nc.semaphore("s_" + k)) for k in self.eng}
        self.cnt = {k: 0 for k in self.eng}
        self.waited = {}
        self.dsem = [st.enter_context(nc.semaphore("d%d" % i)) for i in range(self.NDS)]
        self.dcnt = [0] * self.NDS
        self.dnext = 0
        self.uid = 0

    def sb(self, shape, dtype, name=None):
        self.uid += 1
        t = self.st.enter_context(self.nc.sbuf_tensor(name or ("sb%d" % self.uid), list(shape), dtype))
        return T(t)

    def ps(self, shape, dtype, name=None):
        self.uid += 1
        t = self.st.enter_context(self.nc.psum_tensor(name or ("ps%d" % self.uid), list(shape), dtype))
        return T(t, Buf(excl=True))

    def _wait(self, eng, tok):
        kind, who, val = tok
        if kind == "e":
            if who == eng and eng == "pe":
                return
            sem = self.sem[who]
            key = (eng, "e", who)
        else:
            sem = self.dsem[who]
            key = (eng, "d", who)
            val = val * 16
        if self.waited.get(key, 0) >= val:
            return
        self.waited[key] = val
        self.eng[eng].wait_ge(sem, val)

    def _deps(self, eng, r, w):
        for b in r:
            b = b.b if isinstance(b, T) else b
            if b.w is not None:
                self._wait(eng, b.w)
            if b.excl:
                for tok in b.r:
                    if not (tok[0] == "e" and tok[1] == eng):
                        self._wait(eng, tok)
        for b in w:
            b = b.b if isinstance(b, T) else b
            if b.w is not None:
                self._wait(eng, b.w)
            for tok in b.r:
                if tok[0] == "e" and tok[1] == eng:
                    continue
                self._wait(eng, tok)

    def _commit(self, tok, r, w):
        wl = [b.b if isinstance(b, T) else b for b in w]
        for b in wl:
            b.w = tok
            b.r = []
        for b in r:
            b = b.b if isinstance(b, T) else b
            if b not in wl:
                if len(b.r) > 12:
                    last = {}
                    for t_ in b.r:
                        key = (t_[0], t_[1])
                        if key not in last or last[key][2] < t_[2]:
                            last[key] = t_
                    b.r = list(last.values())
                b.r.append(tok)

    def op(self, eng, fn, r=(), w=(), inc=True):
        self._deps(eng, r, w)
        inst = fn()
        if inc:
            self.cnt[eng] += 1
            inst.then_inc(self.sem[eng], 1)
            self._commit(("e", eng, self.cnt[eng]), r, w)
        else:
            self._commit(("e", eng, self.cnt[eng] + 1), r, w)
        return inst

    def dma(self, q, out, in_, r=(), w=(), **kw):
        self._deps(q, r, w)
        i = self.dnext
        self.dnext = (self.dnext + 1) % self.NDS
        if self.dcnt[i] > 0:
            self._wait(q, ("d", i, self.dcnt[i]))
        self.dcnt[i] += 1
        inst = self.eng[q].dma_start(out=out, in_=in_, **kw)
        inst.then_inc(self.dsem[i], 16)
        self._commit(("d", i, self.dcnt[i]), r, w)
        return inst

    def finish(self, bufs):
        for b in bufs:
            b = b.b if isinstance(b, T) else b
            if b.w is not None:
                self._wait("sp", b.w)

    def mm(self, out, lhsT, rhs, start, stop, r=(), w=(), inc=True):
        nc = self.nc
        return self.op("pe", lambda: nc.tensor.matmul(out, lhsT=lhsT, rhs=rhs, start=start, stop=stop), r, w, inc=inc)

    def tr(self, out, in_, ident, r=(), w=()):
        nc = self.nc
        return self.op("pe", lambda: nc.tensor.transpose(out, in_, ident), r, w)

    def act(self, out, in_, func, r=(), w=(), **kw):
        nc = self.nc
        return self.op("act", lambda: nc.scalar.activation(out=out, in_=in_, func=func, **kw), r, w)

    def ts(self, eng, out, in0, s1, s2, op0, op1=None, r=(), w=(), accum_out=None):
        e = self.eng[eng]
        kw = {}
        if op1 is not None:
            kw["op1"] = op1
        if accum_out is not None:
            kw["accum_out"] = accum_out
        return self.op(eng, lambda: e.tensor_scalar(out=out, in0=in0, scalar1=s1, scalar2=s2, op0=op0, **kw), r, w)

    def tt(self, eng, out, in0, in1, op, r=(), w=()):
        e = self.eng[eng]
        return self.op(eng, lambda: e.tensor_tensor(out=out, in0=in0, in1=in1, op=op), r, w)

    def stt(self, eng, out, in0, scalar, in1, op0, op1, r=(), w=()):
        e = self.eng[eng]
        return self.op(eng, lambda: e.scalar_tensor_tensor(out=out, in0=in0, scalar=scalar, in1=in1, op0=op0, op1=op1), r, w)

    def copy(self, eng, out, in_, r=(), w=()):
        if eng == "act":
            nc = self.nc
            return self.op("act", lambda: nc.scalar.copy(out=out, in_=in_), r, w)
        e = self.eng[eng]
        return self.op(eng, lambda: e.tensor_copy(out=out, in_=in_), r, w)

    def memset(self, eng, ap, val, w=()):
        e = self.eng[eng]
        return self.op(eng, lambda: e.memset(ap, val), (), w)


def bcast_rows(handle_ap, nparts):
    ap = handle_ap
    n = ap.shape[-1]
    return bass.AP(ap.tensor, ap.offset, [[0, nparts], [1, n]])


def make_consts(k):
    c = {}
    c["ident_f"] = k.sb([128, 128], F32, "ident_f")
    c["ident_b"] = k.sb([128, 128], BF16, "ident_b")
    c["ones_b"] = k.sb([128, 128], BF16, "ones_b")
    c["ones_f"] = k.sb([128, 128], F32, "ones_f")
    nc = k.nc
    k.memset("pool", c["ident_f"][:], 1.0, w=[c["ident_f"]])
    k.op("pool", lambda: nc.gpsimd.affine_select(out=c["ident_f"][:], in_=c["ident_f"][:], pattern=[[-1, 128]],
                                                  compare_op=ALU.is_equal, fill=0.0, base=0, channel_multiplier=1),
         r=[c["ident_f"]], w=[c["ident_f"]])
    k.copy("pool", c["ident_b"][:], c["ident_f"][:], r=[c["ident_f"]], w=[c["ident_b"]])
    k.memset("pool", c["ones_b"][:], 1.0, w=[c["ones_b"]])
    k.memset("pool", c["ones_f"][:], 1.0, w=[c["ones_f"]])
    c["cmask"] = k.sb([128, 128], F32, "cmask")
    k.memset("pool", c["cmask"][:], 0.0, w=[c["cmask"]])
    k.op("pool", lambda: nc.gpsimd.affine_select(out=c["cmask"][:], in_=c["cmask"][:], pattern=[[-1, 128]],
                                                  compare_op=ALU.is_ge, fill=NEG, base=0, channel_multiplier=1),
         r=[c["cmask"]], w=[c["cmask"]])
    return c


def adaln_cols(k, c, psb, cT_d, w_ada_d, b_adaT_d, which):
    nc = k.nc
    nw = len(which)
    modT = k.sb([128, nw, 8], F32)
    st = ExitStack()
    k_st, k.st = k.st, st
    condT = k.sb([128, 8], F32)
    k.dma("sp", condT[:], cT_d, w=[condT])
    sig = k.sb([128, 8], F32)
    k.act(sig[:], condT[:], AF.Sigmoid, r=[condT], w=[sig])
    k.tt("dve", condT[:], condT[:], sig[:], ALU.mult, r=[condT, sig], w=[condT])
    bT = k.sb([128, 48], F32)
    k.dma("sp", bT[:], b_adaT_d, w=[bT])
    wbufs = [k.sb([128, 8, 512], F32) for _ in range(2)]
    n = 0
    for j, wh in enumerate(which):
        for half in range(2):
            wb = wbufs[n % 2]
            n += 1
            c0 = wh * 1024 + half * 512
            k.dma("sp", wb[:], w_ada_d[:, c0:c0 + 512].rearrange("(kk p) n -> p kk n", p=128), w=[wb])
            for cc in range(4):
                for kk in range(8):
                    k.mm(psb[:, cc:cc + 1], wb[:, kk, cc * 128:(cc + 1) * 128], condT[:, kk:kk + 1],
                         start=(kk == 0), stop=(kk == 7), inc=(kk == 7), r=[wb, condT], w=[psb])
            cb = wh * 8 + half * 4
            k.tt("dve", modT[:, j, half * 4:half * 4 + 4], psb[:, 0:4], bT[:, cb:cb + 4], ALU.add,
                 r=[psb, bT], w=[modT])
    barrier(k)
    k.st = k_st
    st.close()
    return modT


def load_w(k, dst, src2d, c0, n, q="pool"):
    return k.dma(q, dst, src2d[:, c0:c0 + n].rearrange("(kk p) n -> p kk n", p=128), w=[])


def transpose_modulate(k, c, xt, hT, pbanks, scp1, sh, ntb, extra=None):
    for kk in range(8):
        pb = pbanks[kk % 2]
        for tb in range(ntb):
            k.tr(pb[:, tb * 128:(tb + 1) * 128], xt[:, tb, kk * 128:(kk + 1) * 128], c["ident_f"][:],
                 r=[xt, c["ident_f"]], w=[pb])
        k.act(hT[:, kk, 0:ntb * 128], pb[:, 0:ntb * 128], AF.Identity, scale=scp1[:, kk:kk + 1], bias=sh[:, kk:kk + 1],
              r=[pb], w=[hT])
        if extra is not None:
            k.act(extra[:, kk, 0:ntb * 128], pb[:, 0:ntb * 128], AF.Identity, scale=scp1[:, kk:kk + 1],
                  bias=sh[:, kk:kk + 1], r=[pb], w=[extra])


def phase_kq(k, c, P, io, jq, res, scr):
    nc = k.nc
    st = ExitStack()
    k_st, k.st = k.st, st
    modT = res["modT"]
    scp1 = k.sb([128, 8], F32)
    k.ts("dve", scp1[:], modT[:, 1, :], 1.0, None, ALU.add, r=[modT], w=[scp1])
    sh1 = modT[:, 0, :]
    w_in = io["w_in"]
    wk = k.sb([128, 8, 320], BF16)
    k.dma("pool", wk[:, :, 0:256], w_in[:, C_CKV:C_CKV + 256].rearrange("(kk p) n -> p kk n", p=128), w=[wk])
    k.dma("pool", wk[:, :, 256:320], w_in[:, C_KIDX:C_KIDX + 64].rearrange("(kk p) n -> p kk n", p=128), w=[wk])
    wq = k.sb([128, 8, 1536], BF16)
    k.dma("pool", wq[:, :, 0:1024], w_in[:, C_QATT:C_QATT + 1024].rearrange("(kk p) n -> p kk n", p=128), w=[wq])
    k.dma("pool", wq[:, :, 1024:1536], w_in[:, C_QIDX:C_QIDX + 512].rearrange("(kk p) n -> p kk n", p=128), w=[wq])
    wwi = k.sb([128, 8, 8], BF16)
    k.dma("pool", wwi[:], w_in[:, C_WIDX:C_WIDX + 8].rearrange("(kk p) n -> p kk n", p=128), w=[wwi])
    wukT = k.sb([128, 8, 256], BF16)
    k.dma("pool", wukT[:], io["w_ukT"].rearrange("h d c -> d h c"), w=[wukT])
    gB = k.sb([128, 256], F32)
    k.dma("sp", gB[:], bcast_rows(io["kv_norm_g"], 128), w=[gB])
    xbufs = [k.sb([128, 4, 1024], F32) for _ in range(2)]
    hbufs = [k.sb([128, 8, 512], BF16) for _ in range(2)]
    qi_sb = [k.sb([128, 4, 128], BF16) for _ in range(2)]
    qa_sb = [k.sb([128, 8, 128], BF16) for _ in range(2)]
    ql_sb = [k.sb([128, 2, 8, 128], BF16) for _ in range(2)]
    ckv_tok, ckvT, kidxT, widx = res["ckv_tok"], res["ckvT"], res["kidxT"], res["widx"]
    import os
    junks = [k.sb([128, 256], F32) for _ in range(4)]
    sss = [k.sb([128, 1], F32) for _ in range(4)]
    rstds = [k.sb([128, 1], F32) for _ in range(4)]
    ckx4 = [k.sb([128, 384], F32) for _ in range(4)]
    pts = [P["m"], P["t"]]
    for t in range(16):
        xt = xbufs[t % 2]
        hT = hbufs[t % 2]
        for tb in range(4):
            k.dma("sp", xt[:, tb, :], io["xrow"](t * 4 + tb), r=[io["xb"]], w=[xt])
        transpose_modulate(k, c, xt, hT, P["tr"], scp1, sh1, 4)

        def kblock(tb):
            blk = t * 4 + tb
            pc = P["c"][tb % 2]
            ckx, junk_, ss_, rstd_ = ckx4[tb], junks[tb], sss[tb], rstds[tb]
            for kk in range(8):
                k.mm(pc[:, 0:320], hT[:, kk, tb * 128:(tb + 1) * 128], wk[:, kk, :], start=(kk == 0), stop=(kk == 7), inc=(kk == 7),
                     r=[hT, wk], w=[pc])
            yield
            k.act(junk_[:], pc[:, 0:256], AF.Square, accum_out=ss_[:, 0:1], r=[pc], w=[junk_, ss_])
            yield
            k.ts("dve", rstd_[:], ss_[:], 1.0 / 256, RMS_EPS, ALU.mult, ALU.add, r=[ss_], w=[rstd_])
            k.act(rstd_[:], rstd_[:], AF.Sqrt, r=[rstd_], w=[rstd_])
            yield
            k.op("dve", lambda: nc.vector.reciprocal(out=rstd_[:], in_=rstd_[:]), r=[rstd_], w=[rstd_])
            k.stt("dve", ckx[:, 0:256], pc[:, 0:256], rstd_[:, 0:1], gB[:], ALU.mult, ALU.mult, r=[pc, rstd_, gB], w=[ckx])
            k.copy("dve", ckx[:, 256:320], pc[:, 256:320], r=[pc], w=[ckx])
            k.copy("dve", ckx[:, 320:384], pc[:, 256:320], r=[pc], w=[ckx])
            yield
            k.copy("pool", ckv_tok[:, blk, :], ckx[:, 0:256], r=[ckx], w=[ckv_tok])
            pt = pts[tb % 2]
            for m in range(3):
                k.tr(pt[:, m * 128:(m + 1) * 128], ckx[:, m * 128:(m + 1) * 128], c["ident_f"][:],
                     r=[ckx, c["ident_f"]], w=[pt])
            yield
            k.copy("act", ckvT[:, :, blk * 128:(blk + 1) * 128], pt[:, 0:256].rearrange("p (cc t) -> p cc t", cc=2),
                   r=[pt], w=[ckvT])
            k.copy("act", kidxT[:, blk * 128:(blk + 1) * 128], pt[:, 256:384], r=[pt], w=[kidxT])

        def qside():
            i = t
            ts_ = slice(jq * 128, (jq + 1) * 128)
            k.dma("sp", scr["hT_own"][i], hT[:, :, ts_], r=[hT], w=[scr["hT_own_b"][i]])
            qi = qi_sb[i % 2]
            pq = P["q"][0]
            for m in range(4):
                for kk in range(8):
                    k.mm(pq[:, m * 128:(m + 1) * 128], wq[:, kk, 1024 + m * 128:1024 + (m + 1) * 128], hT[:, kk, ts_],
                         start=(kk == 0), stop=(kk == 7), inc=(kk == 7), r=[wq, hT], w=[pq])
            yield
            k.copy("act", qi[:], pq[:, 0:512].rearrange("p (m t) -> p m t", m=4), r=[pq], w=[qi])
            k.dma("sp", scr["qidxT"][i], qi[:], r=[qi], w=[scr["qidxT_b"][i]])
            qa = qa_sb[i % 2]
            for half in range(2):
                pq = P["q"][1 - half]
                for m in range(4):
                    h = half * 4 + m
                    for kk in range(8):
                        k.mm(pq[:, m * 128:(m + 1) * 128], wq[:, kk, h * 128:(h + 1) * 128], hT[:, kk, ts_],
                             start=(kk == 0), stop=(kk == 7), inc=(kk == 7), r=[wq, hT], w=[pq])
                yield
                k.copy("dve", qa[:, half * 4:half * 4 + 4, :], pq[:, 0:512].rearrange("p (m t) -> p m t", m=4), r=[pq], w=[qa])
            ql = ql_sb[i % 2]
            for cc in range(2):
                for half in range(2):
                    pq = P["q"][(cc * 2 + half) % 2]
                    for m in range(4):
                        h = half * 4 + m
                        k.mm(pq[:, m * 128:(m + 1) * 128], wukT[:, h, cc * 128:(cc + 1) * 128], qa[:, h, :],
                             start=True, stop=True, r=[wukT, qa], w=[pq])
                    yield
                    k.act(ql[:, cc, half * 4:half * 4 + 4, :], pq[:, 0:512].rearrange("p (m t) -> p m t", m=4), AF.Copy,
                          scale=ATT_SCALE, r=[pq], w=[ql])
            k.dma("sp", scr["qlatT"][i], ql[:], r=[ql], w=[scr["qlatT_b"][i]])
            pw = P["q"][0]
            for kk in range(8):
                k.mm(pw[:, 0:8], hT[:, kk, ts_], wwi[:, kk, :], start=(kk == 0), stop=(kk == 7), inc=(kk == 7), r=[hT, wwi], w=[pw])
            yield
            k.copy("dve", widx[:, i, :], pw[:, 0:8], r=[pw], w=[widx])

        qs = qside()
        for grp in ([kblock(0), kblock(1), qs], [kblock(2), kblock(3), qs]):
            alive = [True] * len(grp)
            while any(alive[0:2]):
                for gi_ in range(len(grp)):
                    if alive[gi_]:
                        try:
                            next(grp[gi_])
                        except StopIteration:
                            alive[gi_] = False
        for _ in qs:
            pass
    k.st = k_st
    return st


def barrier(k):
    for e in k.eng:
        for f in k.eng:
            if f != e and k.cnt[f] > 0:
                k._wait(e, ("e", f, k.cnt[f]))
        for i in range(k.NDS):
            if k.dcnt[i] > 0:
                k._wait(e, ("d", i, k.dcnt[i]))


def dram(nc, name, shape, dtype, kind):
    return nc.dram_tensor(name, list(shape), dtype, kind=kind).ap()


def alloc_psum(k):
    P = {}
    P["tr"] = [k.ps([128, 512], F32) for _ in range(2)]
    P["c"] = [k.ps([128, 512], F32) for _ in range(2)]
    P["q"] = [k.ps([128, 512], F32) for _ in range(2)]
    P["t"] = k.ps([128, 512], F32)
    P["m"] = k.ps([128, 512], F32)
    return P


def alloc_scratch_B(nc):
    scr = {}
    scr["hT_own"] = [dram(nc, "s_hT%d" % i, [128, 8, 128], BF16, "Internal") for i in range(16)]
    scr["hT_own_b"] = [Buf() for _ in range(16)]
    scr["qidxT"] = [dram(nc, "s_qi%d" % i, [128, 4, 128], BF16, "Internal") for i in range(16)]
    scr["qidxT_b"] = [Buf() for _ in range(16)]
    scr["qlatT"] = [dram(nc, "s_ql%d" % i, [128, 2, 8, 128], BF16, "Internal") for i in range(16)]
    scr["qlatT_b"] = [Buf() for _ in range(16)]
    scr["yattT"] = [dram(nc, "s_ya%d" % i, [128, 8, 128], BF16, "Internal") for i in range(16)]
    scr["yattT_b"] = [Buf() for _ in range(16)]
    scr["xm"] = [dram(nc, "s_xm%d" % i, [128, D], F32, "Internal") for i in range(16)]
    scr["xm_b"] = [Buf() for _ in range(16)]
    return scr


def phase_dsa(k, c, P, io, jq, res, scr, nblk=16):
    nc = k.nc
    st = ExitStack()
    k_st, k.st = k.st, st
    ckv_tok, ckvT, kidxT, widx = res["ckv_tok"], res["ckvT"], res["kidxT"], res["widx"]
    wuv = k.sb([128, 8, 2, 128], BF16)
    k.dma("pool", wuv[:], io["w_uv"].rearrange("h (cc c) d -> c h cc d", c=128), w=[wuv])
    isc = k.sb([128, S], F32)
    junk = k.sb([128, S], BF16)
    maskT = k.sb([128, 64, 128], BF16)
    rl = [k.sb([128, 512], F32) for _ in range(2)]
    mk = [k.sb([128, 512], F32) for _ in range(2)]
    qis = [k.sb([128, 4, 128], BF16) for _ in range(2)]
    qls = [k.sb([128, 2, 8, 128], BF16) for _ in range(2)]
    es = [k.sb([128, 4, 128], BF16) for _ in range(2)]
    pts = [k.sb([128, 4, 128], BF16) for _ in range(2)]
    rden = k.sb([128, 512], F32)
    olat = k.sb([128, 2, 512], BF16)
    yas = [k.sb([128, 8, 128], BF16) for _ in range(2)]
    tmpd = k.sb([128, 128], F32)
    tmpp = k.sb([128, 384], F32)
    padb = k.sb([128, 384], F32)
    k.dma("sp", padb[:], io["padb"], w=[padb])
    half = k.sb([128, 1], F32)
    k.memset("dve", half[:], 0.5, w=[half])
    sm = {n: k.sb([128, 1], F32) for n in ("lo", "hi", "mid", "cnt", "sel", "d1", "d2", "m1", "m2", "m3")}
    for i in range(nblk):
        g = 4 * i + jq
        nk = (g + 1) * 128
        nkt = (nk + 511) // 512
        qi, ql, ya = qis[i % 2], qls[i % 2], yas[i % 2]
        k.dma("sp", qi[:], scr["qidxT"][i], r=[scr["qidxT_b"][i]], w=[qi])
        k.dma("sp", ql[:], scr["qlatT"][i], r=[scr["qlatT_b"][i]], w=[ql])
        for kt in range(nkt):
            w_ = min(512, nk - kt * 512)
            cols = slice(kt * 512, kt * 512 + w_)
            for h in range(8):
                m, po = h // 2, (h % 2) * 64
                ps = P["c"][h % 2]
                r_ = rl[h % 2]
                k.mm(ps[:, 0:w_], qi[po:po + 64, m, :], kidxT[po:po + 64, cols], start=True, stop=True,
                     r=[qi, kidxT], w=[ps])
                k.act(r_[:, 0:w_], ps[:, 0:w_], AF.Relu, r=[ps], w=[r_])
                if h == 0:
                    k.ts("dve", isc[:, cols], r_[:, 0:w_], widx[:, i, 0:1], None, ALU.mult, r=[r_, widx], w=[isc])
                else:
                    k.stt("dve", isc[:, cols], r_[:, 0:w_], widx[:, i, h:h + 1], isc[:, cols], ALU.mult, ALU.add,
                          r=[r_, widx, isc], w=[isc])
        dg = slice(nk - 128, nk)
        k.tt("dve", tmpp[:], isc[:, 0:384], padb[:], ALU.subtract, r=[isc, padb], w=[tmpp])
        k.tt("dve", isc[:, 0:384], isc[:, 0:384], padb[:], ALU.add, r=[isc, padb], w=[isc])
        k.op("dve", lambda: nc.vector.tensor_reduce(out=sm["m3"][:], in_=tmpp[:], axis=AX.X, op=ALU.min),
             r=[tmpp], w=[sm["m3"]])
        k.tt("dve", tmpd[:], isc[:, dg], c["cmask"][:], ALU.subtract, r=[isc, c["cmask"]], w=[tmpd])
        k.tt("dve", isc[:, dg], isc[:, dg], c["cmask"][:], ALU.add, r=[isc, c["cmask"]], w=[isc])
        k.op("dve", lambda: nc.vector.tensor_reduce(out=sm["hi"][:], in_=isc[:, 0:nk], axis=AX.X, op=ALU.max),
             r=[isc], w=[sm["hi"]])
        k.op("dve", lambda: nc.vector.tensor_reduce(out=sm["m2"][:], in_=tmpd[:], axis=AX.X, op=ALU.min),
             r=[tmpd], w=[sm["m2"]])
        k.tt("dve", sm["m2"][:], sm["m2"][:], sm["m3"][:], ALU.min, r=[sm["m3"], sm["m2"]], w=[sm["m2"]])
        if nk - 128 > 384:
            k.op("dve", lambda: nc.vector.tensor_reduce(out=sm["m1"][:], in_=isc[:, 384:nk - 128], axis=AX.X, op=ALU.min),
                 r=[isc], w=[sm["m1"]])
            k.tt("dve", sm["m2"][:], sm["m2"][:], sm["m1"][:], ALU.min, r=[sm["m1"], sm["m2"]], w=[sm["m2"]])
        k.ts("dve", sm["lo"][:], sm["m2"][:], -1.0, None, ALU.add, r=[sm["m2"]], w=[sm["lo"]])
        k.ts("dve", sm["hi"][:], sm["hi"][:], 1.0, None, ALU.add, r=[sm["hi"]], w=[sm["hi"]])
        lo, hi, mid, cnt, sel, d1, d2 = (sm[n] for n in ("lo", "hi", "mid", "cnt", "sel", "d1", "d2"))
        for it in range(26):
            k.stt("dve", mid[:], lo[:], hi[:, 0:1], half[:], ALU.add, ALU.mult, r=[lo, hi, half], w=[mid])
            k.memset("dve", cnt[:], 0.0, w=[cnt])
            k.ts("dve", junk[:, 0:nk], isc[:, 0:nk], mid[:, 0:1], 0.0, ALU.is_ge, ALU.add, r=[isc, mid, cnt],
                 w=[junk, cnt], accum_out=cnt[:, 0:1])
            k.ts("dve", sel[:], cnt[:], float(TOPK), None, ALU.is_ge, r=[cnt], w=[sel])
            k.tt("dve", d1[:], mid[:], lo[:], ALU.subtract, r=[mid, lo], w=[d1])
            k.tt("dve", d2[:], hi[:], mid[:], ALU.subtract, r=[mid, hi], w=[d2])
            k.stt("dve", lo[:], d1[:], sel[:, 0:1], lo[:], ALU.mult, ALU.add, r=[d1, sel, lo], w=[lo])
            k.stt("dve", hi[:], d2[:], sel[:, 0:1], mid[:], ALU.mult, ALU.add, r=[d2, sel, mid], w=[hi])
        for kt in range(nkt):
            w_ = min(512, nk - kt * 512)
            cols = slice(kt * 512, kt * 512 + w_)
            m_ = mk[kt % 2]
            k.ts("dve", m_[:, 0:w_], isc[:, cols], lo[:, 0:1], None, ALU.is_ge, r=[isc, lo], w=[m_])
            pt = P["t"]
            nb_ = w_ // 128
            for bb in range(nb_):
                k.tr(pt[:, bb * 128:(bb + 1) * 128], m_[:, bb * 128:(bb + 1) * 128], c["ident_f"][:],
                     r=[m_, c["ident_f"]], w=[pt])
            k.copy("act", maskT[:, kt * 4:kt * 4 + nb_, :], pt[:, 0:w_].rearrange("p (b t) -> p b t", b=nb_),
                   r=[pt], w=[maskT])
        for hg in range(2):
            acc = P["q"]
            den = P["m"]
            for kb in range(g + 1):
                psS = P["tr"][kb % 2]
                e, pT = es[kb % 2], pts[kb % 2]
                for cc in range(2):
                    k.mm(psS[:, :], ckvT[:, cc, kb * 128:(kb + 1) * 128],
                         ql[:, cc, hg * 4:hg * 4 + 4, :].rearrange("p h t -> p (h t)"),
                         start=(cc == 0), stop=(cc == 1), r=[ckvT, ql], w=[psS])
                k.act(e[:].rearrange("p h t -> p (h t)"), psS[:, :], AF.Exp, r=[psS], w=[e])
                for hh in range(4):
                    eng = "dve" if hh % 2 == 0 else "pool"
                    k.tt(eng, pT[:, hh, :], e[:, hh, :], maskT[:, kb, :], ALU.mult, r=[e, maskT], w=[pT])
                pf = pT[:].rearrange("p h t -> p (h t)")
                for cc in range(2):
                    k.mm(acc[cc][:, :], ckv_tok[:, kb, cc * 128:(cc + 1) * 128], pf, start=(kb == 0), stop=(kb == g),
                         r=[ckv_tok, pT], w=[acc[cc]])
                k.mm(den[:, :], c["ones_b"][:], pf, start=(kb == 0), stop=(kb == g), r=[c["ones_b"], pT], w=[den])
            k.op("dve", lambda: nc.vector.reciprocal(out=rden[:], in_=den[:, :]), r=[den], w=[rden])
            for cc in range(2):
                k.tt("dve", olat[:, cc, :], acc[cc][:, :], rden[:], ALU.mult, r=[acc[cc], rden], w=[olat])
            for hh in range(4):
                h = hg * 4 + hh
                psY = P["c"][hh % 2]
                for cc in range(2):
                    k.mm(psY[:, 0:128], wuv[:, h, cc, :], olat[:, cc, hh * 128:(hh + 1) * 128], start=(cc == 0),
                         stop=(cc == 1), r=[wuv, olat], w=[psY])
                k.copy("act", ya[:, h, :], psY[:, 0:128], r=[psY], w=[ya])
        k.dma("sp", scr["yattT"][i], ya[:], r=[ya], w=[scr["yattT_b"][i]])
    barrier(k)
    k.st = k_st
    st.close()


def layer_norm_tile(k, nc, x1, out, gB, bB, sm, junk):
    s1, s2, mean, var, rstd, nb = (sm[n] for n in ("s1", "s2", "mean", "var", "rstd", "nb"))
    k.op("dve", lambda: nc.vector.reduce_sum(out=s1[:], in_=x1, axis=AX.X), r=[sm["x1b"]], w=[s1])
    k.op("act", lambda: nc.scalar.memzero(s2[:]), (), [s2])
    k.act(junk[:], x1, AF.Square, accum_out=s2[:, 0:1], r=[sm["x1b"], s2], w=[junk, s2])
    k.ts("dve", mean[:], s1[:], 1.0 / D, None, ALU.mult, r=[s1], w=[mean])
    k.tt("dve", var[:], mean[:], mean[:], ALU.mult, r=[mean], w=[var])
    k.stt("dve", var[:], s2[:], 1.0 / D, var[:], ALU.mult, ALU.subtract, r=[s2, var], w=[var])
    k.ts("dve", var[:], var[:], LN_EPS, None, ALU.add, r=[var], w=[var])
    k.act(rstd[:], var[:], AF.Sqrt, r=[var], w=[rstd])
    k.op("dve", lambda: nc.vector.reciprocal(out=rstd[:], in_=rstd[:]), r=[rstd], w=[rstd])
    k.stt("dve", nb[:], mean[:], -1.0, rstd[:], ALU.mult, ALU.mult, r=[mean, rstd], w=[nb])
    k.act(junk[:], x1, AF.Identity, scale=rstd[:, 0:1], bias=nb[:, 0:1], r=[sm["x1b"], rstd, nb], w=[junk])
    k.tt("dve", junk[:], junk[:], gB[:], ALU.mult, r=[junk, gB], w=[junk])
    k.tt("dve", out, junk[:], bB[:], ALU.add, r=[junk, bB], w=[sm["outb"]])


def layer_norm_gen(k, nc, x1, out, gB, bB, sm, junk):
    s1, s2, mean, var, rstd, nb = (sm[n] for n in ("s1", "s2", "mean", "var", "rstd", "nb"))
    k.op("dve", lambda: nc.vector.reduce_sum(out=s1[:], in_=x1, axis=AX.X), r=[sm["x1b"]], w=[s1])
    k.act(junk[:], x1, AF.Square, accum_out=s2[:, 0:1], r=[sm["x1b"]], w=[junk, s2])
    yield
    k.ts("dve", mean[:], s1[:], 1.0 / D, None, ALU.mult, r=[s1], w=[mean])
    k.tt("dve", var[:], mean[:], mean[:], ALU.mult, r=[mean], w=[var])
    k.stt("dve", var[:], s2[:], 1.0 / D, var[:], ALU.mult, ALU.subtract, r=[s2, var], w=[var])
    k.ts("dve", var[:], var[:], LN_EPS, None, ALU.add, r=[var], w=[var])
    yield
    k.act(rstd[:], var[:], AF.Sqrt, r=[var], w=[rstd])
    yield
    k.op("dve", lambda: nc.vector.reciprocal(out=rstd[:], in_=rstd[:]), r=[rstd], w=[rstd])
    k.stt("dve", nb[:], mean[:], -1.0, rstd[:], ALU.mult, ALU.mult, r=[mean, rstd], w=[nb])
    yield
    k.act(junk[:], x1, AF.Identity, scale=rstd[:, 0:1], bias=nb[:, 0:1], r=[sm["x1b"], rstd, nb], w=[junk])
    yield
    k.tt("dve", junk[:], junk[:], gB[:], ALU.mult, r=[junk, gB], w=[junk])
    k.tt("dve", out, junk[:], bB[:], ALU.add, r=[junk, bB], w=[sm["outb"]])


def gate_rows(k, c, P, io, which, condB, out):
    st = ExitStack()
    k_st, k.st = k.st, st
    wb = [k.sb([128, 8, 512], F32) for _ in range(2)]
    for half in range(2):
        c0 = which * 1024 + half * 512
        k.dma("sp", wb[half][:], io["w_ada"][:, c0:c0 + 512].rearrange("(kk p) n -> p kk n", p=128), w=[wb[half]])
        ps = P["c"][half]
        for kk in range(8):
            k.mm(ps[:, :], condB[:, kk, :], wb[half][:, kk, :], start=(kk == 0), stop=(kk == 7), inc=(kk == 7), r=[condB, wb[half]], w=[ps])
        bb = k.sb([128, 512], F32)
        k.dma("sp", bb[:], bcast_rows(io["b_ada"][c0:c0 + 512], 128), w=[bb])
        k.stt("dve", out[:, half * 512:(half + 1) * 512], ps[:, :], 1.0, bb[:], ALU.add, ALU.add, r=[ps, bb], w=[out])
    barrier(k)
    k.st = k_st
    st.close()


def make_condB(k, io):
    condT = k.sb([128, 8], F32)
    k.dma("sp", condT[:], io["cT"], w=[condT])
    sig = k.sb([128, 8], F32)
    k.act(sig[:], condT[:], AF.Sigmoid, r=[condT], w=[sig])
    k.tt("dve", condT[:], condT[:], sig[:], ALU.mult, r=[condT, sig], w=[condT])
    condB = k.sb([128, 8, 128], F32)
    for kk in range(8):
        k.ts("dve", condB[:, kk, :], k.c["ones_f"][:], condT[:, kk:kk + 1], None, ALU.mult, r=[k.c["ones_f"], condT], w=[condB])
    return condB


def phase_merge(k, c, P, io, jq, res, scr, lyr):
    nc = k.nc
    st = ExitStack()
    k_st, k.st = k.st, st
    G1 = res["G1"]
    xo = [k.sb([128, D], F32) for _ in range(2)]
    wA = k.sb([128, 8, 1024], BF16)
    wDn = k.sb([128, 8, 1024], BF16)
    wO = k.sb([128, 8, 1024], BF16)
    wG = k.sb([128, 8, 2048], BF16)
    for dst, src in ((wA, io["w_br_att"]), (wDn, io["w_br_dn"]), (wO, io["w_out"])):
        k.dma("pool", dst[:], src.rearrange("(kk p) n -> p kk n", p=128), w=[dst])
    k.dma("pool", wG[:], io["w_in"][:, C_GATES:C_GATES + 2048].rearrange("(kk p) n -> p kk n", p=128), w=[wG])
    gB = k.sb([128, D], F32)
    bB = k.sb([128, D], F32)
    k.dma("sp", gB[:], bcast_rows(io["ln_g"][0], 128), w=[gB])
    k.dma("sp", bB[:], bcast_rows(io["ln_b"][0], 128), w=[bB])
    yaT = k.sb([128, 8, 512], BF16)
    ydT = k.sb([128, 8, 512], BF16)
    hT = k.sb([128, 8, 512], BF16)
    xt = k.sb([128, 4, D], F32)
    mg = k.sb([128, 8, 512], BF16)
    sg = [k.sb([128, 512], F32) for _ in range(2)]
    m1 = k.sb([128, 512], F32)
    m2 = k.sb([128, 512], F32)
    x1s = [k.sb([128, D], F32) for _ in range(2)]
    junks = [k.sb([128, D], F32) for _ in range(2)]
    sms = [{n: k.sb([128, 1], F32) for n in ("s1", "s2", "mean", "var", "rstd", "nb")} for _ in range(2)]
    for tl in range(4):
        for bb in range(4):
            i = tl * 4 + bb
            g = 4 * i + jq
            ts_ = slice(bb * 128, (bb + 1) * 128)
            k.dma("sp", yaT[:, :, ts_], scr["yattT"][i], r=[scr["yattT_b"][i]], w=[yaT])
            for h_ in range(8):
                k.dma("sp", ydT[:, h_, ts_], io["ycol"](i, h_), r=[io["yb"]], w=[ydT])
            k.dma("sp", hT[:, :, ts_], scr["hT_own"][i], r=[scr["hT_own_b"][i]], w=[hT])
            k.dma("sp", xt[:, bb, :], io["xrow"](g), r=[io["xb"]], w=[xt])
        for n in range(8):
            ns = slice(n * 128, (n + 1) * 128)
            psA, psG = P["tr"][0], P["tr"][1]
            for kk in range(8):
                k.mm(psA[:, :], wA[:, kk, ns], yaT[:, kk, :], start=(kk == 0), stop=(kk == 7), inc=(kk == 7), r=[wA, yaT], w=[psA])
            for kk in range(8):
                k.mm(psG[:, :], wG[:, kk, ns], hT[:, kk, :], start=(kk == 0), stop=(kk == 7), inc=(kk == 7), r=[wG, hT], w=[psG])
            k.act(sg[0][:], psG[:, :], AF.Sigmoid, r=[psG], w=[sg[0]])
            k.tt("dve", m1[:], psA[:, :], sg[0][:], ALU.mult, r=[psA, sg[0]], w=[m1])
            psD, psG2 = P["q"][0], P["q"][1]
            for kk in range(8):
                k.mm(psD[:, :], wDn[:, kk, ns], ydT[:, kk, :], start=(kk == 0), stop=(kk == 7), inc=(kk == 7), r=[wDn, ydT], w=[psD])
            for kk in range(8):
                k.mm(psG2[:, :], wG[:, kk, 1024 + n * 128:1024 + (n + 1) * 128], hT[:, kk, :], start=(kk == 0),
                     stop=(kk == 7), inc=(kk == 7), r=[wG, hT], w=[psG2])
            k.act(sg[1][:], psG2[:, :], AF.Sigmoid, r=[psG2], w=[sg[1]])
            k.tt("dve", m2[:], psD[:, :], sg[1][:], ALU.mult, r=[psD, sg[1]], w=[m2])
            k.tt("dve", mg[:, n, :], m1[:], m2[:], ALU.add, r=[m1, m2], w=[mg])
        def y_block(bb, banks, s_):
            i = tl * 4 + bb
            x1, sm = x1s[s_], sms[s_]
            sm["x1b"] = x1.b
            for hf in range(2):
                psY = banks[hf]
                hs = slice(hf * 512, (hf + 1) * 512)
                for n in range(8):
                    k.mm(psY[:, :], mg[:, n, bb * 128:(bb + 1) * 128], wO[:, n, hs], start=(n == 0), stop=(n == 7), inc=(n == 7),
                         r=[mg, wO], w=[psY])
                yield
                k.tt("dve", x1[:, hs], psY[:, :], G1[:, hs], ALU.mult, r=[psY, G1], w=[x1])
                k.stt("dve", x1[:, hs], xt[:, bb, hs], ALPHA, x1[:, hs], ALU.mult, ALU.add, r=[xt, x1], w=[x1])
            yield
            xo_ = xo[i % 2]
            sm["outb"] = xo_.b
            for _ in layer_norm_gen(k, nc, x1[:], xo_[:], gB, bB, sm, junks[s_]):
                yield
            k.dma("sp", scr["xm"][i], xo_[:], r=[xo_], w=[scr["xm_b"][i]])

        for pr_ in range(2):
            gens = [y_block(2 * pr_, P["c"], 0), y_block(2 * pr_ + 1, [P["m"], P["t"]], 1)]
            alive = [True, True]
            while any(alive):
                for gi_ in range(2):
                    if alive[gi_]:
                        try:
                            next(gens[gi_])
                        except StopIteration:
                            alive[gi_] = False
    barrier(k)
    k.st = k_st
    st.close()


def phase_moe(k, c, P, io, jq, res, scr, out_d, on_block=None):
    nc = k.nc
    st = ExitStack()
    k_st, k.st = k.st, st
    G2, modT = res["G2"], res["modT"]
    scp1 = k.sb([128, 8], F32)
    k.ts("dve", scp1[:], modT[:, 4, :], 1.0, None, ALU.add, r=[modT], w=[scp1])
    sh2 = modT[:, 3, :]
    h2T = k.sb([128, 8, 2048], BF16)
    Gm_all = k.sb([128, 16, 32], F32)
    wr = k.sb([128, 8, 36], F32)
    k.dma("sp", wr[:, :, 0:4], io["w_route_grp"].rearrange("(kk p) n -> p kk n", p=128), w=[wr])
    k.dma("sp", wr[:, :, 4:36], io["w_route_exp"].rearrange("(kk p) n -> p kk n", p=128), w=[wr])
    st2 = ExitStack()
    k.st = st2
    h2f = k.sb([128, 8, 512], F32)
    xts = [k.sb([128, 4, D], F32) for _ in range(2)]
    lg = k.sb([128, 36], F32)
    Gm = k.sb([128, 4, 8], F32)
    s = {n: k.sb([128, 1], F32) for n in ("gmax", "ngmax", "sg", "tgp", "m1", "m2", "dm", "g1", "g2")}
    selg = k.sb([128, 4], F32)
    eg = k.sb([128, 4], F32)
    ing = k.sb([128, 8], F32)
    oh1 = k.sb([128, 8], F32)
    oh2 = k.sb([128, 8], F32)
    x2 = k.sb([128, 8], F32)
    ge = k.sb([128, 8], F32)
    for tl in range(4):
        xt_ = xts[tl % 2]
        for bb in range(4):
            k.dma("sp", xt_[:, bb, :], scr["xm"][tl * 4 + bb], r=[scr["xm_b"][tl * 4 + bb]], w=[xt_])
        transpose_modulate(k, c, xt_, T(h2T.t[:, :, tl * 512:(tl + 1) * 512], h2T.b),
                           P["tr"], scp1, sh2, 4, extra=h2f)
        for bb in range(4):
            i = tl * 4 + bb
            pl = P["t"]
            for kk in range(8):
                k.mm(pl[:, 0:36], h2f[:, kk, bb * 128:(bb + 1) * 128], wr[:, kk, :], start=(kk == 0), stop=(kk == 7), inc=(kk == 7),
                     r=[h2f, wr], w=[pl])
            k.copy("dve", lg[:], pl[:, 0:36], r=[pl], w=[lg])
            grp = lg[:, 0:4]
            k.op("dve", lambda: nc.vector.tensor_reduce(out=s["gmax"][:], in_=grp, axis=AX.X, op=ALU.max), r=[lg], w=[s["gmax"]])
            k.ts("dve", selg[:], grp, s["gmax"][:, 0:1], None, ALU.is_equal, r=[lg, s["gmax"]], w=[selg])
            k.ts("dve", s["ngmax"][:], s["gmax"][:], -1.0, None, ALU.mult, r=[s["gmax"]], w=[s["ngmax"]])
            k.op("act", lambda: nc.scalar.memzero(s["sg"][:]), (), [s["sg"]])
            k.act(eg[:], grp, AF.Exp, bias=s["ngmax"][:, 0:1], accum_out=s["sg"][:, 0:1], r=[lg, s["ngmax"], s["sg"]],
                  w=[eg, s["sg"]])
            k.op("dve", lambda: nc.vector.reciprocal(out=s["tgp"][:], in_=s["sg"][:]), r=[s["sg"]], w=[s["tgp"]])
            for g_ in range(4):
                le = lg[:, 4 + g_ * 8:12 + g_ * 8]
                if g_ == 0:
                    k.ts("dve", ing[:], le, selg[:, 0:1], None, ALU.mult, r=[lg, selg], w=[ing])
                else:
                    k.stt("dve", ing[:], le, selg[:, g_:g_ + 1], ing[:], ALU.mult, ALU.add, r=[lg, selg, ing], w=[ing])
            k.op("dve", lambda: nc.vector.tensor_reduce(out=s["m1"][:], in_=ing[:], axis=AX.X, op=ALU.max), r=[ing], w=[s["m1"]])
            k.ts("dve", oh1[:], ing[:], s["m1"][:, 0:1], None, ALU.is_equal, r=[ing, s["m1"]], w=[oh1])
            k.stt("dve", x2[:], oh1[:], NEG, ing[:], ALU.mult, ALU.add, r=[oh1, ing], w=[x2])
            k.op("dve", lambda: nc.vector.tensor_reduce(out=s["m2"][:], in_=x2[:], axis=AX.X, op=ALU.max), r=[x2], w=[s["m2"]])
            k.ts("dve", oh2[:], x2[:], s["m2"][:, 0:1], None, ALU.is_equal, r=[x2, s["m2"]], w=[oh2])
            k.tt("dve", s["dm"][:], s["m2"][:], s["m1"][:], ALU.subtract, r=[s["m1"], s["m2"]], w=[s["dm"]])
            k.act(s["g2"][:], s["dm"][:], AF.Sigmoid, r=[s["dm"]], w=[s["g2"]])
            k.tt("dve", s["g2"][:], s["g2"][:], s["tgp"][:], ALU.mult, r=[s["g2"], s["tgp"]], w=[s["g2"]])
            k.tt("dve", s["g1"][:], s["tgp"][:], s["g2"][:], ALU.subtract, r=[s["g2"], s["tgp"]], w=[s["g1"]])
            k.ts("dve", ge[:], oh1[:], s["g1"][:, 0:1], None, ALU.mult, r=[oh1, s["g1"]], w=[ge])
            k.stt("dve", ge[:], oh2[:], s["g2"][:, 0:1], ge[:], ALU.mult, ALU.add, r=[oh2, s["g2"], ge], w=[ge])
            for g_ in range(4):
                k.ts("dve", Gm[:, g_, :], ge[:], selg[:, g_:g_ + 1], None, ALU.mult, r=[ge, selg], w=[Gm])
            k.copy("dve", Gm_all[:, i, :], Gm[:].rearrange("p g e -> p (g e)"), r=[Gm], w=[Gm_all])
    barrier(k)
    k.st = st
    st2.close()
    yacc = k.sb([128, 16, D], F32)
    st3 = ExitStack()
    k.st = st3
    wgs = [k.sb([128, 8, 512], BF16) for _ in range(2)]
    wus = [k.sb([128, 8, 512], BF16) for _ in range(2)]
    wds = [k.sb([128, 4, 1024], BF16) for _ in range(2)]
    sgt = [k.sb([128, 512], F32) for _ in range(2)]
    hp = k.sb([128, 4, 512], BF16)
    hps = [hp, k.sb([128, 4, 512], BF16)]

    def load_w(e):
        wg, wu, wd = wgs[e % 2], wus[e % 2], wds[e % 2]
        k.dma("pool", wg[:], io["w_gate"][e].rearrange("(kk p) n -> p kk n", p=128), w=[wg])
        k.dma("pool", wu[:], io["w_up"][e].rearrange("(kk p) n -> p kk n", p=128), w=[wu])
        k.dma("pool", wd[:], io["w_down"][e].rearrange("(kk p) n -> p kk n", p=128), w=[wd])

    def gate_up(s_):
        e, tl = s_ // 4, s_ % 4
        if tl == 0:
            load_w(e)
        wg, wu = wgs[e % 2], wus[e % 2]
        tsl = slice(tl * 512, (tl + 1) * 512)
        hp_ = hps[s_ % 2]
        for f in range(4):
            fs = slice(f * 128, (f + 1) * 128)
            psg, psu = P["tr"][f % 2], P["q"][f % 2]
            for kk in range(8):
                k.mm(psg[:, :], wg[:, kk, fs], h2T[:, kk, tsl], start=(kk == 0), stop=(kk == 7), r=[wg, h2T], w=[psg], inc=(kk == 7))
            for kk in range(8):
                k.mm(psu[:, :], wu[:, kk, fs], h2T[:, kk, tsl], start=(kk == 0), stop=(kk == 7), r=[wu, h2T], w=[psu], inc=(kk == 7))
            sg_ = sgt[f % 2]
            k.act(sg_[:], psg[:, :], AF.Silu, r=[psg], w=[sg_])
            k.tt("dve", hp_[:, f, :], psu[:, :], sg_[:], ALU.mult, r=[psu, sg_], w=[hp_])

    def down(s_):
        e, tl = s_ // 4, s_ % 4
        wd = wds[e % 2]
        hp_ = hps[s_ % 2]
        for bb in range(4):
            i = tl * 4 + bb
            for hf in range(2):
                psy = P["c"][hf]
                hs = slice(hf * 512, (hf + 1) * 512)
                for f in range(4):
                    k.mm(psy[:, :], hp_[:, f, bb * 128:(bb + 1) * 128], wd[:, f, hs], start=(f == 0), stop=(f == 3),
                         r=[hp_, wd], w=[psy], inc=(f == 3))
                if e == 0:
                    k.ts("dve", yacc[:, i, hs], psy[:, :], Gm_all[:, i, e:e + 1], None, ALU.mult, r=[psy, Gm_all], w=[yacc])
                else:
                    k.stt("dve", yacc[:, i, hs], psy[:, :], Gm_all[:, i, e:e + 1], yacc[:, i, hs], ALU.mult, ALU.add,
                          r=[psy, Gm_all, yacc], w=[yacc])

    gate_up(0)
    for s_ in range(128):
        if s_ + 1 < 128:
            gate_up(s_ + 1)
        down(s_)
    barrier(k)
    k.st = st
    st3.close()
    gB = k.sb([128, D], F32)
    bB = k.sb([128, D], F32)
    k.dma("sp", gB[:], bcast_rows(io["ln_g"][1], 128), w=[gB])
    k.dma("sp", bB[:], bcast_rows(io["ln_b"][1], 128), w=[bB])
    xr = [k.sb([128, D], F32) for _ in range(2)]
    x1s = [k.sb([128, D], F32) for _ in range(2)]
    junks = [k.sb([128, D], F32) for _ in range(2)]
    ob = [k.sb([128, D], F32) for _ in range(2)]
    sms = [{n: k.sb([128, 1], F32) for n in ("s1", "s2", "mean", "var", "rstd", "nb")} for _ in range(2)]
    outb = [Buf() for _ in range(16)]

    def ln_block(i):
        s_ = i % 2
        xr_, o_, x1, sm = xr[s_], ob[s_], x1s[s_], sms[s_]
        sm["x1b"] = x1.b
        sm["outb"] = o_.b
        k.dma("sp", xr_[:], scr["xm"][i], r=[scr["xm_b"][i]], w=[xr_])
        k.tt("dve", x1[:], yacc[:, i, :], G2[:], ALU.mult, r=[yacc, G2], w=[x1])
        k.stt("dve", x1[:], xr_[:], ALPHA, x1[:], ALU.mult, ALU.add, r=[xr_, x1], w=[x1])
        yield
        for _ in layer_norm_gen(k, nc, x1[:], o_[:], gB, bB, sm, junks[s_]):
            yield
        k.dma("sp", out_d[i * 128:(i + 1) * 128, :], o_[:], r=[o_], w=[outb[i]])

    for i2 in range(8):
        gens = [ln_block(2 * i2), ln_block(2 * i2 + 1)]
        alive = [True, True]
        while any(alive):
            for gi_ in range(2):
                if alive[gi_]:
                    try:
                        next(gens[gi_])
                    except StopIteration:
                        alive[gi_] = False
        if on_block is not None:
            on_block(2 * i2 + 1, outb)
    barrier(k)
    k.st = k_st
    st.close()
    return outb


B_INPUTS = [("x", [S, D], F32), ("cT", [128, 8], F32), ("w_ada", [D, 6 * D], F32), ("b_adaT", [128, 48], F32),
            ("b_ada", [6 * D], F32), ("w_in", [D, N_IN], F32), ("w_ukT", [8, 128, 256], F32), ("w_uv", [8, 256, 128], F32),
            ("kv_norm_g", [256], F32), ("y_dnT", [D, S], BF16), ("w_br_att", [D, D], F32), ("w_br_dn", [D, D], F32),
            ("w_out", [D, D], F32), ("ln_g", [2, D], F32), ("ln_b", [2, D], F32), ("w_route_grp", [D, 4], F32),
            ("w_route_exp", [D, 32], F32), ("w_gate", [32, D, 512], F32), ("w_up", [32, D, 512], F32),
            ("w_down", [32, 512, D], F32), ("padb", [128, 384], F32)]


def build_B(jq):
    nc = bass.Bass("TRN2", target_bir_lowering=False)
    io = {n: dram(nc, n, shp, dt, "ExternalInput") for n, shp, dt in B_INPUTS}
    out_d = dram(nc, "out", [2048, D], F32, "ExternalOutput")
    io["xrow"] = lambda p: io["x"][p * 128:(p + 1) * 128, :]
    io["xb"] = Buf()
    io["ycol"] = lambda i, h: io["y_dnT"][h * 128:(h + 1) * 128, (4 * i + jq) * 128:(4 * i + jq + 1) * 128]
    io["yb"] = Buf()
    scr = alloc_scratch_B(nc)
    with ExitStack() as st:
        k = K(nc, st)
        c = make_consts(k)
        k.c = c
        P = alloc_psum(k)
        res = {}
        res["modT"] = adaln_cols(k, c, P["m"], io["cT"], io["w_ada"], io["b_adaT"], [0, 1, 2, 3, 4, 5])
        res["G1"] = k.sb([128, D], F32)
        res["G2"] = k.sb([128, D], F32)
        stc = ExitStack()
        k_st, k.st = k.st, stc
        condB = make_condB(k, io)
        gate_rows(k, c, P, io, 2, condB, res["G1"])
        gate_rows(k, c, P, io, 5, condB, res["G2"])
        barrier(k)
        k.st = k_st
        stc.close()
        sta = ExitStack()
        k.st = sta
        res["ckv_tok"] = k.sb([128, 64, 256], BF16)
        res["ckvT"] = k.sb([128, 2, S], BF16)
        res["kidxT"] = k.sb([128, S], BF16)
        res["widx"] = k.sb([128, 16, 8], F32)
        stp = phase_kq(k, c, P, io, jq, res, scr)
        barrier(k)
        stp.close()
        DSA_IMPL[0](k, c, P, io, jq, res, scr)
        k.st = k_st
        sta.close()
        phase_merge(k, c, P, io, jq, res, scr, 0)
        outb = phase_moe(k, c, P, io, jq, res, scr, out_d)
        k.finish(outb)
        barrier(k)
    return nc


A_INPUTS = [("x", [S, D], F32), ("cT", [128, 8], F32), ("w_ada", [D, 6 * D], F32), ("b_adaT", [128, 48], F32),
            ("w_dn", [D, 1028], F32), ("convw", [128, 3, 2, 4], F32), ("alog", [2, 1], F32), ("dtb", [2, 1], F32),
            ("normg", [128, 1], F32)]


def build_A(hp, ntiles=16):
    nc = bass.Bass("TRN2", target_bir_lowering=False)
    io = {n: dram(nc, n, shp, dt, "ExternalInput") for n, shp, dt in A_INPUTS}
    y_out = dram(nc, "y_dnT", [256, S], BF16, "ExternalOutput")
    io["xrow_true"] = lambda p: io["x"][p * 128:(p + 1) * 128, :]
    io["xb"] = Buf()
    with ExitStack() as st:
        k = K(nc, st)
        c = make_consts(k)
        k.c = c
        P = alloc_psum(k)
        modT = adaln_cols(k, c, P["m"], io["cT"], io["w_ada"], io["b_adaT"], [0, 1])
        outb = DN_IMPL[0](k, c, P, io, modT, hp, y_out, ntiles)
        k.finish([outb])
        barrier(k)
    return nc


def phase_dn(k, c, P, io, modT, hp, y_out, ntiles=16):
    nc = k.nc
    if True:
        st = ExitStack()
        k_st, k.st = k.st, st
        scp1 = k.sb([128, 8], F32)
        k.ts("dve", scp1[:], modT[:, 1, :], 1.0, None, ALU.add, r=[modT], w=[scp1])
        sh1 = modT[:, 0, :]
        w_dn = io["w_dn"]
        wq = k.sb([128, 8, 4, 2, 128], BF16)
        for si in range(4):
            k.dma("pool", wq[:, :, si, :, :].rearrange("p kk hh n -> p kk (hh n)"),
                  w_dn[:, si * 256:si * 256 + 256].rearrange("(kk p) n -> p kk n", p=128), w=[wq])
        wa = k.sb([128, 8, 2], BF16)
        wb = k.sb([128, 8, 2], BF16)
        k.dma("pool", wa[:], w_dn[:, 1024:1026].rearrange("(kk p) n -> p kk n", p=128), w=[wa])
        k.dma("pool", wb[:], w_dn[:, 1026:1028].rearrange("(kk p) n -> p kk n", p=128), w=[wb])
        cw = k.sb([128, 3, 2, 4], F32)
        k.dma("sp", cw[:], io["convw"], w=[cw])
        alog = k.sb([2, 1], F32)
        dtb = k.sb([2, 1], F32)
        normg = k.sb([128, 1], F32)
        k.dma("sp", alog[:], io["alog"], w=[alog])
        k.dma("sp", dtb[:], io["dtb"], w=[dtb])
        k.dma("sp", normg[:], io["normg"], w=[normg])
        nA = k.sb([2, 1], F32)
        k.act(nA[:], alog[:], AF.Exp, r=[alog], w=[nA])
        k.ts("dve", nA[:], nA[:], -1.0, None, ALU.mult, r=[nA], w=[nA])
        SEL2 = k.sb([2, 2, 128], F32)
        k.memset("pool", SEL2[:], 1.0, w=[SEL2])
        k.op("pool", lambda: nc.gpsimd.affine_select(out=SEL2[:], in_=SEL2[:], pattern=[[-1, 2], [0, 128]],
                                                      compare_op=ALU.is_equal, fill=0.0, base=0, channel_multiplier=1),
             r=[SEL2], w=[SEL2])
        xbufs = [k.sb([128, 4, D], F32) for _ in range(2)]
        hT = k.sb([128, 8, 512], BF16)
        pre = [[k.sb([128, 515], F32) for _ in range(2)] for _ in range(3)]
        for s_ in range(3):
            for hh in range(2):
                k.memset("pool", pre[s_][hh][:, 0:3], 0.0, w=[pre[s_][hh]])
        qkv = [[k.sb([128, 512], F32) for _ in range(2)] for _ in range(3)]
        zs = [k.sb([128, 512], F32) for _ in range(2)]
        acc = k.sb([128, 512], F32)
        sq = k.sb([128, 512], F32)
        rn = k.sb([128, 512], F32)
        arow = k.sb([2, 512], F32)
        brow = k.sb([2, 512], F32)
        aB = [k.sb([128, 512], F32) for _ in range(2)]
        nbB = [k.sb([128, 512], F32) for _ in range(2)]
        Sst = [k.sb([128, 128], F32) for _ in range(2)]
        for hh in range(2):
            k.memset("pool", Sst[hh][:], 0.0, w=[Sst[hh]])
        vp = [k.sb([128, 1], F32) for _ in range(2)]
        VL = [k.sb([128, 128], F32) for _ in range(2)]
        oT = k.sb([128, 512], F32)
        yb = [k.sb([128, 512], BF16) for _ in range(2)]
        outb = Buf()
        pc1 = [P["c"][0], P["c"][1]]
        pVB = [P["q"][0], P["q"][1]]
        pso = [P["tr"][0], P["tr"][1]]
        for t in range(ntiles):
            xt = xbufs[t % 2]
            for tb in range(4):
                k.dma("sp", xt[:, tb, :], io["xrow_true"](t * 4 + tb), r=[io.get("xb_true", io["xb"])], w=[xt])
            transpose_modulate(k, c, xt, hT, [P["m"], P["t"]], scp1, sh1, 4)
            pa = P["m"]
            for kk in range(8):
                k.mm(pa[0:2, :], wa[:, kk, :], hT[:, kk, :], start=(kk == 0), stop=(kk == 7), inc=(kk == 7), r=[wa, hT], w=[pa])
            k.act(arow[:], pa[0:2, :], AF.Exp, bias=dtb[:, 0:1], r=[pa, dtb], w=[arow])
            k.ts("dve", arow[:], arow[:], 1.0, None, ALU.add, r=[arow], w=[arow])
            k.act(arow[:], arow[:], AF.Ln, r=[arow], w=[arow])
            k.act(arow[:], arow[:], AF.Exp, scale=nA[:, 0:1], r=[arow, nA], w=[arow])
            pb_ = P["t"]
            for kk in range(8):
                k.mm(pb_[0:2, :], wb[:, kk, :], hT[:, kk, :], start=(kk == 0), stop=(kk == 7), inc=(kk == 7), r=[wb, hT], w=[pb_])
            k.act(brow[:], pb_[0:2, :], AF.Sigmoid, r=[pb_], w=[brow])
            k.ts("dve", brow[:], brow[:], -1.0, None, ALU.mult, r=[brow], w=[brow])
            for hh in range(2):
                k.mm(pa[:, :], SEL2[:, hh, :], arow[:], start=True, stop=True, r=[SEL2, arow], w=[pa])
                k.copy("act", aB[hh][:], pa[:, :], r=[pa], w=[aB[hh]])
                k.mm(pb_[:, :], SEL2[:, hh, :], brow[:], start=True, stop=True, r=[SEL2, brow], w=[pb_])
                k.copy("act", nbB[hh][:], pb_[:, :], r=[pb_], w=[nbB[hh]])
            for hh in range(2):
                for s_ in range(4):
                    pp = P["m"] if s_ % 2 == 0 else P["t"]
                    for kk in range(8):
                        k.mm(pp[:, :], wq[:, kk, s_, hh, :], hT[:, kk, :], start=(kk == 0), stop=(kk == 7), inc=(kk == 7), r=[wq, hT], w=[pp])
                    if s_ == 3:
                        k.act(zs[hh][:], pp[:, :], AF.Silu, r=[pp], w=[zs[hh]])
                        continue
                    pr = pre[s_][hh]
                    k.copy("act", pr[:, 3:515], pp[:, :], r=[pp], w=[pr])
                    k.ts("dve", acc[:], pr[:, 0:512], cw[:, s_, hh, 0:1], None, ALU.mult, r=[pr, cw], w=[acc])
                    for j in range(1, 4):
                        k.stt("dve", acc[:], pr[:, j:j + 512], cw[:, s_, hh, j:j + 1], acc[:], ALU.mult, ALU.add,
                              r=[pr, cw, acc], w=[acc])
                    k.copy("pool", pr[:, 0:3], pr[:, 512:515], r=[pr], w=[pr])
                    dst = qkv[s_][hh]
                    if s_ == 2:
                        k.act(dst[:], acc[:], AF.Silu, r=[acc], w=[dst])
                        continue
                    k.act(acc[:], acc[:], AF.Silu, r=[acc], w=[acc])
                    k.tt("dve", sq[:], acc[:], acc[:], ALU.mult, r=[acc], w=[sq])
                    k.mm(pp[:, :], c["ones_f"][:], sq[:], start=True, stop=True, r=[c["ones_f"], sq], w=[pp])
                    k.ts("dve", rn[:], pp[:, :], RMS_EPS, None, ALU.add, r=[pp], w=[rn])
                    k.act(rn[:], rn[:], AF.Sqrt, r=[rn], w=[rn])
                    k.op("dve", lambda: nc.vector.reciprocal(out=rn[:], in_=rn[:]), r=[rn], w=[rn])
                    if s_ == 0:
                        k.stt("dve", dst[:], acc[:], 128 ** -0.5, rn[:], ALU.mult, ALU.mult, r=[acc, rn], w=[dst])
                    else:
                        k.tt("dve", dst[:], acc[:], rn[:], ALU.mult, r=[acc, rn], w=[dst])
            for tk in range(512):
                for hh in range(2):
                    S_, q_, k_, v_ = Sst[hh], qkv[0][hh], qkv[1][hh], qkv[2][hh]
                    tc_ = slice(tk, tk + 1)
                    k.mm(pc1[hh][:, 0:1], S_[:], k_[:, tc_], start=True, stop=True, r=[S_, k_], w=[pc1[hh]])
                    k.stt("dve", vp[hh][:], pc1[hh][:, 0:1], aB[hh][:, tc_], v_[:, tc_], ALU.mult, ALU.subtract,
                          r=[pc1[hh], aB[hh], v_], w=[vp[hh]])
                    k.ts("dve", vp[hh][:], vp[hh][:], nbB[hh][:, tc_], None, ALU.mult, r=[vp[hh], nbB[hh]], w=[vp[hh]])
                    k.ts("dve", VL[hh][:], c["ones_f"][:], vp[hh][:, 0:1], None, ALU.mult, r=[c["ones_f"], vp[hh]], w=[VL[hh]])
                    k.mm(pVB[hh][:, 0:128], VL[hh][:], c["ident_f"][:], start=True, stop=True, r=[VL[hh], c["ident_f"]],
                         w=[pVB[hh]])
                    k.ts("pool", S_[:], S_[:], aB[hh][:, tc_], None, ALU.mult, r=[S_, aB[hh]], w=[S_])
                    k.stt("dve", S_[:], pVB[hh][:, 0:128], k_[:, tc_], S_[:], ALU.mult, ALU.add, r=[pVB[hh], k_, S_], w=[S_])
                    k.mm(pso[hh][:, tc_], S_[:], q_[:, tc_], start=True, stop=True, r=[S_, q_], w=[pso[hh]])
            for hh in range(2):
                k.copy("act", oT[:], pso[hh][:, :], r=[pso[hh]], w=[oT])
                k.tt("dve", sq[:], oT[:], oT[:], ALU.mult, r=[oT], w=[sq])
                pp = P["m"]
                k.mm(pp[:, :], c["ones_f"][:], sq[:], start=True, stop=True, r=[c["ones_f"], sq], w=[pp])
                k.ts("dve", rn[:], pp[:, :], 1.0 / 128, RMS_EPS, ALU.mult, ALU.add, r=[pp], w=[rn])
                k.act(rn[:], rn[:], AF.Sqrt, r=[rn], w=[rn])
                k.op("dve", lambda: nc.vector.reciprocal(out=rn[:], in_=rn[:]), r=[rn], w=[rn])
                k.stt("dve", oT[:], oT[:], normg[:, 0:1], rn[:], ALU.mult, ALU.mult, r=[oT, normg, rn], w=[oT])
                y_ = yb[hh]
                k.tt("dve", y_[:], oT[:], zs[hh][:], ALU.mult, r=[oT, zs[hh]], w=[y_])
                k.dma("sp", y_out[hh * 128:(hh + 1) * 128, t * 512:(t + 1) * 512], y_[:], r=[y_], w=[outb])
        barrier(k)
        k.st = k_st
        st.close()
    return outb


JQ = 3
_PROGS = {}


def _prog(name):
    if name not in _PROGS:
        _PROGS[name] = build_A(0) if name == "A" else build_B(JQ)
    return _PROGS[name]


def _w_dn(inp, l, hp):
    w = np.asarray(inp["w_in"][l])
    cols = [w[:, c0 + hp * 256:c0 + hp * 256 + 256] for c0 in (C_DNQ, C_DNK, C_DNV, C_Z)]
    cols += [w[:, C_A + 2 * hp:C_A + 2 * hp + 2], w[:, C_B + 2 * hp:C_B + 2 * hp + 2]]
    return np.ascontiguousarray(np.concatenate(cols, axis=1), dtype=np.float32)


def _a_inputs(inp, l, b, hp, x_b):
    w_dn = _w_dn(inp, l, hp)
    cw = np.asarray(inp["conv_w"][l]).reshape(4, 3, 8, 128)[:, :, 2 * hp:2 * hp + 2, :]
    return {"x": np.ascontiguousarray(x_b, dtype=np.float32),
            "cT": np.ascontiguousarray(np.asarray(inp["c"][b]).reshape(8, 128).T),
            "w_ada": np.ascontiguousarray(inp["w_ada"][l]),
            "b_adaT": np.ascontiguousarray(np.asarray(inp["b_ada"][l]).reshape(48, 128).T),
            "w_dn": w_dn, "convw": np.ascontiguousarray(cw.transpose(3, 1, 2, 0)),
            "alog": np.ascontiguousarray(np.asarray(inp["a_log"][l])[2 * hp:2 * hp + 2].reshape(2, 1)),
            "dtb": np.ascontiguousarray(np.asarray(inp["dt_bias"][l])[2 * hp:2 * hp + 2].reshape(2, 1)),
            "normg": np.ascontiguousarray(np.asarray(inp["dn_norm_g"][l]).reshape(128, 1))}


def _b_inputs(inp, l, b, jq, x_b, y_dnT_b):
    s = (JQ - jq) * 128
    x_c = np.zeros((S, D), np.float32)
    x_c[s:] = x_b[:S - s]
    y_c = np.zeros((D, S), y_dnT_b.dtype)
    y_c[:, s:] = y_dnT_b[:, :S - s]
    padb = np.zeros((128, 384), np.float32)
    padb[:, :s] = NEG
    g = lambda n: np.ascontiguousarray(inp[n][l])
    return {"x": x_c, "cT": np.ascontiguousarray(np.asarray(inp["c"][b]).reshape(8, 128).T),
            "w_ada": g("w_ada"), "b_adaT": np.ascontiguousarray(np.asarray(inp["b_ada"][l]).reshape(48, 128).T),
            "b_ada": g("b_ada"), "w_in": g("w_in"),
            "w_ukT": np.ascontiguousarray(np.asarray(inp["w_uk"][l]).transpose(0, 2, 1)), "w_uv": g("w_uv"),
            "kv_norm_g": g("kv_norm_g"), "y_dnT": y_c, "w_br_att": g("w_br_att"), "w_br_dn": g("w_br_dn"),
            "w_out": g("w_out"), "ln_g": g("ln_g"), "ln_b": g("ln_b"), "w_route_grp": g("w_route_grp"),
            "w_route_exp": g("w_route_exp"), "w_gate": g("w_gate"), "w_up": g("w_up"), "w_down": g("w_down"),
            "padb": padb}


def kernel(**inp):
    inp = {k_: np.asarray(v) for k_, v in inp.items()}
    x_cur = np.asarray(inp["x"], dtype=np.float32)
    cores = list(range(8))
    for l in range(DEPTH):
        maps = [_a_inputs(inp, l, cid // 4, cid % 4, x_cur[cid // 4]) for cid in cores]
        resA = run_bass_kernel_spmd(_prog("A"), maps, core_ids=cores).results
        y_dnT = [np.concatenate([resA[b * 4 + hp]["y_dnT"] for hp in range(4)], axis=0) for b in range(NB)]
        maps = [_b_inputs(inp, l, cid // 4, cid % 4, x_cur[cid // 4], y_dnT[cid // 4]) for cid in cores]
        resB = run_bass_kernel_spmd(_prog("B"), maps, core_ids=cores).results
        x_next = np.empty_like(x_cur)
        for cid in cores:
            b, jq = cid // 4, cid % 4
            o = resB[cid]["out"]
            for i in range(16):
                gblk = 4 * i + jq
                x_next[b, gblk * 128:(gblk + 1) * 128] = o[i * 128:(i + 1) * 128]
        x_cur = x_next
    return x_cur.astype(np.float32)


F_SHARED = [("cT", [128, 8], F32), ("w_ada", [2, D, 6 * D], F32), ("b_adaT", [2, 128, 48], F32), ("b_ada", [2, 6 * D], F32),
            ("w_in", [2, D, N_IN], F32), ("w_ukT", [2, 8, 128, 256], F32), ("w_uv", [2, 8, 256, 128], F32),
            ("kv_norm_g", [2, 256], F32), ("w_br_att", [2, D, D], F32), ("w_br_dn", [2, D, D], F32), ("w_out", [2, D, D], F32),
            ("ln_g", [2, 2, D], F32), ("ln_b", [2, 2, D], F32), ("w_route_grp", [2, D, 4], F32),
            ("w_route_exp", [2, D, 32], F32), ("w_gate", [2, 32, D, 512], F32), ("w_up", [2, 32, D, 512], F32),
            ("w_down", [2, 32, 512, D], F32), ("w_dn", [2, D, 1028], F32), ("convw", [2, 128, 3, 2, 4], F32),
            ("alog", [2, 2, 1], F32), ("dtb", [2, 2, 1], F32), ("normg", [2, 128, 1], F32)]
F_OTHER = [("xpad", [67 * 128, D], F32), ("padb", [128, 384], F32)]
GROUPS = [[0, 1, 2, 3], [4, 5, 6, 7]]


def build_fused():
    nc = bass.Bass("TRN2", target_bir_lowering=False)
    io = {n: dram(nc, n, shp, dt, "ExternalInput") for n, shp, dt in F_SHARED + F_OTHER}
    out_d = dram(nc, "out", [2048, D], F32, "ExternalOutput")
    xt1 = dram(nc, "xt1", [67 * 128, D], F32, "Internal")
    ysrc = [dram(nc, "ysrc%d" % l, [256, S], BF16, "Internal") for l in range(2)]
    ygc = [[dram(nc, "ygc%d_%d" % (l, q), [256, S], BF16, "Internal") for q in range(4)] for l in range(2)]
    osrc = dram(nc, "osrc", [2048, D], F32, "Internal")
    ogc = [dram(nc, "ogc%d" % q, [4 * 256, D], F32, "Internal") for q in range(8)]
    xloc = dram(nc, "xloc", [64 * 128, D], F32, "Internal")
    yloc = dram(nc, "yloc", [1024, S - 384], BF16, "Internal")
    scr = alloc_scratch_B(nc)
    from concourse.bass import ds
    with ExitStack() as st:
        jv = nc.sync.snap(nc.sync.partition_id() % 4, min_val=0, max_val=3)
        jva = nc.scalar.snap(nc.scalar.partition_id() % 4, min_val=0, max_val=3)
        k = K(nc, st)
        c = make_consts(k)
        k.c = c
        P = alloc_psum(k)
        xt1b = Buf()
        zt = k.sb([128, D], F32)
        k.memset("pool", zt[:], 0.0, w=[zt])
        for p_ in range(3):
            k.dma("sp", xt1[p_ * 128:(p_ + 1) * 128, :], zt[:], r=[zt], w=[xt1b])
        xsrc = [io["xpad"], xt1]
        xbufs_ = [Buf(), xt1b]
        def _probe(tag):
            import os
            if not os.environ.get("PROBE"):
                return
            try:
                k.dma("sp", zt[:], io["xpad"][ds(jv * 128, 128), :], w=[zt])
                print("probe", tag, "ok", flush=True)
            except Exception as e:
                print("probe", tag, "FAIL", repr(e)[:80], flush=True)
        final = None
        for l in range(DEPTH):
            iol = {n: io[n][l] for n, _, _ in F_SHARED if n != "cT"}
            iol["cT"] = io["cT"]
            iol["padb"] = io["padb"]
            X = xsrc[l]
            iol["xb"] = xbufs_[l]
            iol["xb_true"] = xbufs_[l]
            iol["xrow_true"] = (lambda X_: (lambda p: X_[(3 + p) * 128:(4 + p) * 128, :]))(X)
            xlocb = Buf()
            for q_ in range(4):
                k.dma("act", xloc[q_ * 2048:(q_ + 1) * 2048, :], X[ds(jva * 128 + q_ * 2048, 2048), :],
                      r=[xbufs_[l]], w=[xlocb])
            iol["xrow"] = lambda p: xloc[p * 128:(p + 1) * 128, :]
            iol["xb"] = xlocb
            iol["ycol"] = lambda i, h: yloc[h * 128:(h + 1) * 128, i * 512:i * 512 + 128]
            iol["yb"] = Buf()
            ygb = Buf()
            stl = ExitStack()
            k_st, k.st = k.st, stl
            res = {}
            _probe("layer start")
            res["modT"] = adaln_cols(k, c, P["m"], iol["cT"], iol["w_ada"], iol["b_adaT"], [0, 1, 2, 3, 4, 5])
            _probe("after adaln")
            res["G1"] = k.sb([128, D], F32)
            res["G2"] = k.sb([128, D], F32)
            stc = ExitStack()
            k.st = stc
            condB = make_condB(k, iol)
            gate_rows(k, c, P, iol, 2, condB, res["G1"])
            gate_rows(k, c, P, iol, 5, condB, res["G2"])
            barrier(k)
            k.st = stl
            stc.close()
            _probe("after gates")
            youtb = DN_IMPL[0](k, c, P, iol, res["modT"], 0, ysrc[l])
            _probe("after dn")
            ylv = yloc.rearrange("(r q w) t -> q r w t", r=4, q=4)
            for q_ in range(4):
                k.op("pool", lambda: nc.gpsimd.collective_compute(
                    "AllGather", ALU.bypass, replica_groups=GROUPS,
                    ins=[ysrc[l][q_ * 64:(q_ + 1) * 64, :].opt()], outs=[ygc[l][q_].opt()]), r=[youtb], w=[ygb])
            sta = ExitStack()
            k.st = sta
            res["ckv_tok"] = k.sb([128, 64, 256], BF16)
            res["ckvT"] = k.sb([128, 2, S], BF16)
            res["kidxT"] = k.sb([128, S], BF16)
            res["widx"] = k.sb([128, 16, 8], F32)
            stp = phase_kq(k, c, P, iol, JQ, res, scr)
            barrier(k)
            stp.close()
            DSA_IMPL[0](k, c, P, iol, JQ, res, scr)
            k.st = stl
            sta.close()
            for q_ in range(4):
                k.dma("sp", ylv[q_], ygc[l][q_][:, ds(jv * 128, S - 384)].rearrange("(r w) t -> r w t", r=4),
                      r=[ygb], w=[iol["yb"]])
            phase_merge(k, c, P, iol, JQ, res, scr, l)
            dest = osrc if l == 0 else out_d
            if l == 0:
                xv = xt1[384:, :].rearrange("(i j p) d -> i j p d", j=4, p=128)

                def on_block(i_, outb_):
                    if i_ % 2 == 0:
                        return
                    q_ = i_ // 2
                    ogb = Buf()
                    k.op("pool", lambda: nc.gpsimd.collective_compute(
                        "AllGather", ALU.bypass, replica_groups=GROUPS,
                        ins=[osrc[q_ * 256:(q_ + 1) * 256, :].opt()], outs=[ogc[q_].opt()]),
                         r=[outb_[i_ - 1], outb_[i_]], w=[ogb])
                    sv = ogc[q_].rearrange("(j i p) d -> i j p d", j=4, i=2, p=128)
                    for i2 in range(2):
                        k.dma("act", xv[2 * q_ + i2], sv[i2], r=[ogb], w=[xt1b])

                phase_moe(k, c, P, iol, JQ, res, scr, dest, on_block=on_block)
            else:
                final = phase_moe(k, c, P, iol, JQ, res, scr, dest)
            barrier(k)
            k.st = k_st
            stl.close()
        k.finish(final)
        barrier(k)
    return nc


def _fused_inputs(inp, cid):
    b, j = cid // 4, cid % 4
    m = {}
    for n, _, _ in F_SHARED:
        if n in ("cT", "b_adaT", "w_ukT", "w_dn", "convw", "alog", "dtb", "normg"):
            continue
        m[n] = np.ascontiguousarray(inp[n], dtype=np.float32)
    m["cT"] = np.ascontiguousarray(inp["c"][b].reshape(8, 128).T)
    m["b_adaT"] = np.ascontiguousarray(inp["b_ada"].reshape(2, 48, 128).transpose(0, 2, 1))
    m["w_ukT"] = np.ascontiguousarray(inp["w_uk"].transpose(0, 1, 3, 2))
    m["w_dn"] = np.stack([_w_dn(inp, l, j) for l in range(2)])
    cw = inp["conv_w"].reshape(2, 4, 3, 8, 128)[:, :, :, 2 * j:2 * j + 2, :]
    m["convw"] = np.ascontiguousarray(cw.transpose(0, 4, 2, 3, 1))
    m["alog"] = np.ascontiguousarray(inp["a_log"][:, 2 * j:2 * j + 2].reshape(2, 2, 1))
    m["dtb"] = np.ascontiguousarray(inp["dt_bias"][:, 2 * j:2 * j + 2].reshape(2, 2, 1))
    m["normg"] = np.ascontiguousarray(inp["dn_norm_g"].reshape(2, 128, 1))
    xpad = np.zeros((67 * 128, D), np.float32)
    xpad[384:] = inp["x"][b]
    m["xpad"] = xpad
    padb = np.zeros((128, 384), np.float32)
    padb[:, :(JQ - j) * 128] = NEG
    m["padb"] = padb
    return m


def kernel_unfused(**inp):
    return _kernel_unfused(**inp)


_kernel_unfused = kernel


def kernel(**inp):
    inp = {k_: np.asarray(v) for k_, v in inp.items()}
    if "F" not in _PROGS:
        _PROGS["F"] = build_fused()
    cores = list(range(8))
    maps = [_fused_inputs(inp, cid) for cid in cores]
    res = run_bass_kernel_spmd(_PROGS["F"], maps, core_ids=cores).results
    out = np.empty((NB, S, D), np.float32)
    for cid in cores:
        b, j = cid // 4, cid % 4
        o = res[cid]["out"]
        for i in range(16):
            g = 4 * i + j
            out[b, g * 128:(g + 1) * 128] = o[i * 128:(i + 1) * 128]
    return out


def phase_dn2(k, c, P, io, modT, hp, y_out, ntiles=16):
    nc = k.nc
    st = ExitStack()
    k_st, k.st = k.st, st
    scp1 = k.sb([128, 8], F32)
    k.ts("dve", scp1[:], modT[:, 1, :], 1.0, None, ALU.add, r=[modT], w=[scp1])
    sh1 = modT[:, 0, :]
    w_dn = io["w_dn"]
    wq = k.sb([128, 8, 4, 2, 128], BF16)
    for si in range(4):
        k.dma("pool", wq[:, :, si, :, :].rearrange("p kk hh n -> p kk (hh n)"),
              w_dn[:, si * 256:si * 256 + 256].rearrange("(kk p) n -> p kk n", p=128), w=[wq])
    wab = k.sb([128, 8, 4], BF16)
    k.dma("pool", wab[:], w_dn[:, 1024:1028].rearrange("(kk p) n -> p kk n", p=128), w=[wab])
    cw = k.sb([128, 3, 2, 4], F32)
    k.dma("sp", cw[:], io["convw"], w=[cw])
    I64 = c["ident_f"][0:64, 0:64]
    ones64 = c["ones_f"][0:64, 0:64]
    ones64_128 = c["ones_f"][0:64, :]
    dtbB = k.sb([64, 2], F32)
    nAB = k.sb([64, 2], F32)
    k.dma("sp", dtbB[:], bass.AP(io["dtb"].tensor, io["dtb"].offset, [[0, 64], [1, 2]]), w=[dtbB])
    k.dma("sp", nAB[:], bass.AP(io["alog"].tensor, io["alog"].offset, [[0, 64], [1, 2]]), w=[nAB])
    k.act(nAB[:], nAB[:], AF.Exp, r=[nAB], w=[nAB])
    k.ts("dve", nAB[:], nAB[:], -1.0, None, ALU.mult, r=[nAB], w=[nAB])
    ngB = k.sb([64, 128], F32)
    k.dma("sp", ngB[:], bass.AP(io["normg"].tensor, io["normg"].offset, [[0, 64], [1, 128]]), w=[ngB])
    UT = k.sb([64, 64], F32)
    k.memset("pool", UT[:], 1.0, w=[UT])
    k.op("pool", lambda: nc.gpsimd.affine_select(out=UT[:], in_=UT[:], pattern=[[1, 64]], compare_op=ALU.is_ge,
                                                  fill=0.0, base=0, channel_multiplier=-1), r=[UT], w=[UT])
    TRIU = k.sb([64, 8, 64], F32)
    TRILS = k.sb([64, 8, 64], F32)
    k.memset("pool", TRIU[:], 1.0, w=[TRIU])
    k.op("pool", lambda: nc.gpsimd.affine_select(out=TRIU[:], in_=TRIU[:], pattern=[[0, 8], [1, 64]], compare_op=ALU.is_ge,
                                                  fill=0.0, base=0, channel_multiplier=-1), r=[TRIU], w=[TRIU])
    k.memset("pool", TRILS[:], 1.0, w=[TRILS])
    k.op("pool", lambda: nc.gpsimd.affine_select(out=TRILS[:], in_=TRILS[:], pattern=[[0, 8], [-1, 64]], compare_op=ALU.is_gt,
                                                  fill=0.0, base=0, channel_multiplier=1), r=[TRILS], w=[TRILS])
    xbufs = [k.sb([128, 4, D], F32) for _ in range(2)]
    hT = k.sb([128, 8, 512], BF16)
    pre = [[k.sb([128, 515], F32) for _ in range(2)] for _ in range(3)]
    for s_ in range(3):
        for hh in range(2):
            k.memset("pool", pre[s_][hh][:, 0:3], 0.0, w=[pre[s_][hh]])
    qkv = [[k.sb([128, 512], F32) for _ in range(2)] for _ in range(3)]
    zs = [k.sb([128, 512], F32) for _ in range(2)]
    acc = k.sb([128, 512], F32)
    sq = k.sb([128, 512], F32)
    rn = k.sb([128, 512], F32)
    Sst = [k.sb([128, 128], F32) for _ in range(2)]
    for hh in range(2):
        k.memset("pool", Sst[hh][:], 0.0, w=[Sst[hh]])
    gtok = k.sb([64, 2, 8], F32)
    betok = k.sb([64, 2, 8], F32)
    Gs = k.sb([64, 16], F32)
    eG = k.sb([64, 16], F32)
    eGlG = k.sb([64, 16], F32)
    eGl128 = k.sb([128, 16], F32)
    rhsG = k.sb([64, 8, 64], F32)
    eGB = k.sb([128, 512], F32)
    qd = k.sb([128, 512], F32)
    dd = k.sb([64, 8, 64], F32)
    e1 = k.sb([64, 8, 64], F32)
    e2 = k.sb([64, 8, 64], F32)
    Am = [k.sb([64, 8, 64], F32), k.sb([64, 4, 64], F32)]
    An = [k.sb([64, 8, 64], F32), k.sb([64, 4, 64], F32)]
    Mq = [k.sb([64, 4, 64], F32) for _ in range(2)]
    Nq = [k.sb([64, 4, 64], F32) for _ in range(2)]
    qkT = k.sb([64, 8, 64], F32)
    ktok = k.sb([64, 8, 128], F32)
    vtok = k.sb([64, 8, 128], F32)
    ztok = k.sb([64, 8, 128], F32)
    ktail = k.sb([64, 8, 128], F32)
    xs = k.sb([64, 4, 256], F32)
    WT = k.sb([128, 4, 64], F32)
    vnew = [k.sb([64, 128], F32) for _ in range(2)]
    otok = [k.sb([64, 128], F32) for _ in range(2)]
    ytok = [k.sb([64, 128], F32) for _ in range(2)]
    junk = k.sb([64, 128], F32)
    ss = k.sb([64, 1], F32)
    yb = [k.sb([128, 512], BF16) for _ in range(2)]
    outb = Buf()
    B_GB, B_KK, B_QK, B_TR, B_XA, B_XB, B_MN, B_SC = P["c"][0], P["c"][1], P["q"][0], P["tr"][0], P["m"], P["t"], P["q"][1], P["tr"][1]
    for t in range(ntiles):
        xt = xbufs[t % 2]
        for tb in range(4):
            k.dma("sp", xt[:, tb, :], io["xrow_true"](t * 4 + tb), r=[io.get("xb_true", io["xb"])], w=[xt])
        transpose_modulate(k, c, xt, hT, [P["m"], P["t"]], scp1, sh1, 4)
        pab = B_XA
        for cc in range(8):
            for kk in range(8):
                k.mm(pab[0:64, cc * 4:cc * 4 + 4], hT[:, kk, cc * 64:(cc + 1) * 64], wab[:, kk, :], start=(kk == 0),
                     stop=(kk == 7), inc=(kk == 7), r=[hT, wab], w=[pab])
        pabv = pab[0:64, 0:32].rearrange("p (cc f) -> p cc f", f=4)
        for hh in range(2):
            k.act(gtok[:, hh, :], pabv[:, :, hh], AF.Exp, bias=dtbB[:, hh:hh + 1], r=[pab, dtbB], w=[gtok])
            k.act(betok[:, hh, :], pabv[:, :, 2 + hh], AF.Sigmoid, r=[pab], w=[betok])
        k.ts("dve", gtok[:], gtok[:], 1.0, None, ALU.add, r=[gtok], w=[gtok])
        k.act(gtok[:], gtok[:], AF.Ln, r=[gtok], w=[gtok])
        for hh in range(2):
            k.ts("dve", gtok[:, hh, :], gtok[:, hh, :], nAB[:, hh:hh + 1], None, ALU.mult, r=[gtok, nAB], w=[gtok])
        gflat = gtok[:].rearrange("p h cc -> p (h cc)")
        pG = B_XB
        k.mm(pG[0:64, 0:16], UT[:], gflat, start=True, stop=True, r=[UT, gtok], w=[pG])
        k.mm(pG[0:64, 16:32], ones64, gflat, start=True, stop=True, r=[c["ones_f"], gtok], w=[pG])
        k.mm(pG[:, 32:48], ones64_128, gflat, start=True, stop=True, r=[c["ones_f"], gtok], w=[pG])
        k.copy("dve", Gs[:], pG[0:64, 0:16], r=[pG], w=[Gs])
        k.tt("dve", eGlG[:], pG[0:64, 16:32], Gs[:], ALU.subtract, r=[pG, Gs], w=[eGlG])
        k.copy("dve", eGl128[:], pG[:, 32:48], r=[pG], w=[eGl128])
        k.act(eG[:], Gs[:], AF.Exp, r=[Gs], w=[eG])
        k.act(eGlG[:], eGlG[:], AF.Exp, r=[eGlG], w=[eGlG])
        k.act(eGl128[:], eGl128[:], AF.Exp, r=[eGl128], w=[eGl128])
        for hh in range(2):
            for s_ in range(4):
                pp = P["m"] if s_ % 2 == 0 else P["t"]
                for kk in range(8):
                    k.mm(pp[:, :], wq[:, kk, s_, hh, :], hT[:, kk, :], start=(kk == 0), stop=(kk == 7), inc=(kk == 7), r=[wq, hT], w=[pp])
                if s_ == 3:
                    k.act(zs[hh][:], pp[:, :], AF.Silu, r=[pp], w=[zs[hh]])
                    continue
                pr = pre[s_][hh]
                k.copy("act", pr[:, 3:515], pp[:, :], r=[pp], w=[pr])
                k.ts("dve", acc[:], pr[:, 0:512], cw[:, s_, hh, 0:1], None, ALU.mult, r=[pr, cw], w=[acc])
                for j in range(1, 4):
                    k.stt("dve", acc[:], pr[:, j:j + 512], cw[:, s_, hh, j:j + 1], acc[:], ALU.mult, ALU.add,
                          r=[pr, cw, acc], w=[acc])
                k.copy("pool", pr[:, 0:3], pr[:, 512:515], r=[pr], w=[pr])
                dst = qkv[s_][hh]
                if s_ == 2:
                    k.act(dst[:], acc[:], AF.Silu, r=[acc], w=[dst])
                    continue
                k.act(acc[:], acc[:], AF.Silu, r=[acc], w=[acc])
                k.tt("dve", sq[:], acc[:], acc[:], ALU.mult, r=[acc], w=[sq])
                k.mm(pp[:, :], c["ones_f"][:], sq[:], start=True, stop=True, r=[c["ones_f"], sq], w=[pp])
                k.ts("dve", rn[:], pp[:, :], RMS_EPS, None, ALU.add, r=[pp], w=[rn])
                k.act(rn[:], rn[:], AF.Sqrt, r=[rn], w=[rn])
                k.op("dve", lambda: nc.vector.reciprocal(out=rn[:], in_=rn[:]), r=[rn], w=[rn])
                if s_ == 0:
                    k.stt("dve", dst[:], acc[:], 128 ** -0.5, rn[:], ALU.mult, ALU.mult, r=[acc, rn], w=[dst])
                else:
                    k.tt("dve", dst[:], acc[:], rn[:], ALU.mult, r=[acc, rn], w=[dst])
        for hh in range(2):
            q_, k_, v_, S_ = qkv[0][hh], qkv[1][hh], qkv[2][hh], Sst[hh]
            gi = lambda cc: hh * 8 + cc
            for cc in range(8):
                k.ts("pool", rhsG[:, cc, :], UT[:], gtok[:, hh, cc:cc + 1], None, ALU.mult, r=[UT, gtok], w=[rhsG])
            k.mm(B_GB[:, :], ones64_128, rhsG[:].rearrange("p cc i -> p (cc i)"), start=True, stop=True,
                 r=[c["ones_f"], rhsG], w=[B_GB])
            k.act(eGB[:], B_GB[:, :], AF.Exp, r=[B_GB], w=[eGB])
            k.tt("pool", qd[:], q_[:], eGB[:], ALU.mult, r=[q_, eGB], w=[qd])
            for cc in range(8):
                k.ts("dve", dd[:, cc, :], B_GB[0:64, cc * 64:(cc + 1) * 64], Gs[:, gi(cc):gi(cc) + 1], None, ALU.subtract,
                     r=[B_GB, Gs], w=[dd])
            k.ts("dve", e1[:], dd[:], 0.0, None, ALU.min, r=[dd], w=[e1])
            k.ts("dve", e2[:], dd[:], 0.0, None, ALU.max, r=[dd], w=[e2])
            k.act(e1[:], e1[:], AF.Exp, r=[e1], w=[e1])
            k.act(e2[:], e2[:], AF.Exp, scale=-1.0, r=[e2], w=[e2])
            k.tt("pool", e1[:], e1[:], TRIU[:], ALU.mult, r=[e1, TRIU], w=[e1])
            k.tt("pool", e2[:], e2[:], TRILS[:], ALU.mult, r=[e2, TRILS], w=[e2])
            for cc in range(8):
                cs = slice(cc * 64, (cc + 1) * 64)
                k.mm(B_KK[0:64, cs], k_[:, cs], k_[:, cs], start=True, stop=True, r=[k_], w=[B_KK])
                k.mm(B_QK[0:64, cs], k_[:, cs], q_[:, cs], start=True, stop=True, r=[k_, q_], w=[B_QK])
            A0, AT0 = Am[0], An[0]
            for cc in range(8):
                k.stt("dve", A0[:, cc, :], B_KK[0:64, cc * 64:(cc + 1) * 64], betok[:, hh, cc:cc + 1], e2[:, cc, :],
                      ALU.mult, ALU.mult, r=[B_KK, betok, e2], w=[A0])
            k.tt("dve", qkT[:].rearrange("p cc i -> p (cc i)"), B_QK[0:64, :], e1[:].rearrange("p cc i -> p (cc i)"), ALU.mult,
                 r=[B_QK, e1], w=[qkT])
            for cc in range(8):
                k.tr(B_TR[0:64, cc * 64:(cc + 1) * 64], A0[:, cc, :], I64, r=[A0, c["ident_f"]], w=[B_TR])
            k.copy("act", AT0[:].rearrange("p cc i -> p (cc i)"), B_TR[0:64, :], r=[B_TR], w=[AT0])
            for src, dst in ((k_, ktok), (v_, vtok), (zs[hh], ztok)):
                for half in range(2):
                    for c4 in range(4):
                        cc = half * 4 + c4
                        k.tr(B_TR[0:64, c4 * 128:(c4 + 1) * 128], src[:, cc * 64:(cc + 1) * 64], c["ident_f"][:],
                             r=[src, c["ident_f"]], w=[B_TR])
                    k.copy("act", dst[:, half * 4:half * 4 + 4, :].rearrange("p cc d -> p (cc d)"), B_TR[0:64, :],
                           r=[B_TR], w=[dst])
            for cc in range(8):
                k.ts("pool", ktail[:, cc, :], ktok[:, cc, :], eGlG[:, gi(cc):gi(cc) + 1], None, ALU.mult, r=[ktok, eGlG], w=[ktail])
            pyT = B_TR
            for grp in range(2):
                c0 = grp * 4
                for c4 in range(4):
                    cc = c0 + c4
                    k.ts("dve", xs[:, c4, 0:128], vtok[:, cc, :], betok[:, hh, cc:cc + 1], None, ALU.mult, r=[vtok, betok], w=[xs])
                    k.ts("dve", xs[:, c4, 128:256], ktok[:, cc, :], betok[:, hh, cc:cc + 1], eG[:, gi(cc):gi(cc) + 1], ALU.mult,
                         ALU.mult, r=[ktok, betok, eG], w=[xs])
                Mcur = T(A0.t[:, c0:c0 + 4, :], A0.b)
                Ncur = T(AT0.t[:, c0:c0 + 4, :], AT0.b)
                for lev in range(6):
                    if lev > 0:
                        Mn, Nn = Mq[lev % 2], Nq[lev % 2]
                        for c4 in range(4):
                            k.mm(B_MN[0:64, c4 * 64:(c4 + 1) * 64], Ncur[:, c4, :], Mcur[:, c4, :], start=True, stop=True,
                                 r=[Ncur, Mcur], w=[B_MN])
                            k.mm(B_MN[0:64, 256 + c4 * 64:256 + (c4 + 1) * 64], Mcur[:, c4, :], Ncur[:, c4, :], start=True,
                                 stop=True, r=[Ncur, Mcur], w=[B_MN])
                        k.copy("act", Mn[:].rearrange("p cc i -> p (cc i)"), B_MN[0:64, 0:256], r=[B_MN], w=[Mn])
                        k.copy("act", Nn[:].rearrange("p cc i -> p (cc i)"), B_MN[0:64, 256:512], r=[B_MN], w=[Nn])
                        Mcur, Ncur = Mn, Nn
                    for c4 in range(4):
                        pb_ = B_XA if c4 < 2 else B_XB
                        k.mm(pb_[0:64, (c4 % 2) * 256:(c4 % 2) * 256 + 256], Ncur[:, c4, :], xs[:, c4, :], start=True, stop=True,
                             r=[Ncur, xs], w=[pb_])
                    op_ = ALU.subtract if lev == 0 else ALU.add
                    k.tt("dve", xs[:, 0:2, :].rearrange("p cc n -> p (cc n)"), xs[:, 0:2, :].rearrange("p cc n -> p (cc n)"),
                         B_XA[0:64, :], op_, r=[xs, B_XA], w=[xs])
                    k.tt("dve", xs[:, 2:4, :].rearrange("p cc n -> p (cc n)"), xs[:, 2:4, :].rearrange("p cc n -> p (cc n)"),
                         B_XB[0:64, :], op_, r=[xs, B_XB], w=[xs])
                for c4 in range(4):
                    k.tr(B_MN[:, c4 * 64:(c4 + 1) * 64], xs[:, c4, 128:256], I64, r=[xs, c["ident_f"]], w=[B_MN])
                k.copy("act", WT[:].rearrange("p cc i -> p (cc i)"), B_MN[:, 0:256], r=[B_MN], w=[WT])
                for c4 in range(4):
                    cc = c0 + c4
                    cs = slice(cc * 64, (cc + 1) * 64)
                    vn, ot, yt = vnew[cc % 2], otok[cc % 2], ytok[cc % 2]
                    k.mm(B_SC[0:64, 0:128], WT[:, c4, :], S_[:], start=True, stop=True, r=[WT, S_], w=[B_SC])
                    k.tt("dve", vn[:], xs[:, c4, 0:128], B_SC[0:64, 0:128], ALU.subtract, r=[xs, B_SC], w=[vn])
                    k.mm(B_SC[0:64, 128:256], qd[:, cs], S_[:], start=True, stop=False, r=[qd, S_], w=[B_SC])
                    k.mm(B_SC[0:64, 128:256], qkT[:, cc, :], vn[:], start=False, stop=True, r=[qkT, vn], w=[B_SC])
                    k.mm(B_SC[:, 256:384], ktail[:, cc, :], vn[:], start=True, stop=True, r=[ktail, vn], w=[B_SC])
                    k.stt("dve", S_[:], S_[:], eGl128[:, gi(cc):gi(cc) + 1], B_SC[:, 256:384], ALU.mult, ALU.add,
                          r=[S_, eGl128, B_SC], w=[S_])
                    k.copy("dve", ot[:], B_SC[0:64, 128:256], r=[B_SC], w=[ot])
                    k.op("act", lambda: nc.scalar.memzero(ss[:]), (), [ss])
                    k.act(junk[:], ot[:], AF.Square, accum_out=ss[:, 0:1], r=[ot, ss], w=[junk, ss])
                    k.ts("dve", ss[:], ss[:], 1.0 / 128, RMS_EPS, ALU.mult, ALU.add, r=[ss], w=[ss])
                    k.act(ss[:], ss[:], AF.Sqrt, r=[ss], w=[ss])
                    k.op("dve", lambda: nc.vector.reciprocal(out=ss[:], in_=ss[:]), r=[ss], w=[ss])
                    k.stt("dve", yt[:], ot[:], ss[:, 0:1], ngB[:], ALU.mult, ALU.mult, r=[ot, ss, ngB], w=[yt])
                    k.tt("pool", yt[:], yt[:], ztok[:, cc, :], ALU.mult, r=[yt, ztok], w=[yt])
                    k.tr(pyT[:, 256 + c4 * 64:256 + (c4 + 1) * 64], yt[:], I64, r=[yt, c["ident_f"]], w=[pyT])
                y_ = yb[hh]
                k.copy("act", y_[:, c0 * 64:(c0 + 4) * 64], pyT[:, 256:512], r=[pyT], w=[y_])
            k.dma("sp", y_out[hh * 128:(hh + 1) * 128, t * 512:(t + 1) * 512], yb[hh][:], r=[yb[hh]], w=[outb])
    barrier(k)
    k.st = k_st
    st.close()
    return outb


def phase_dn3(k, c, P, io, modT, hp, y_out, ntiles=16):
    nc = k.nc
    st = ExitStack()
    k_st, k.st = k.st, st
    scp1 = k.sb([128, 8], F32)
    k.ts("dve", scp1[:], modT[:, 1, :], 1.0, None, ALU.add, r=[modT], w=[scp1])
    sh1 = modT[:, 0, :]
    w_dn = io["w_dn"]
    wq = k.sb([128, 8, 4, 2, 128], BF16)
    for si in range(4):
        k.dma("pool", wq[:, :, si, :, :].rearrange("p kk hh n -> p kk (hh n)"),
              w_dn[:, si * 256:si * 256 + 256].rearrange("(kk p) n -> p kk n", p=128), w=[wq])
    wab = k.sb([128, 8, 4], BF16)
    k.dma("pool", wab[:], w_dn[:, 1024:1028].rearrange("(kk p) n -> p kk n", p=128), w=[wab])
    cw = k.sb([128, 3, 2, 4], F32)
    k.dma("sp", cw[:], io["convw"], w=[cw])
    I64 = c["ident_f"][0:64, 0:64]
    ones64 = c["ones_f"][0:64, 0:64]
    ones64_128 = c["ones_f"][0:64, :]
    dtbB = k.sb([64, 2], F32)
    nAB = k.sb([64, 2], F32)
    k.dma("sp", dtbB[:], bass.AP(io["dtb"].tensor, io["dtb"].offset, [[0, 64], [1, 2]]), w=[dtbB])
    k.dma("sp", nAB[:], bass.AP(io["alog"].tensor, io["alog"].offset, [[0, 64], [1, 2]]), w=[nAB])
    k.act(nAB[:], nAB[:], AF.Exp, r=[nAB], w=[nAB])
    k.ts("dve", nAB[:], nAB[:], -1.0, None, ALU.mult, r=[nAB], w=[nAB])
    ngB = k.sb([64, 128], F32)
    k.dma("sp", ngB[:], bass.AP(io["normg"].tensor, io["normg"].offset, [[0, 64], [1, 128]]), w=[ngB])
    UT = k.sb([64, 64], F32)
    k.memset("pool", UT[:], 1.0, w=[UT])
    k.op("pool", lambda: nc.gpsimd.affine_select(out=UT[:], in_=UT[:], pattern=[[1, 64]], compare_op=ALU.is_ge,
                                                  fill=0.0, base=0, channel_multiplier=-1), r=[UT], w=[UT])
    TRIU = k.sb([64, 8, 64], F32)
    TRILS = k.sb([64, 8, 64], F32)
    k.memset("pool", TRIU[:], 1.0, w=[TRIU])
    k.op("pool", lambda: nc.gpsimd.affine_select(out=TRIU[:], in_=TRIU[:], pattern=[[0, 8], [1, 64]], compare_op=ALU.is_ge,
                                                  fill=0.0, base=0, channel_multiplier=-1), r=[TRIU], w=[TRIU])
    k.memset("pool", TRILS[:], 1.0, w=[TRILS])
    k.op("pool", lambda: nc.gpsimd.affine_select(out=TRILS[:], in_=TRILS[:], pattern=[[0, 8], [-1, 64]], compare_op=ALU.is_gt,
                                                  fill=0.0, base=0, channel_multiplier=1), r=[TRILS], w=[TRILS])
    xbufs = [k.sb([128, 4, D], F32) for _ in range(2)]
    hT = k.sb([128, 8, 512], BF16)
    pre = [[k.sb([128, 515], F32) for _ in range(2)] for _ in range(3)]
    for s_ in range(3):
        for hh in range(2):
            k.memset("pool", pre[s_][hh][:, 0:3], 0.0, w=[pre[s_][hh]])
    qkv = [[k.sb([128, 512], F32) for _ in range(2)] for _ in range(3)]
    zs = [k.sb([128, 512], F32) for _ in range(2)]
    acc = k.sb([128, 512], F32)
    sq = k.sb([128, 512], F32)
    rn = k.sb([128, 512], F32)
    Sst = [k.sb([128, 128], F32) for _ in range(2)]
    for hh in range(2):
        k.memset("pool", Sst[hh][:], 0.0, w=[Sst[hh]])
    gtok = k.sb([64, 2, 8], F32)
    betok = k.sb([64, 2, 8], F32)
    Gs = k.sb([64, 16], F32)
    eG = k.sb([64, 16], F32)
    eGlG = k.sb([64, 16], F32)
    eGl128 = k.sb([128, 16], F32)
    rhsG = k.sb([64, 8, 64], F32)
    eGB = k.sb([128, 512], F32)
    dd = k.sb([64, 8, 64], F32)
    e1 = k.sb([64, 8, 64], F32)
    e2 = k.sb([64, 8, 64], F32)
    A0s = [k.sb([64, 8, 64], F32) for _ in range(2)]
    A0bs = [k.sb([64, 8, 64], BF16) for _ in range(2)]
    AT0s = [k.sb([64, 8, 64], BF16) for _ in range(2)]
    Mqs = [[k.sb([64, 2, 64], BF16) for _ in range(2)] for _ in range(2)]
    Nqs = [[k.sb([64, 2, 64], BF16) for _ in range(2)] for _ in range(2)]
    xsbs = [[k.sb([64, 2, 256], BF16) for _ in range(2)] for _ in range(2)]
    qkTs = [k.sb([64, 8, 64], F32) for _ in range(2)]
    ktoks = [k.sb([64, 8, 128], F32) for _ in range(2)]
    vtoks = [k.sb([64, 8, 128], F32) for _ in range(2)]
    ztoks = [k.sb([64, 8, 128], F32) for _ in range(2)]
    ktails = [k.sb([64, 8, 128], F32) for _ in range(2)]
    qds = [k.sb([128, 512], F32) for _ in range(2)]
    xss = [[k.sb([64, 2, 256], F32) for _ in range(2)] for _ in range(2)]
    WTs = [[k.sb([128, 2, 64], F32) for _ in range(2)] for _ in range(2)]
    vnews = [[k.sb([64, 128], F32) for _ in range(2)] for _ in range(2)]
    otoks = [[k.sb([64, 128], F32) for _ in range(2)] for _ in range(2)]
    ytoks = [[k.sb([64, 128], F32) for _ in range(2)] for _ in range(2)]
    junks = [k.sb([64, 128], F32) for _ in range(2)]
    sss = [k.sb([64, 1], F32) for _ in range(2)]
    yb = [k.sb([128, 512], BF16) for _ in range(2)]
    outb = Buf()
    B_GB, B_KK, B_QK, B_TR, B_XA, B_XB = P["c"][0], P["c"][1], P["q"][0], P["q"][1], P["m"], P["t"]
    BX, BMN, BSC, BY = [P["c"][0], P["c"][1]], [P["q"][0], P["q"][1]], [P["tr"][0], P["tr"][1]], [P["m"], P["t"]]
    for t in range(ntiles):
        xt = xbufs[t % 2]
        for tb in range(4):
            k.dma("sp", xt[:, tb, :], io["xrow_true"](t * 4 + tb), r=[io.get("xb_true", io["xb"])], w=[xt])
        transpose_modulate(k, c, xt, hT, [P["m"], P["t"]], scp1, sh1, 4)
        pab = B_XA
        for cc in range(8):
            for kk in range(8):
                k.mm(pab[0:64, cc * 4:cc * 4 + 4], hT[:, kk, cc * 64:(cc + 1) * 64], wab[:, kk, :], start=(kk == 0),
                     stop=(kk == 7), inc=(kk == 7), r=[hT, wab], w=[pab])
        pabv = pab[0:64, 0:32].rearrange("p (cc f) -> p cc f", f=4)
        for hh in range(2):
            k.act(gtok[:, hh, :], pabv[:, :, hh], AF.Exp, bias=dtbB[:, hh:hh + 1], r=[pab, dtbB], w=[gtok])
            k.act(betok[:, hh, :], pabv[:, :, 2 + hh], AF.Sigmoid, r=[pab], w=[betok])
        k.ts("dve", gtok[:], gtok[:], 1.0, None, ALU.add, r=[gtok], w=[gtok])
        k.act(gtok[:], gtok[:], AF.Ln, r=[gtok], w=[gtok])
        for hh in range(2):
            k.ts("dve", gtok[:, hh, :], gtok[:, hh, :], nAB[:, hh:hh + 1], None, ALU.mult, r=[gtok, nAB], w=[gtok])
        gflat = gtok[:].rearrange("p h cc -> p (h cc)")
        pG = B_XB
        k.mm(pG[0:64, 0:16], UT[:], gflat, start=True, stop=True, r=[UT, gtok], w=[pG])
        k.mm(pG[0:64, 16:32], ones64, gflat, start=True, stop=True, r=[c["ones_f"], gtok], w=[pG])
        k.mm(pG[:, 32:48], ones64_128, gflat, start=True, stop=True, r=[c["ones_f"], gtok], w=[pG])
        k.copy("dve", Gs[:], pG[0:64, 0:16], r=[pG], w=[Gs])
        k.tt("dve", eGlG[:], pG[0:64, 16:32], Gs[:], ALU.subtract, r=[pG, Gs], w=[eGlG])
        k.copy("dve", eGl128[:], pG[:, 32:48], r=[pG], w=[eGl128])
        k.act(eG[:], Gs[:], AF.Exp, r=[Gs], w=[eG])
        k.act(eGlG[:], eGlG[:], AF.Exp, r=[eGlG], w=[eGlG])
        k.act(eGl128[:], eGl128[:], AF.Exp, r=[eGl128], w=[eGl128])
        for hh in range(2):
            for s_ in range(4):
                pp = P["m"] if s_ % 2 == 0 else P["t"]
                for kk in range(8):
                    k.mm(pp[:, :], wq[:, kk, s_, hh, :], hT[:, kk, :], start=(kk == 0), stop=(kk == 7), inc=(kk == 7), r=[wq, hT], w=[pp])
                if s_ == 3:
                    k.act(zs[hh][:], pp[:, :], AF.Silu, r=[pp], w=[zs[hh]])
                    continue
                pr = pre[s_][hh]
                k.copy("act", pr[:, 3:515], pp[:, :], r=[pp], w=[pr])
                k.ts("dve", acc[:], pr[:, 0:512], cw[:, s_, hh, 0:1], None, ALU.mult, r=[pr, cw], w=[acc])
                for j in range(1, 4):
                    k.stt("dve", acc[:], pr[:, j:j + 512], cw[:, s_, hh, j:j + 1], acc[:], ALU.mult, ALU.add,
                          r=[pr, cw, acc], w=[acc])
                k.copy("pool", pr[:, 0:3], pr[:, 512:515], r=[pr], w=[pr])
                dst = qkv[s_][hh]
                if s_ == 2:
                    k.act(dst[:], acc[:], AF.Silu, r=[acc], w=[dst])
                    continue
                k.act(acc[:], acc[:], AF.Silu, r=[acc], w=[acc])
                k.tt("dve", sq[:], acc[:], acc[:], ALU.mult, r=[acc], w=[sq])
                k.mm(pp[:, :], c["ones_f"][:], sq[:], start=True, stop=True, r=[c["ones_f"], sq], w=[pp])
                k.ts("dve", rn[:], pp[:, :], RMS_EPS, None, ALU.add, r=[pp], w=[rn])
                k.act(rn[:], rn[:], AF.Sqrt, r=[rn], w=[rn])
                k.op("dve", lambda: nc.vector.reciprocal(out=rn[:], in_=rn[:]), r=[rn], w=[rn])
                if s_ == 0:
                    k.stt("dve", dst[:], acc[:], 128 ** -0.5, rn[:], ALU.mult, ALU.mult, r=[acc, rn], w=[dst])
                else:
                    k.tt("dve", dst[:], acc[:], rn[:], ALU.mult, r=[acc, rn], w=[dst])
        for hh in range(2):
            q_, k_, v_, S_ = qkv[0][hh], qkv[1][hh], qkv[2][hh], Sst[hh]
            qd, qkT, ktok, vtok, ztok, ktail = qds[hh], qkTs[hh], ktoks[hh], vtoks[hh], ztoks[hh], ktails[hh]
            gi = lambda cc: hh * 8 + cc
            for cc in range(8):
                k.ts("pool", rhsG[:, cc, :], UT[:], gtok[:, hh, cc:cc + 1], None, ALU.mult, r=[UT, gtok], w=[rhsG])
            k.mm(B_GB[:, :], ones64_128, rhsG[:].rearrange("p cc i -> p (cc i)"), start=True, stop=True,
                 r=[c["ones_f"], rhsG], w=[B_GB])
            k.act(eGB[:], B_GB[:, :], AF.Exp, r=[B_GB], w=[eGB])
            k.tt("pool", qd[:], q_[:], eGB[:], ALU.mult, r=[q_, eGB], w=[qd])
            for cc in range(8):
                k.ts("dve", dd[:, cc, :], B_GB[0:64, cc * 64:(cc + 1) * 64], Gs[:, gi(cc):gi(cc) + 1], None, ALU.subtract,
                     r=[B_GB, Gs], w=[dd])
            k.ts("dve", e1[:], dd[:], 0.0, None, ALU.min, r=[dd], w=[e1])
            k.ts("dve", e2[:], dd[:], 0.0, None, ALU.max, r=[dd], w=[e2])
            k.act(e1[:], e1[:], AF.Exp, r=[e1], w=[e1])
            k.act(e2[:], e2[:], AF.Exp, scale=-1.0, r=[e2], w=[e2])
            k.tt("pool", e1[:], e1[:], TRIU[:], ALU.mult, r=[e1, TRIU], w=[e1])
            k.tt("pool", e2[:], e2[:], TRILS[:], ALU.mult, r=[e2, TRILS], w=[e2])
            for cc in range(8):
                cs = slice(cc * 64, (cc + 1) * 64)
                k.mm(B_KK[0:64, cs], k_[:, cs], k_[:, cs], start=True, stop=True, r=[k_], w=[B_KK])
                k.mm(B_QK[0:64, cs], k_[:, cs], q_[:, cs], start=True, stop=True, r=[k_, q_], w=[B_QK])
            A0, AT0 = A0s[hh], AT0s[hh]
            for cc in range(8):
                k.stt("dve", A0[:, cc, :], B_KK[0:64, cc * 64:(cc + 1) * 64], betok[:, hh, cc:cc + 1], e2[:, cc, :],
                      ALU.mult, ALU.mult, r=[B_KK, betok, e2], w=[A0])
            k.tt("dve", qkT[:].rearrange("p cc i -> p (cc i)"), B_QK[0:64, :], e1[:].rearrange("p cc i -> p (cc i)"), ALU.mult,
                 r=[B_QK, e1], w=[qkT])
            for cc in range(8):
                k.tr(B_TR[0:64, cc * 64:(cc + 1) * 64], A0[:, cc, :], I64, r=[A0, c["ident_f"]], w=[B_TR])
            k.copy("act", AT0[:].rearrange("p cc i -> p (cc i)"), B_TR[0:64, :], r=[B_TR], w=[AT0])
            k.copy("pool", A0bs[hh][:], A0[:], r=[A0], w=[A0bs[hh]])
            for src, dst in ((k_, ktok), (v_, vtok), (zs[hh], ztok)):
                for half in range(2):
                    for c4 in range(4):
                        cc = half * 4 + c4
                        k.tr(B_TR[0:64, c4 * 128:(c4 + 1) * 128], src[:, cc * 64:(cc + 1) * 64], c["ident_f"][:],
                             r=[src, c["ident_f"]], w=[B_TR])
                    k.copy("act", dst[:, half * 4:half * 4 + 4, :].rearrange("p cc d -> p (cc d)"), B_TR[0:64, :],
                           r=[B_TR], w=[dst])
            for cc in range(8):
                k.ts("pool", ktail[:, cc, :], ktok[:, cc, :], eGlG[:, gi(cc):gi(cc) + 1], None, ALU.mult, r=[ktok, eGlG], w=[ktail])

        def solve(hh, g2):
            A0, AT0, ktok, vtok = A0bs[hh], AT0s[hh], ktoks[hh], vtoks[hh]
            xs, WT = xss[hh][g2 % 2], WTs[hh][g2 % 2]
            xb = xsbs[hh][g2 % 2]
            bx, bmn = BX[hh], BMN[hh]
            c0 = g2 * 2
            gi = lambda cc: hh * 8 + cc
            for c2 in range(2):
                cc = c0 + c2
                k.ts("dve", xs[:, c2, 0:128], vtok[:, cc, :], betok[:, hh, cc:cc + 1], None, ALU.mult, r=[vtok, betok], w=[xs])
                k.ts("dve", xs[:, c2, 128:256], ktok[:, cc, :], betok[:, hh, cc:cc + 1], eG[:, gi(cc):gi(cc) + 1], ALU.mult,
                     ALU.mult, r=[ktok, betok, eG], w=[xs])
            Mcur = T(A0.t[:, c0:c0 + 2, :], A0.b)
            Ncur = T(AT0.t[:, c0:c0 + 2, :], AT0.b)
            xf = xs[:].rearrange("p cc n -> p (cc n)")
            k.copy("pool", xb[:], xs[:], r=[xs], w=[xb])
            for lev in range(6):
                if lev > 0:
                    Mn, Nn = Mqs[hh][lev % 2], Nqs[hh][lev % 2]
                    for c2 in range(2):
                        k.mm(bmn[0:64, c2 * 64:(c2 + 1) * 64], Ncur[:, c2, :], Mcur[:, c2, :], start=True, stop=True,
                             r=[Ncur, Mcur], w=[bmn])
                        k.mm(bmn[0:64, 128 + c2 * 64:128 + (c2 + 1) * 64], Mcur[:, c2, :], Ncur[:, c2, :], start=True,
                             stop=True, r=[Ncur, Mcur], w=[bmn])
                    k.copy("act", Mn[:].rearrange("p cc i -> p (cc i)"), bmn[0:64, 0:128], r=[bmn], w=[Mn])
                    k.copy("act", Nn[:].rearrange("p cc i -> p (cc i)"), bmn[0:64, 128:256], r=[bmn], w=[Nn])
                    Mcur, Ncur = Mn, Nn
                for c2 in range(2):
                    k.mm(bx[0:64, c2 * 256:(c2 + 1) * 256], Ncur[:, c2, :], xb[:, c2, :], start=True, stop=True,
                         r=[Ncur, xb], w=[bx])
                k.tt("dve", xf, xf, bx[0:64, :], ALU.subtract if lev == 0 else ALU.add, r=[xs, bx], w=[xs])
                if lev < 5:
                    k.copy("pool", xb[:], xs[:], r=[xs], w=[xb])
                yield
            for c2 in range(2):
                k.tr(bmn[:, 256 + c2 * 64:256 + (c2 + 1) * 64], xs[:, c2, 128:256], I64, r=[xs, c["ident_f"]], w=[bmn])
            k.copy("act", WT[:].rearrange("p cc i -> p (cc i)"), bmn[:, 256:384], r=[bmn], w=[WT])
            yield

        def scan(hh, g2):
            S_, qd, qkT, ktail, ztok = Sst[hh], qds[hh], qkTs[hh], ktails[hh], ztoks[hh]
            xs, WT = xss[hh][g2 % 2], WTs[hh][g2 % 2]
            bsc, by = BSC[hh], BY[hh]
            ss, junk = sss[hh], junks[hh]
            gi = lambda cc: hh * 8 + cc
            for c2 in range(2):
                cc = g2 * 2 + c2
                cs = slice(cc * 64, (cc + 1) * 64)
                vn, ot, yt = vnews[hh][cc % 2], otoks[hh][cc % 2], ytoks[hh][cc % 2]
                k.mm(bsc[0:64, 0:128], WT[:, c2, :], S_[:], start=True, stop=True, r=[WT, S_], w=[bsc])
                k.tt("dve", vn[:], xs[:, c2, 0:128], bsc[0:64, 0:128], ALU.subtract, r=[xs, bsc], w=[vn])
                k.mm(bsc[0:64, 128:256], qd[:, cs], S_[:], start=True, stop=False, r=[qd, S_], w=[bsc])
                k.mm(bsc[0:64, 128:256], qkT[:, cc, :], vn[:], start=False, stop=True, r=[qkT, vn], w=[bsc])
                k.mm(bsc[:, 256:384], ktail[:, cc, :], vn[:], start=True, stop=True, r=[ktail, vn], w=[bsc])
                k.stt("dve", S_[:], S_[:], eGl128[:, gi(cc):gi(cc) + 1], bsc[:, 256:384], ALU.mult, ALU.add,
                      r=[S_, eGl128, bsc], w=[S_])
                k.copy("dve", ot[:], bsc[0:64, 128:256], r=[bsc], w=[ot])
                k.act(junk[:], ot[:], AF.Square, accum_out=ss[:, 0:1], r=[ot], w=[junk, ss])
                k.ts("dve", ss[:], ss[:], 1.0 / 128, RMS_EPS, ALU.mult, ALU.add, r=[ss], w=[ss])
                k.act(ss[:], ss[:], AF.Sqrt, r=[ss], w=[ss])
                k.op("dve", lambda: nc.vector.reciprocal(out=ss[:], in_=ss[:]), r=[ss], w=[ss])
                k.stt("dve", yt[:], ot[:], ss[:, 0:1], ngB[:], ALU.mult, ALU.mult, r=[ot, ss, ngB], w=[yt])
                k.tt("pool", yt[:], yt[:], ztok[:, cc, :], ALU.mult, r=[yt, ztok], w=[yt])
                k.tr(by[:, cc * 64:(cc + 1) * 64], yt[:], I64, r=[yt, c["ident_f"]], w=[by])
                yield
            if g2 == 3:
                k.copy("act", yb[hh][:], by[:, :], r=[by], w=[yb[hh]])
                k.dma("sp", y_out[hh * 128:(hh + 1) * 128, t * 512:(t + 1) * 512], yb[hh][:], r=[yb[hh]], w=[outb])

        def head_flow(hh):
            for _ in solve(hh, 0):
                yield
            for g2 in range(4):
                ga = scan(hh, g2)
                gb = solve(hh, g2 + 1) if g2 + 1 < 4 else iter(())
                alive = [True, True]
                gens = [gb, ga]
                while any(alive):
                    for gi_ in range(2):
                        if alive[gi_]:
                            try:
                                next(gens[gi_])
                            except StopIteration:
                                alive[gi_] = False
                    yield

        flows = [head_flow(0), head_flow(1)]
        alive = [True, True]
        while any(alive):
            for hh in range(2):
                if alive[hh]:
                    try:
                        next(flows[hh])
                    except StopIteration:
                        alive[hh] = False
    barrier(k)
    k.st = k_st
    st.close()
    return outb


def phase_dn4(k, c, P, io, modT, hp, y_out, ntiles=16):
    nc = k.nc
    st = ExitStack()
    k_st, k.st = k.st, st
    scp1 = k.sb([128, 8], F32)
    k.ts("dve", scp1[:], modT[:, 1, :], 1.0, None, ALU.add, r=[modT], w=[scp1])
    sh1 = modT[:, 0, :]
    w_dn = io["w_dn"]
    wq = k.sb([128, 8, 4, 2, 128], BF16)
    for si in range(4):
        k.dma("pool", wq[:, :, si, :, :].rearrange("p kk hh n -> p kk (hh n)"),
              w_dn[:, si * 256:si * 256 + 256].rearrange("(kk p) n -> p kk n", p=128), w=[wq])
    wab = k.sb([128, 8, 4], BF16)
    k.dma("pool", wab[:], w_dn[:, 1024:1028].rearrange("(kk p) n -> p kk n", p=128), w=[wab])
    cw = k.sb([128, 3, 2, 4], F32)
    k.dma("sp", cw[:], io["convw"], w=[cw])
    I64 = c["ident_f"][0:64, 0:64]
    ones64 = c["ones_f"][0:64, 0:64]
    ones64_128 = c["ones_f"][0:64, :]
    dtbB = k.sb([128, 2], F32)
    nAB = k.sb([128, 2], F32)
    k.dma("sp", dtbB[:], bass.AP(io["dtb"].tensor, io["dtb"].offset, [[0, 128], [1, 2]]), w=[dtbB])
    k.dma("sp", nAB[:], bass.AP(io["alog"].tensor, io["alog"].offset, [[0, 128], [1, 2]]), w=[nAB])
    k.act(nAB[:], nAB[:], AF.Exp, r=[nAB], w=[nAB])
    k.ts("dve", nAB[:], nAB[:], -1.0, None, ALU.mult, r=[nAB], w=[nAB])
    ngB = k.sb([128, 128], F32)
    k.dma("sp", ngB[:], bass.AP(io["normg"].tensor, io["normg"].offset, [[0, 128], [1, 128]]), w=[ngB])
    I128 = c["ident_f"][:]
    UTbd = k.sb([128, 128], F32)
    k.memset("pool", UTbd[:], 1.0, w=[UTbd])
    k.op("pool", lambda: nc.gpsimd.affine_select(out=UTbd[:], in_=UTbd[:], pattern=[[1, 128]], compare_op=ALU.is_ge,
                                                  fill=0.0, base=0, channel_multiplier=-1), r=[UTbd], w=[UTbd])
    k.memset("pool", UTbd[0:64, 64:128], 0.0, w=[UTbd])
    ONESbd = k.sb([128, 128], F32)
    k.memset("pool", ONESbd[:], 0.0, w=[ONESbd])
    k.memset("pool", ONESbd[0:64, 0:64], 1.0, w=[ONESbd])
    k.memset("pool", ONESbd[64:128, 64:128], 1.0, w=[ONESbd])
    L0 = k.sb([128, 128], F32)
    L1 = k.sb([128, 128], F32)
    k.memset("pool", L0[:], 0.0, w=[L0])
    k.memset("pool", L0[0:64, :], 1.0, w=[L0])
    k.memset("pool", L1[:], 0.0, w=[L1])
    k.memset("pool", L1[64:128, :], 1.0, w=[L1])
    TRIU = k.sb([128, 4, 128], F32)
    for p_ in range(4):
        k.copy("pool", TRIU[:, p_, :], UTbd[:], r=[UTbd], w=[TRIU])
    TRILS = k.sb([128, 4, 128], F32)
    k.memset("pool", TRILS[:], 1.0, w=[TRILS])
    k.op("pool", lambda: nc.gpsimd.affine_select(out=TRILS[:], in_=TRILS[:], pattern=[[0, 4], [-1, 128]], compare_op=ALU.is_gt,
                                                  fill=0.0, base=0, channel_multiplier=1), r=[TRILS], w=[TRILS])
    k.memset("pool", TRILS[64:128, :, 0:64], 0.0, w=[TRILS])
    xbufs = [k.sb([128, 4, D], F32) for _ in range(2)]
    hT = k.sb([128, 8, 512], BF16)
    pre = [[k.sb([128, 515], F32) for _ in range(2)] for _ in range(3)]
    for s_ in range(3):
        for hh in range(2):
            k.memset("pool", pre[s_][hh][:, 0:3], 0.0, w=[pre[s_][hh]])
    qkv = [[k.sb([128, 512], F32) for _ in range(2)] for _ in range(3)]
    zs = [k.sb([128, 512], F32) for _ in range(2)]
    accs6 = [k.sb([128, 512], F32) for _ in range(6)]
    rns6 = [k.sb([128, 512], F32) for _ in range(6)]
    Sst = [k.sb([128, 128], F32) for _ in range(2)]
    for hh in range(2):
        k.memset("pool", Sst[hh][:], 0.0, w=[Sst[hh]])
    gtok = k.sb([128, 2, 4], F32)
    betok = k.sb([128, 2, 4], F32)
    Gs = k.sb([128, 8], F32)
    eG = k.sb([128, 8], F32)
    eGlG = k.sb([128, 8], F32)
    eGlA = k.sb([128, 8], F32)
    eGlB = k.sb([128, 8], F32)
    rhsG = k.sb([128, 4, 128], F32)
    eGB = k.sb([128, 512], F32)
    dd = k.sb([128, 4, 128], F32)
    e1 = k.sb([128, 4, 128], F32)
    e2 = k.sb([128, 4, 128], F32)
    A0s = [k.sb([128, 4, 128], F32) for _ in range(2)]
    A0bs = [k.sb([128, 4, 128], BF16) for _ in range(2)]
    AT0s = [k.sb([128, 4, 128], BF16) for _ in range(2)]
    Mqs = [[[k.sb([128, 128], BF16) for _ in range(2)] for _ in range(2)] for _ in range(2)]
    Nqs = [[[k.sb([128, 128], BF16) for _ in range(2)] for _ in range(2)] for _ in range(2)]
    qkTs = [k.sb([128, 4, 128], F32) for _ in range(2)]
    ktoks = [k.sb([128, 4, 128], F32) for _ in range(2)]
    vtoks = [k.sb([128, 4, 128], F32) for _ in range(2)]
    ztoks = [k.sb([128, 4, 128], F32) for _ in range(2)]
    ktails = [k.sb([128, 4, 128], F32) for _ in range(2)]
    qds = [k.sb([128, 512], F32) for _ in range(2)]
    xss = [[k.sb([128, 256], F32) for _ in range(4)] for _ in range(2)]
    xsbs = [[k.sb([128, 256], BF16) for _ in range(4)] for _ in range(2)]
    WTs = [[k.sb([128, 128], F32) for _ in range(4)] for _ in range(2)]
    vns = [k.sb([128, 128], F32) for _ in range(2)]
    for hh in range(2):
        k.memset("pool", vns[hh][:], 0.0, w=[vns[hh]])
    ots = [[k.sb([128, 128], F32) for _ in range(2)] for _ in range(2)]
    yts = [[k.sb([128, 128], F32) for _ in range(2)] for _ in range(2)]
    junks = [k.sb([128, 128], F32) for _ in range(2)]
    sss = [k.sb([128, 1], F32) for _ in range(2)]
    yb = [k.sb([128, 512], BF16) for _ in range(2)]
    outb = Buf()
    SB = [[P["c"][0], P["c"][1]], [P["q"][0], P["q"][1]]]
    BSC, BY = [P["tr"][0], P["tr"][1]], [P["m"], P["t"]]
    for t in range(ntiles):
        xt = xbufs[t % 2]
        for tb in range(4):
            k.dma("sp", xt[:, tb, :], io["xrow_true"](t * 4 + tb), r=[io.get("xb_true", io["xb"])], w=[xt])
        transpose_modulate(k, c, xt, hT, [P["m"], P["t"]], scp1, sh1, 4)
        pab = P["m"]
        for pr_ in range(4):
            for kk in range(8):
                k.mm(pab[:, pr_ * 4:pr_ * 4 + 4], hT[:, kk, pr_ * 128:(pr_ + 1) * 128], wab[:, kk, :], start=(kk == 0),
                     stop=(kk == 7), inc=(kk == 7), r=[hT, wab], w=[pab])
        pabv = pab[:, 0:16].rearrange("p (cc f) -> p cc f", f=4)
        for hh in range(2):
            k.act(gtok[:, hh, :], pabv[:, :, hh], AF.Exp, bias=dtbB[:, hh:hh + 1], r=[pab, dtbB], w=[gtok])
            k.act(betok[:, hh, :], pabv[:, :, 2 + hh], AF.Sigmoid, r=[pab], w=[betok])
        k.ts("dve", gtok[:], gtok[:], 1.0, None, ALU.add, r=[gtok], w=[gtok])
        k.act(gtok[:], gtok[:], AF.Ln, r=[gtok], w=[gtok])
        for hh in range(2):
            k.ts("dve", gtok[:, hh, :], gtok[:, hh, :], nAB[:, hh:hh + 1], None, ALU.mult, r=[gtok, nAB], w=[gtok])
        gflat = gtok[:].rearrange("p h cc -> p (h cc)")
        pG = P["t"]
        k.mm(pG[:, 0:8], UTbd[:], gflat, start=True, stop=True, r=[UTbd, gtok], w=[pG])
        k.mm(pG[:, 8:16], ONESbd[:], gflat, start=True, stop=True, r=[ONESbd, gtok], w=[pG])
        k.mm(pG[:, 16:24], L0[:], gflat, start=True, stop=True, r=[L0, gtok], w=[pG])
        k.mm(pG[:, 24:32], L1[:], gflat, start=True, stop=True, r=[L1, gtok], w=[pG])
        k.copy("dve", Gs[:], pG[:, 0:8], r=[pG], w=[Gs])
        k.tt("dve", eGlG[:], pG[:, 8:16], Gs[:], ALU.subtract, r=[pG, Gs], w=[eGlG])
        k.copy("dve", eGlA[:], pG[:, 16:24], r=[pG], w=[eGlA])
        k.copy("dve", eGlB[:], pG[:, 24:32], r=[pG], w=[eGlB])
        k.act(eG[:], Gs[:], AF.Exp, r=[Gs], w=[eG])
        k.act(eGlG[:], eGlG[:], AF.Exp, r=[eGlG], w=[eGlG])
        k.act(eGlA[:], eGlA[:], AF.Exp, r=[eGlA], w=[eGlA])
        k.act(eGlB[:], eGlB[:], AF.Exp, r=[eGlB], w=[eGlB])
        def section(hh, s_, pp):
            for kk in range(8):
                k.mm(pp[:, :], wq[:, kk, s_, hh, :], hT[:, kk, :], start=(kk == 0), stop=(kk == 7), inc=(kk == 7), r=[wq, hT], w=[pp])
            yield
            if s_ == 3:
                k.act(zs[hh][:], pp[:, :], AF.Silu, r=[pp], w=[zs[hh]])
                return
            pr = pre[s_][hh]
            acc, rn = accs6[hh * 3 + s_], rns6[hh * 3 + s_]
            dst = qkv[s_][hh]
            k.copy("act", pr[:, 3:515], pp[:, :], r=[pp], w=[pr])
            yield
            k.ts("dve", acc[:], pr[:, 0:512], cw[:, s_, hh, 0:1], None, ALU.mult, r=[pr, cw], w=[acc])
            for j in range(1, 4):
                k.stt("dve", acc[:], pr[:, j:j + 512], cw[:, s_, hh, j:j + 1], acc[:], ALU.mult, ALU.add,
                      r=[pr, cw, acc], w=[acc])
                if j == 2:
                    yield
            k.copy("pool", pr[:, 0:3], pr[:, 512:515], r=[pr], w=[pr])
            yield
            if s_ == 2:
                k.act(dst[:], acc[:], AF.Silu, r=[acc], w=[dst])
                return
            k.act(acc[:], acc[:], AF.Silu, r=[acc], w=[acc])
            yield
            k.tt("dve", dst[:], acc[:], acc[:], ALU.mult, r=[acc], w=[dst])
            k.mm(pp[:, :], c["ones_f"][:], dst[:], start=True, stop=True, r=[c["ones_f"], dst], w=[pp])
            yield
            k.ts("dve", rn[:], pp[:, :], RMS_EPS, None, ALU.add, r=[pp], w=[rn])
            k.act(rn[:], rn[:], AF.Sqrt, r=[rn], w=[rn])
            yield
            k.op("dve", lambda: nc.vector.reciprocal(out=rn[:], in_=rn[:]), r=[rn], w=[rn])
            if s_ == 0:
                k.stt("dve", dst[:], acc[:], 128 ** -0.5, rn[:], ALU.mult, ALU.mult, r=[acc, rn], w=[dst])
            else:
                k.tt("dve", dst[:], acc[:], rn[:], ALU.mult, r=[acc, rn], w=[dst])

        pbanks = [SB[0][0], SB[0][1], SB[1][0], SB[1][1], BSC[0], BSC[1], BY[0], BY[1]]
        sgens = [section(hh, s_, pbanks[hh * 4 + s_]) for s_ in range(4) for hh in range(2)]
        salive = [True] * 8
        while any(salive):
            for gi_ in range(8):
                if salive[gi_]:
                    try:
                        next(sgens[gi_])
                    except StopIteration:
                        salive[gi_] = False
        def front(hh):
            q_, k_, v_ = qkv[0][hh], qkv[1][hh], qkv[2][hh]
            qd, qkT, ktok, vtok, ztok, ktail = qds[hh], qkTs[hh], ktoks[hh], vtoks[hh], ztoks[hh], ktails[hh]
            A0, AT0 = A0s[hh], AT0s[hh]
            B_GB, B_KK, B_QK, B_TR = SB[hh][0], SB[hh][1], BSC[hh], BY[hh]
            gi = lambda p_: hh * 4 + p_
            for p_ in range(4):
                k.ts("dve", rhsG[:, p_, :], UTbd[:], gtok[:, hh, p_:p_ + 1], None, ALU.mult, r=[UTbd, gtok], w=[rhsG])
            k.mm(B_GB[:, :], c["ones_f"][:], rhsG[:].rearrange("p cc i -> p (cc i)"), start=True, stop=True,
                 r=[c["ones_f"], rhsG], w=[B_GB])
            k.act(eGB[:], B_GB[:, :], AF.Exp, r=[B_GB], w=[eGB])
            k.tt("dve", qd[:], q_[:], eGB[:], ALU.mult, r=[q_, eGB], w=[qd])
            yield
            for p_ in range(4):
                k.ts("dve", dd[:, p_, :], B_GB[:, p_ * 128:(p_ + 1) * 128], Gs[:, gi(p_):gi(p_) + 1], None, ALU.subtract,
                     r=[B_GB, Gs], w=[dd])
            k.ts("dve", e1[:], dd[:], 0.0, None, ALU.min, r=[dd], w=[e1])
            k.ts("dve", e2[:], dd[:], 0.0, None, ALU.max, r=[dd], w=[e2])
            k.act(e1[:], e1[:], AF.Exp, r=[e1], w=[e1])
            k.act(e2[:], e2[:], AF.Exp, scale=-1.0, r=[e2], w=[e2])
            k.tt("dve", e1[:], e1[:], TRIU[:], ALU.mult, r=[e1, TRIU], w=[e1])
            k.tt("pool", e2[:], e2[:], TRILS[:], ALU.mult, r=[e2, TRILS], w=[e2])
            yield
            for p_ in range(4):
                cs = slice(p_ * 128, (p_ + 1) * 128)
                k.mm(B_KK[:, cs], k_[:, cs], k_[:, cs], start=True, stop=True, r=[k_], w=[B_KK])
                k.mm(B_QK[:, cs], k_[:, cs], q_[:, cs], start=True, stop=True, r=[k_, q_], w=[B_QK])
            for p_ in range(4):
                k.stt("dve", A0[:, p_, :], B_KK[:, p_ * 128:(p_ + 1) * 128], betok[:, hh, p_:p_ + 1], e2[:, p_, :],
                      ALU.mult, ALU.mult, r=[B_KK, betok, e2], w=[A0])
            k.tt("dve", qkT[:].rearrange("p cc i -> p (cc i)"), B_QK[:, :], e1[:].rearrange("p cc i -> p (cc i)"), ALU.mult,
                 r=[B_QK, e1], w=[qkT])
            for p_ in range(4):
                k.tr(B_TR[:, p_ * 128:(p_ + 1) * 128], A0[:, p_, :], I128, r=[A0, c["ident_f"]], w=[B_TR])
            k.copy("act", AT0[:].rearrange("p cc i -> p (cc i)"), B_TR[:, :], r=[B_TR], w=[AT0])
            k.copy("act", A0bs[hh][:], A0[:], r=[A0], w=[A0bs[hh]])
            yield
            for src_, dst_ in ((k_, ktok), (v_, vtok), (zs[hh], ztok)):
                for p_ in range(4):
                    k.tr(B_TR[:, p_ * 128:(p_ + 1) * 128], src_[:, p_ * 128:(p_ + 1) * 128], I128, r=[src_, c["ident_f"]], w=[B_TR])
                k.copy("act", dst_[:].rearrange("p cc d -> p (cc d)"), B_TR[:, :], r=[B_TR], w=[dst_])
                yield
            for p_ in range(4):
                k.act(ktail[:, p_, :], ktok[:, p_, :], AF.Copy, scale=eGlG[:, gi(p_):gi(p_) + 1], r=[ktok, eGlG], w=[ktail])
            yield

        def solve(hh, p_):
            A0b, AT0, ktok, vtok = A0bs[hh], AT0s[hh], ktoks[hh], vtoks[hh]
            xs, xb, WT = xss[hh][p_], xsbs[hh][p_], WTs[hh][p_]
            bsc_ = BSC[hh]
            g_ = hh * 4 + p_
            k.ts("dve", xs[:, 0:128], vtok[:, p_, :], betok[:, hh, p_:p_ + 1], None, ALU.mult, r=[vtok, betok], w=[xs])
            k.ts("dve", xs[:, 128:256], ktok[:, p_, :], betok[:, hh, p_:p_ + 1], eG[:, g_:g_ + 1], ALU.mult, ALU.mult,
                 r=[ktok, betok, eG], w=[xs])
            k.copy("act", xb[:], xs[:], r=[xs], w=[xb])
            Mcur = T(A0b.t[:, p_, :], A0b.b)
            Ncur = T(AT0.t[:, p_, :], AT0.b)
            bankX, bankY = SB[hh][0], SB[hh][1]
            cx = slice((p_ % 2) * 256, (p_ % 2) * 256 + 256)
            cm = slice((p_ % 2) * 256, (p_ % 2) * 256 + 128)
            cn = slice((p_ % 2) * 256 + 128, (p_ % 2) * 256 + 256)

            def square(lev, Mc, Nc):
                Mn, Nn = Mqs[hh][p_ % 2][lev % 2], Nqs[hh][p_ % 2][lev % 2]
                k.mm(bankY[:, cm], Nc[:], Mc[:], start=True, stop=True, r=[Nc, Mc], w=[bankY])
                k.mm(bankY[:, cn], Mc[:], Nc[:], start=True, stop=True, r=[Nc, Mc], w=[bankY])
                k.copy("act", Mn[:], bankY[:, cm], r=[bankY], w=[Mn])
                k.copy("act", Nn[:], bankY[:, cn], r=[bankY], w=[Nn])
                return Mn, Nn

            nxt = None
            for lev in range(6):
                if lev < 5:
                    nxt = square(lev + 1, Mcur, Ncur)
                k.mm(bankX[:, cx], Ncur[:], xb[:], start=True, stop=True, r=[Ncur, xb], w=[bankX])
                k.tt("dve", xs[:], xs[:], bankX[:, cx], ALU.subtract if lev == 0 else ALU.add, r=[xs, bankX], w=[xs])
                if lev < 5:
                    k.copy("act", xb[:], xs[:], r=[xs], w=[xb])
                    Mcur, Ncur = nxt
                yield
            k.tr(bsc_[:, 384:512], xs[:, 128:256], I128, r=[xs, c["ident_f"]], w=[bsc_])
            k.copy("dve", WT[:], bsc_[:, 384:512], r=[bsc_], w=[WT])
            yield

        def scan(hh, p_):
            S_, qd, qkT, ktail, ztok = Sst[hh], qds[hh], qkTs[hh], ktails[hh], ztoks[hh]
            xs, WT = xss[hh][p_], WTs[hh][p_]
            bsc, by = BSC[hh], BY[hh]
            ss, junk, vn = sss[hh], junks[hh], vns[hh]
            ot, yt = ots[hh][p_ % 2], yts[hh][p_ % 2]
            g_ = hh * 4 + p_
            cs = slice(p_ * 128, (p_ + 1) * 128)
            for c2 in range(2):
                rows = slice(c2 * 64, c2 * 64 + 64)
                egl = (eGlA if c2 == 0 else eGlB)
                k.mm(bsc[:, 0:128], WT[:], S_[:], start=True, stop=True, r=[WT, S_], w=[bsc])
                k.tt("dve", vn[rows, :], xs[rows, 0:128], bsc[rows, 0:128], ALU.subtract, r=[xs, bsc], w=[vn])
                k.mm(bsc[:, 128:256], qd[:, cs], S_[:], start=True, stop=False, r=[qd, S_], w=[bsc])
                k.mm(bsc[:, 128:256], qkT[:, p_, :], vn[:], start=False, stop=True, r=[qkT, vn], w=[bsc])
                k.mm(bsc[:, 256:384], ktail[rows, p_, :], vn[rows, :], start=True, stop=True, r=[ktail, vn], w=[bsc])
                k.stt("dve", S_[:], S_[:], egl[:, g_:g_ + 1], bsc[:, 256:384], ALU.mult, ALU.add, r=[S_, egl, bsc], w=[S_])
                k.copy("dve", ot[rows, :], bsc[rows, 128:256], r=[bsc], w=[ot])
                yield
            k.act(junk[:], ot[:], AF.Square, accum_out=ss[:, 0:1], r=[ot], w=[junk, ss])
            k.ts("dve", ss[:], ss[:], 1.0 / 128, RMS_EPS, ALU.mult, ALU.add, r=[ss], w=[ss])
            k.act(ss[:], ss[:], AF.Sqrt, r=[ss], w=[ss])
            k.op("dve", lambda: nc.vector.reciprocal(out=ss[:], in_=ss[:]), r=[ss], w=[ss])
            k.stt("dve", yt[:], ot[:], ss[:, 0:1], ngB[:], ALU.mult, ALU.mult, r=[ot, ss, ngB], w=[yt])
            k.tt("pool", yt[:], yt[:], ztok[:, p_, :], ALU.mult, r=[yt, ztok], w=[yt])
            k.tr(by[:, cs], yt[:], I128, r=[yt, c["ident_f"]], w=[by])
            if p_ == 3:
                k.copy("act", yb[hh][:], by[:, :], r=[by], w=[yb[hh]])
                k.dma("sp", y_out[hh * 128:(hh + 1) * 128, t * 512:(t + 1) * 512], yb[hh][:], r=[yb[hh]], w=[outb])
            yield

        def rr(gens):
            gens = list(gens)
            alive = [True] * len(gens)
            while any(alive):
                for gi_ in range(len(gens)):
                    if alive[gi_]:
                        try:
                            next(gens[gi_])
                        except StopIteration:
                            alive[gi_] = False
                yield

        def seq(gens):
            for g_ in gens:
                for _ in g_:
                    yield

        def head_flow(hh):
            for _ in rr([solve(hh, 0), solve(hh, 1)]):
                yield
            for _ in rr([solve(hh, 2), solve(hh, 3), seq([scan(hh, 0), scan(hh, 1)])]):
                yield
            for _ in seq([scan(hh, 2), scan(hh, 3)]):
                yield

        for hh in range(2):
            for _ in front(hh):
                pass
        flows = [head_flow(0), head_flow(1)]
        alive = [True, True]
        while any(alive):
            for hh in range(2):
                if alive[hh]:
                    try:
                        next(flows[hh])
                    except StopIteration:
                        alive[hh] = False
    barrier(k)
    k.st = k_st
    st.close()
    return outb


def phase_dn5(k, c, P, io, modT, hp, y_out, ntiles=16):
    nc = k.nc
    st = ExitStack()
    k_st, k.st = k.st, st
    scp1 = k.sb([128, 8], F32)
    k.ts("dve", scp1[:], modT[:, 1, :], 1.0, None, ALU.add, r=[modT], w=[scp1])
    sh1 = modT[:, 0, :]
    w_dn = io["w_dn"]
    wq = k.sb([128, 8, 4, 2, 128], BF16)
    for si in range(4):
        k.dma("pool", wq[:, :, si, :, :].rearrange("p kk hh n -> p kk (hh n)"),
              w_dn[:, si * 256:si * 256 + 256].rearrange("(kk p) n -> p kk n", p=128), w=[wq])
    wab = k.sb([128, 8, 4], BF16)
    k.dma("pool", wab[:], w_dn[:, 1024:1028].rearrange("(kk p) n -> p kk n", p=128), w=[wab])
    cw = k.sb([128, 3, 2, 4], F32)
    k.dma("sp", cw[:], io["convw"], w=[cw])
    I64 = c["ident_f"][0:64, 0:64]
    ones64 = c["ones_f"][0:64, 0:64]
    ones64_128 = c["ones_f"][0:64, :]
    dtbB = k.sb([128, 2], F32)
    nAB = k.sb([128, 2], F32)
    k.dma("sp", dtbB[:], bass.AP(io["dtb"].tensor, io["dtb"].offset, [[0, 128], [1, 2]]), w=[dtbB])
    k.dma("sp", nAB[:], bass.AP(io["alog"].tensor, io["alog"].offset, [[0, 128], [1, 2]]), w=[nAB])
    k.act(nAB[:], nAB[:], AF.Exp, r=[nAB], w=[nAB])
    k.ts("dve", nAB[:], nAB[:], -1.0, None, ALU.mult, r=[nAB], w=[nAB])
    ngB = k.sb([128, 128], F32)
    k.dma("sp", ngB[:], bass.AP(io["normg"].tensor, io["normg"].offset, [[0, 128], [1, 128]]), w=[ngB])
    I128 = c["ident_f"][:]
    UTbd = k.sb([128, 128], F32)
    k.memset("pool", UTbd[:], 1.0, w=[UTbd])
    k.op("pool", lambda: nc.gpsimd.affine_select(out=UTbd[:], in_=UTbd[:], pattern=[[1, 128]], compare_op=ALU.is_ge,
                                                  fill=0.0, base=0, channel_multiplier=-1), r=[UTbd], w=[UTbd])
    k.memset("pool", UTbd[0:64, 64:128], 0.0, w=[UTbd])
    ONESbd = k.sb([128, 128], F32)
    k.memset("pool", ONESbd[:], 0.0, w=[ONESbd])
    k.memset("pool", ONESbd[0:64, 0:64], 1.0, w=[ONESbd])
    k.memset("pool", ONESbd[64:128, 64:128], 1.0, w=[ONESbd])
    L0 = k.sb([128, 128], F32)
    L1 = k.sb([128, 128], F32)
    k.memset("pool", L0[:], 0.0, w=[L0])
    k.memset("pool", L0[0:64, :], 1.0, w=[L0])
    k.memset("pool", L1[:], 0.0, w=[L1])
    k.memset("pool", L1[64:128, :], 1.0, w=[L1])
    TRIU = k.sb([128, 4, 128], F32)
    for p_ in range(4):
        k.copy("pool", TRIU[:, p_, :], UTbd[:], r=[UTbd], w=[TRIU])
    TRILS = k.sb([128, 4, 128], F32)
    k.memset("pool", TRILS[:], 1.0, w=[TRILS])
    k.op("pool", lambda: nc.gpsimd.affine_select(out=TRILS[:], in_=TRILS[:], pattern=[[0, 4], [-1, 128]], compare_op=ALU.is_gt,
                                                  fill=0.0, base=0, channel_multiplier=1), r=[TRILS], w=[TRILS])
    k.memset("pool", TRILS[64:128, :, 0:64], 0.0, w=[TRILS])
    xbufs = [k.sb([128, 4, D], F32) for _ in range(2)]
    hT = k.sb([128, 8, 512], BF16)
    pre = [[k.sb([128, 515], F32) for _ in range(2)] for _ in range(3)]
    for s_ in range(3):
        for hh in range(2):
            k.memset("pool", pre[s_][hh][:, 0:3], 0.0, w=[pre[s_][hh]])
    qkv = [[k.sb([128, 512], F32) for _ in range(2)] for _ in range(3)]
    zs = [k.sb([128, 512], F32) for _ in range(2)]
    accs = [k.sb([128, 512], F32) for _ in range(2)]
    sqs = [k.sb([128, 512], F32) for _ in range(2)]
    rns = [k.sb([128, 512], F32) for _ in range(2)]
    Sst = [k.sb([128, 128], F32) for _ in range(2)]
    for hh in range(2):
        k.memset("pool", Sst[hh][:], 0.0, w=[Sst[hh]])
    gtoks = [k.sb([128, 2, 4], F32) for _ in range(2)]
    betoks = [k.sb([128, 2, 4], F32) for _ in range(2)]
    Gss = [k.sb([128, 8], F32) for _ in range(2)]
    eGs = [k.sb([128, 8], F32) for _ in range(2)]
    eGlGs = [k.sb([128, 8], F32) for _ in range(2)]
    eGlAs = [k.sb([128, 8], F32) for _ in range(2)]
    eGlBs = [k.sb([128, 8], F32) for _ in range(2)]
    rhsG = k.sb([128, 4, 128], F32)
    eGB = k.sb([128, 512], F32)
    dd = k.sb([128, 4, 128], F32)
    e1 = k.sb([128, 4, 128], F32)
    e2 = k.sb([128, 4, 128], F32)
    A0s = [k.sb([128, 4, 128], F32) for _ in range(2)]
    A0bs = [[k.sb([128, 4, 128], BF16) for _ in range(2)] for _ in range(2)]
    AT0s = [[k.sb([128, 4, 128], BF16) for _ in range(2)] for _ in range(2)]
    Mqs = [[[k.sb([128, 128], BF16) for _ in range(2)] for _ in range(2)] for _ in range(2)]
    Nqs = [[[k.sb([128, 128], BF16) for _ in range(2)] for _ in range(2)] for _ in range(2)]
    qkTs = [[k.sb([128, 4, 128], F32) for _ in range(2)] for _ in range(2)]
    ktoks = [[k.sb([128, 4, 128], F32) for _ in range(2)] for _ in range(2)]
    vtoks = [[k.sb([128, 4, 128], F32) for _ in range(2)] for _ in range(2)]
    ztoks = [[k.sb([128, 4, 128], F32) for _ in range(2)] for _ in range(2)]
    ktails = [[k.sb([128, 4, 128], F32) for _ in range(2)] for _ in range(2)]
    qds = [[k.sb([128, 512], F32) for _ in range(2)] for _ in range(2)]
    xss = [[k.sb([128, 256], F32) for _ in range(4)] for _ in range(2)]
    xsbs = [[k.sb([128, 256], BF16) for _ in range(4)] for _ in range(2)]
    WTs = [[k.sb([128, 128], F32) for _ in range(4)] for _ in range(2)]
    vns = [k.sb([128, 128], F32) for _ in range(2)]
    for hh in range(2):
        k.memset("pool", vns[hh][:], 0.0, w=[vns[hh]])
    ots = [[k.sb([128, 128], F32) for _ in range(2)] for _ in range(2)]
    yts = [[k.sb([128, 128], F32) for _ in range(2)] for _ in range(2)]
    junks = [k.sb([128, 128], F32) for _ in range(2)]
    sss = [k.sb([128, 1], F32) for _ in range(2)]
    yb = [[k.sb([128, 512], BF16) for _ in range(2)] for _ in range(2)]
    outb = Buf()
    SBK = [P["c"][0], P["c"][1]]
    BSC = [P["tr"][0], P["tr"][1]]
    FB = [P["q"][0], P["q"][1]]
    def pstage(t):
        par = t % 2
        gtok_, betok_, Gs_, eG_, eGlG_, eGlA_, eGlB_ = gtoks[par], betoks[par], Gss[par], eGs[par], eGlGs[par], eGlAs[par], eGlBs[par]
        xt = xbufs[t % 2]
        for tb in range(4):
            k.dma("sp", xt[:, tb, :], io["xrow_true"](t * 4 + tb), r=[io.get("xb_true", io["xb"])], w=[xt])
        transpose_modulate(k, c, xt, hT, [P["m"], P["t"]], scp1, sh1, 4)
        pab = P["m"]
        for pr_ in range(4):
            for kk in range(8):
                k.mm(pab[:, pr_ * 4:pr_ * 4 + 4], hT[:, kk, pr_ * 128:(pr_ + 1) * 128], wab[:, kk, :], start=(kk == 0),
                     stop=(kk == 7), inc=(kk == 7), r=[hT, wab], w=[pab])
        pabv = pab[:, 0:16].rearrange("p (cc f) -> p cc f", f=4)
        for hh in range(2):
            k.act(gtok_[:, hh, :], pabv[:, :, hh], AF.Exp, bias=dtbB[:, hh:hh + 1], r=[pab, dtbB], w=[gtok_])
            k.act(betok_[:, hh, :], pabv[:, :, 2 + hh], AF.Sigmoid, r=[pab], w=[betok_])
        k.ts("dve", gtok_[:], gtok_[:], 1.0, None, ALU.add, r=[gtok_], w=[gtok_])
        k.act(gtok_[:], gtok_[:], AF.Ln, r=[gtok_], w=[gtok_])
        for hh in range(2):
            k.ts("dve", gtok_[:, hh, :], gtok_[:, hh, :], nAB[:, hh:hh + 1], None, ALU.mult, r=[gtok_, nAB], w=[gtok_])
        gflat = gtok_[:].rearrange("p h cc -> p (h cc)")
        pG = P["t"]
        k.mm(pG[:, 0:8], UTbd[:], gflat, start=True, stop=True, r=[UTbd, gtok_], w=[pG])
        k.mm(pG[:, 8:16], ONESbd[:], gflat, start=True, stop=True, r=[ONESbd, gtok_], w=[pG])
        k.mm(pG[:, 16:24], L0[:], gflat, start=True, stop=True, r=[L0, gtok_], w=[pG])
        k.mm(pG[:, 24:32], L1[:], gflat, start=True, stop=True, r=[L1, gtok_], w=[pG])
        k.copy("dve", Gs_[:], pG[:, 0:8], r=[pG], w=[Gs_])
        k.tt("dve", eGlG_[:], pG[:, 8:16], Gs_[:], ALU.subtract, r=[pG, Gs_], w=[eGlG_])
        k.copy("dve", eGlA_[:], pG[:, 16:24], r=[pG], w=[eGlA_])
        k.copy("dve", eGlB_[:], pG[:, 24:32], r=[pG], w=[eGlB_])
        k.act(eG_[:], Gs_[:], AF.Exp, r=[Gs_], w=[eG_])
        k.act(eGlG_[:], eGlG_[:], AF.Exp, r=[eGlG_], w=[eGlG_])
        k.act(eGlA_[:], eGlA_[:], AF.Exp, r=[eGlA_], w=[eGlA_])
        k.act(eGlB_[:], eGlB_[:], AF.Exp, r=[eGlB_], w=[eGlB_])
        yield
        for hh in range(2):
            for s_ in range(4):
                pp = P["m"] if s_ % 2 == 0 else P["t"]
                for kk in range(8):
                    k.mm(pp[:, :], wq[:, kk, s_, hh, :], hT[:, kk, :], start=(kk == 0), stop=(kk == 7), inc=(kk == 7), r=[wq, hT], w=[pp])
                yield
                if s_ == 3:
                    k.act(zs[hh][:], pp[:, :], AF.Silu, r=[pp], w=[zs[hh]])
                    continue
                pr = pre[s_][hh]
                acc, sq, rn = accs[s_ % 2], sqs[s_ % 2], rns[s_ % 2]
                k.copy("act", pr[:, 3:515], pp[:, :], r=[pp], w=[pr])
                k.ts("dve", acc[:], pr[:, 0:512], cw[:, s_, hh, 0:1], None, ALU.mult, r=[pr, cw], w=[acc])
                for j in range(1, 4):
                    k.stt("dve", acc[:], pr[:, j:j + 512], cw[:, s_, hh, j:j + 1], acc[:], ALU.mult, ALU.add,
                          r=[pr, cw, acc], w=[acc])
                k.copy("pool", pr[:, 0:3], pr[:, 512:515], r=[pr], w=[pr])
                dst = qkv[s_][hh]
                if s_ == 2:
                    k.act(dst[:], acc[:], AF.Silu, r=[acc], w=[dst])
                    continue
                k.act(acc[:], acc[:], AF.Silu, r=[acc], w=[acc])
                k.tt("dve", sq[:], acc[:], acc[:], ALU.mult, r=[acc], w=[sq])
                k.mm(pp[:, :], c["ones_f"][:], sq[:], start=True, stop=True, r=[c["ones_f"], sq], w=[pp])
                k.ts("dve", rn[:], pp[:, :], RMS_EPS, None, ALU.add, r=[pp], w=[rn])
                k.act(rn[:], rn[:], AF.Sqrt, r=[rn], w=[rn])
                k.op("dve", lambda: nc.vector.reciprocal(out=rn[:], in_=rn[:]), r=[rn], w=[rn])
                if s_ == 0:
                    k.stt("dve", dst[:], acc[:], 128 ** -0.5, rn[:], ALU.mult, ALU.mult, r=[acc, rn], w=[dst])
                else:
                    k.tt("dve", dst[:], acc[:], rn[:], ALU.mult, r=[acc, rn], w=[dst])

    def front(t, hh):
        par = t % 2
        gtok, betok, Gs, eGlG = gtoks[par], betoks[par], Gss[par], eGlGs[par]
        q_, k_, v_ = qkv[0][hh], qkv[1][hh], qkv[2][hh]
        qd, qkT, ktok, vtok, ztok, ktail = qds[hh][par], qkTs[hh][par], ktoks[hh][par], vtoks[hh][par], ztoks[hh][par], ktails[hh][par]
        A0, AT0 = A0s[hh], AT0s[hh][par]
        B_GB, B_KK, B_QK, B_TR = FB[0], FB[1], FB[0], FB[1]
        gi = lambda p_: hh * 4 + p_
        for p_ in range(4):
            k.ts("dve", rhsG[:, p_, :], UTbd[:], gtok[:, hh, p_:p_ + 1], None, ALU.mult, r=[UTbd, gtok], w=[rhsG])
        k.mm(B_GB[:, :], c["ones_f"][:], rhsG[:].rearrange("p cc i -> p (cc i)"), start=True, stop=True,
             r=[c["ones_f"], rhsG], w=[B_GB])
        k.act(eGB[:], B_GB[:, :], AF.Exp, r=[B_GB], w=[eGB])
        k.tt("dve", qd[:], q_[:], eGB[:], ALU.mult, r=[q_, eGB], w=[qd])
        yield
        for p_ in range(4):
            k.ts("dve", dd[:, p_, :], B_GB[:, p_ * 128:(p_ + 1) * 128], Gs[:, gi(p_):gi(p_) + 1], None, ALU.subtract,
                 r=[B_GB, Gs], w=[dd])
        k.ts("dve", e1[:], dd[:], 0.0, None, ALU.min, r=[dd], w=[e1])
        k.ts("dve", e2[:], dd[:], 0.0, None, ALU.max, r=[dd], w=[e2])
        k.act(e1[:], e1[:], AF.Exp, r=[e1], w=[e1])
        k.act(e2[:], e2[:], AF.Exp, scale=-1.0, r=[e2], w=[e2])
        k.tt("dve", e1[:], e1[:], TRIU[:], ALU.mult, r=[e1, TRIU], w=[e1])
        k.tt("pool", e2[:], e2[:], TRILS[:], ALU.mult, r=[e2, TRILS], w=[e2])
        yield
        for p_ in range(4):
            cs = slice(p_ * 128, (p_ + 1) * 128)
            k.mm(B_KK[:, cs], k_[:, cs], k_[:, cs], start=True, stop=True, r=[k_], w=[B_KK])
            k.mm(B_QK[:, cs], k_[:, cs], q_[:, cs], start=True, stop=True, r=[k_, q_], w=[B_QK])
        for p_ in range(4):
            k.stt("dve", A0[:, p_, :], B_KK[:, p_ * 128:(p_ + 1) * 128], betok[:, hh, p_:p_ + 1], e2[:, p_, :],
                  ALU.mult, ALU.mult, r=[B_KK, betok, e2], w=[A0])
        k.tt("dve", qkT[:].rearrange("p cc i -> p (cc i)"), B_QK[:, :], e1[:].rearrange("p cc i -> p (cc i)"), ALU.mult,
             r=[B_QK, e1], w=[qkT])
        for p_ in range(4):
            k.tr(B_TR[:, p_ * 128:(p_ + 1) * 128], A0[:, p_, :], I128, r=[A0, c["ident_f"]], w=[B_TR])
        k.copy("act", AT0[:].rearrange("p cc i -> p (cc i)"), B_TR[:, :], r=[B_TR], w=[AT0])
        k.copy("act", A0bs[hh][par][:], A0[:], r=[A0], w=[A0bs[hh][par]])
        yield
        for src_, dst_ in ((k_, ktok), (v_, vtok), (zs[hh], ztok)):
            for p_ in range(4):
                k.tr(B_TR[:, p_ * 128:(p_ + 1) * 128], src_[:, p_ * 128:(p_ + 1) * 128], I128, r=[src_, c["ident_f"]], w=[B_TR])
            k.copy("act", dst_[:].rearrange("p cc d -> p (cc d)"), B_TR[:, :], r=[B_TR], w=[dst_])
            yield
        for p_ in range(4):
            k.act(ktail[:, p_, :], ktok[:, p_, :], AF.Copy, scale=eGlG[:, gi(p_):gi(p_) + 1], r=[ktok, eGlG], w=[ktail])
        yield


    def solve(t, hh, p_):
        par = t % 2
        betok, eG = betoks[par], eGs[par]
        A0b, AT0, ktok, vtok = A0bs[hh][par], AT0s[hh][par], ktoks[hh][par], vtoks[hh][par]
        xs, xb, WT = xss[hh][p_], xsbs[hh][p_], WTs[hh][p_]
        bank = SBK[hh]
        bsc_ = BSC[hh]
        g_ = hh * 4 + p_
        k.ts("dve", xs[:, 0:128], vtok[:, p_, :], betok[:, hh, p_:p_ + 1], None, ALU.mult, r=[vtok, betok], w=[xs])
        k.ts("dve", xs[:, 128:256], ktok[:, p_, :], betok[:, hh, p_:p_ + 1], eG[:, g_:g_ + 1], ALU.mult, ALU.mult,
             r=[ktok, betok, eG], w=[xs])
        k.copy("act", xb[:], xs[:], r=[xs], w=[xb])
        Mcur = T(A0b.t[:, p_, :], A0b.b)
        Ncur = T(AT0.t[:, p_, :], AT0.b)
        for lev in range(6):
            if lev > 0:
                Mn, Nn = Mqs[hh][p_ % 2][lev % 2], Nqs[hh][p_ % 2][lev % 2]
                k.mm(bank[:, 256:384], Ncur[:], Mcur[:], start=True, stop=True, r=[Ncur, Mcur], w=[bank])
                k.mm(bank[:, 384:512], Mcur[:], Ncur[:], start=True, stop=True, r=[Ncur, Mcur], w=[bank])
                k.copy("act", Mn[:], bank[:, 256:384], r=[bank], w=[Mn])
                k.copy("act", Nn[:], bank[:, 384:512], r=[bank], w=[Nn])
                Mcur, Ncur = Mn, Nn
            k.mm(bank[:, 0:256], Ncur[:], xb[:], start=True, stop=True, r=[Ncur, xb], w=[bank])
            k.tt("dve", xs[:], xs[:], bank[:, 0:256], ALU.subtract if lev == 0 else ALU.add, r=[xs, bank], w=[xs])
            if lev < 5:
                k.copy("act", xb[:], xs[:], r=[xs], w=[xb])
            yield
        k.tr(bsc_[:, 384:512], xs[:, 128:256], I128, r=[xs, c["ident_f"]], w=[bsc_])
        k.copy("dve", WT[:], bsc_[:, 384:512], r=[bsc_], w=[WT])
        yield

    def scan(t, hh, p_):
        par = t % 2
        eGlA, eGlB = eGlAs[par], eGlBs[par]
        S_, qd, qkT, ktail, ztok = Sst[hh], qds[hh][par], qkTs[hh][par], ktails[hh][par], ztoks[hh][par]
        xs, WT = xss[hh][p_], WTs[hh][p_]
        bsc = BSC[hh]
        ss, junk, vn = sss[hh], junks[hh], vns[hh]
        ot, yt = ots[hh][p_ % 2], yts[hh][p_ % 2]
        g_ = hh * 4 + p_
        cs = slice(p_ * 128, (p_ + 1) * 128)
        for c2 in range(2):
            rows = slice(c2 * 64, c2 * 64 + 64)
            egl = (eGlA if c2 == 0 else eGlB)
            k.mm(bsc[:, 0:128], WT[:], S_[:], start=True, stop=True, r=[WT, S_], w=[bsc])
            k.tt("dve", vn[rows, :], xs[rows, 0:128], bsc[rows, 0:128], ALU.subtract, r=[xs, bsc], w=[vn])
            k.mm(bsc[:, 128:256], qd[:, cs], S_[:], start=True, stop=False, r=[qd, S_], w=[bsc])
            k.mm(bsc[:, 128:256], qkT[:, p_, :], vn[:], start=False, stop=True, r=[qkT, vn], w=[bsc])
            k.mm(bsc[:, 256:384], ktail[rows, p_, :], vn[rows, :], start=True, stop=True, r=[ktail, vn], w=[bsc])
            k.stt("dve", S_[:], S_[:], egl[:, g_:g_ + 1], bsc[:, 256:384], ALU.mult, ALU.add, r=[S_, egl, bsc], w=[S_])
            k.copy("dve", ot[rows, :], bsc[rows, 128:256], r=[bsc], w=[ot])
            yield
        k.act(junk[:], ot[:], AF.Square, accum_out=ss[:, 0:1], r=[ot], w=[junk, ss])
        k.ts("dve", ss[:], ss[:], 1.0 / 128, RMS_EPS, ALU.mult, ALU.add, r=[ss], w=[ss])
        k.act(ss[:], ss[:], AF.Sqrt, r=[ss], w=[ss])
        k.op("dve", lambda: nc.vector.reciprocal(out=ss[:], in_=ss[:]), r=[ss], w=[ss])
        k.stt("dve", yt[:], ot[:], ss[:, 0:1], ngB[:], ALU.mult, ALU.mult, r=[ot, ss, ngB], w=[yt])
        k.tt("pool", yt[:], yt[:], ztok[:, p_, :], ALU.mult, r=[yt, ztok], w=[yt])
        k.tr(bsc[:, 384:512], yt[:], I128, r=[yt, c["ident_f"]], w=[bsc])
        k.copy("act", yb[hh][par][:, cs], bsc[:, 384:512], r=[bsc], w=[yb[hh][par]])
        if p_ == 3:
            k.dma("sp", y_out[hh * 128:(hh + 1) * 128, t * 512:(t + 1) * 512], yb[hh][par][:], r=[yb[hh][par]], w=[outb])
        yield


    def rr(gens):
        gens = list(gens)
        alive = [True] * len(gens)
        while any(alive):
            for gi_ in range(len(gens)):
                if alive[gi_]:
                    try:
                        next(gens[gi_])
                    except StopIteration:
                        alive[gi_] = False
            yield

    def seq(gens):
        for g_ in gens:
            for _ in g_:
                yield

    def head_flow(t, hh):
        for _ in solve(t, hh, 0):
            yield
        for p_ in range(4):
            nxt = [solve(t, hh, p_ + 1)] if p_ + 1 < 4 else []
            for _ in rr(nxt + [scan(t, hh, p_)]):
                yield

    def prep(t):
        return seq([pstage(t), front(t, 0), front(t, 1)])

    for _ in prep(0):
        pass
    for t in range(ntiles):
        gens = [rr([head_flow(t, 0), head_flow(t, 1)])]
        if t + 1 < ntiles:
            gens.append(prep(t + 1))
        for _ in rr(gens):
            pass
    barrier(k)
    k.st = k_st
    st.close()
    return outb


DN_IMPL = [phase_dn4]


NIT = 16


def phase_dsa2(k, c, P, io, jq, res, scr, nblk=16):
    nc = k.nc
    st = ExitStack()
    k_st, k.st = k.st, st
    ckv_tok, ckvT, kidxT, widx = res["ckv_tok"], res["ckvT"], res["kidxT"], res["widx"]
    wuv = k.sb([128, 8, 2, 128], BF16)
    k.dma("pool", wuv[:], io["w_uv"].rearrange("h (cc c) d -> c h cc d", c=128), w=[wuv])
    isc = k.sb([128, S], F32)
    maskTs = [k.sb([128, 64, 128], BF16) for _ in range(2)]
    rl = [k.sb([128, 512], F32) for _ in range(2)]
    mk = [k.sb([128, 512], F32) for _ in range(2)]
    qis = [k.sb([128, 4, 128], BF16) for _ in range(2)]
    qls = [k.sb([128, 2, 8, 128], BF16) for _ in range(2)]
    es = [k.sb([128, 4, 128], BF16) for _ in range(3)]
    pts = [k.sb([128, 4, 128], BF16) for _ in range(3)]
    rden = k.sb([128, 512], F32)
    olat = k.sb([128, 2, 512], BF16)
    yas = [k.sb([128, 8, 128], BF16) for _ in range(2)]
    tmpd = k.sb([128, 128], F32)
    tmpp = k.sb([128, 384], F32)
    padb = k.sb([128, 384], F32)
    k.dma("sp", padb[:], io["padb"], w=[padb])
    pw2 = k.sb([128, NIT + 1], F32)
    for it in range(NIT + 1):
        k.memset("pool", pw2[:, it:it + 1], 2.0 ** -(it + 1), w=[pw2])
    steps = k.sb([128, NIT + 1], F32)
    nbias = k.sb([128, 1], F32)
    k.memset("pool", nbias[:], -30000.0, w=[nbias])
    sm = {n: k.sb([128, 1], F32) for n in ("lo", "hi", "mid", "cnt", "u", "R", "m1", "m2", "m3")}
    thr = [k.sb([128, 1], F32) for _ in range(2)]

    def front(i):
        g = 4 * i + jq
        nk = (g + 1) * 128
        nkt = (nk + 511) // 512
        qi, ql = qis[i % 2], qls[i % 2]
        maskT = maskTs[i % 2]
        k.dma("sp", qi[:], scr["qidxT"][i], r=[scr["qidxT_b"][i]], w=[qi])
        k.dma("sp", ql[:], scr["qlatT"][i], r=[scr["qlatT_b"][i]], w=[ql])
        for kt in range(nkt):
            w_ = min(512, nk - kt * 512)
            cols = slice(kt * 512, kt * 512 + w_)
            for h in range(8):
                m, po = h // 2, (h % 2) * 64
                ps = P["c"][h % 2]
                r_ = rl[h % 2]
                k.mm(ps[:, 0:w_], qi[po:po + 64, m, :], kidxT[po:po + 64, cols], start=True, stop=True,
                     r=[qi, kidxT], w=[ps])
                k.act(r_[:, 0:w_], ps[:, 0:w_], AF.Relu, r=[ps], w=[r_])
                if h == 0:
                    k.ts("dve", isc[:, cols], r_[:, 0:w_], widx[:, i, 0:1], None, ALU.mult, r=[r_, widx], w=[isc])
                else:
                    k.stt("dve", isc[:, cols], r_[:, 0:w_], widx[:, i, h:h + 1], isc[:, cols], ALU.mult, ALU.add,
                          r=[r_, widx, isc], w=[isc])
                yield
        dg = slice(nk - 128, nk)
        k.tt("dve", tmpp[:], isc[:, 0:384], padb[:], ALU.subtract, r=[isc, padb], w=[tmpp])
        k.tt("dve", isc[:, 0:384], isc[:, 0:384], padb[:], ALU.add, r=[isc, padb], w=[isc])
        k.op("dve", lambda: nc.vector.tensor_reduce(out=sm["m3"][:], in_=tmpp[:], axis=AX.X, op=ALU.min), r=[tmpp], w=[sm["m3"]])
        k.tt("dve", tmpd[:], isc[:, dg], c["cmask"][:], ALU.subtract, r=[isc, c["cmask"]], w=[tmpd])
        k.tt("dve", isc[:, dg], isc[:, dg], c["cmask"][:], ALU.add, r=[isc, c["cmask"]], w=[isc])
        k.op("dve", lambda: nc.vector.tensor_reduce(out=sm["hi"][:], in_=isc[:, 0:nk], axis=AX.X, op=ALU.max), r=[isc], w=[sm["hi"]])
        k.op("dve", lambda: nc.vector.tensor_reduce(out=sm["m2"][:], in_=tmpd[:], axis=AX.X, op=ALU.min), r=[tmpd], w=[sm["m2"]])
        k.tt("dve", sm["m2"][:], sm["m2"][:], sm["m3"][:], ALU.min, r=[sm["m3"], sm["m2"]], w=[sm["m2"]])
        if nk - 128 > 384:
            k.op("dve", lambda: nc.vector.tensor_reduce(out=sm["m1"][:], in_=isc[:, 384:nk - 128], axis=AX.X, op=ALU.min),
                 r=[isc], w=[sm["m1"]])
            k.tt("dve", sm["m2"][:], sm["m2"][:], sm["m1"][:], ALU.min, r=[sm["m1"], sm["m2"]], w=[sm["m2"]])
        lo, hi, mid, cnt, u, R = (sm[n] for n in ("lo", "hi", "mid", "cnt", "u", "R"))
        k.ts("dve", lo[:], sm["m2"][:], -1.0, None, ALU.add, r=[sm["m2"]], w=[lo])
        k.stt("dve", R[:], hi[:], 1.0, lo[:], ALU.add, ALU.subtract, r=[hi, lo], w=[R])
        k.ts("dve", steps[:], pw2[:], R[:, 0:1], None, ALU.mult, r=[pw2, R], w=[steps])
        k.tt("dve", mid[:], lo[:], steps[:, 0:1], ALU.add, r=[lo, steps], w=[mid])
        yield
        for it in range(NIT):
            k.ts("dve", maskT[:].rearrange("p b t -> p (b t)")[:, 0:nk], isc[:, 0:nk], mid[:, 0:1], 0.0, ALU.is_ge, ALU.add,
                 r=[isc, mid], w=[maskT, cnt], accum_out=cnt[:, 0:1])
            k.ts("dve", u[:], cnt[:], float(TOPK), -0.5, ALU.is_ge, ALU.add, r=[cnt], w=[u])
            k.stt("dve", mid[:], u[:], steps[:, it:it + 1], mid[:], ALU.mult, ALU.add, r=[u, steps, mid], w=[mid])
            yield
        th = thr[i % 2]
        k.tt("dve", th[:], mid[:], steps[:, NIT:NIT + 1], ALU.subtract, r=[mid, steps], w=[th])
        for kt in range(nkt):
            w_ = min(512, nk - kt * 512)
            cols = slice(kt * 512, kt * 512 + w_)
            m_ = mk[kt % 2]
            k.ts("dve", m_[:, 0:w_], isc[:, cols], th[:, 0:1], None, ALU.is_ge, r=[isc, th], w=[m_])
            pt = P["t"]
            nb_ = w_ // 128
            for bb in range(nb_):
                k.tr(pt[:, bb * 128:(bb + 1) * 128], m_[:, bb * 128:(bb + 1) * 128], c["ident_f"][:],
                     r=[m_, c["ident_f"]], w=[pt])
            k.act(maskT[:, kt * 4:kt * 4 + nb_, :], pt[:, 0:w_].rearrange("p (b t) -> p b t", b=nb_), AF.Identity,
                  scale=30000.0, bias=nbias[:, 0:1], r=[pt, nbias], w=[maskT])
            yield

    def n_front(i):
        g = 4 * i + jq
        nkt = ((g + 1) * 128 + 511) // 512
        return nkt * 8 + 1 + NIT + nkt

    def att(i):
        g = 4 * i + jq
        ql, ya = qls[i % 2], yas[i % 2]
        maskT = maskTs[i % 2]
        n = 0
        for hg in range(2):
            acc = P["q"]
            den = P["m"]
            qlf = [ql[:, cc, hg * 4:hg * 4 + 4, :].rearrange("p h t -> p (h t)") for cc in range(2)]

            def scores(kb, n_):
                psS = P["tr"][kb % 2]
                pT = pts[n_ % 3]
                for cc in range(2):
                    k.mm(psS[:, :], ckvT[:, cc, kb * 128:(kb + 1) * 128], qlf[cc], start=(cc == 0), stop=False,
                         r=[ckvT, ql], w=[psS])
                mrow = maskT[:, kb, :]
                apl = [list(x_) for x_ in mrow.ap]
                mb = bass.AP(mrow.tensor, mrow.offset, [apl[0], [0, 4], apl[1]])
                k.mm(psS[:, :], c["ident_b"][:], mb, start=False, stop=True, r=[c["ident_b"], maskT], w=[psS])
                k.act(pT[:].rearrange("p h t -> p (h t)"), psS[:, :], AF.Exp, r=[psS], w=[pT])

            scores(0, n)
            for kb in range(g + 1):
                pT = pts[n % 3]
                if kb + 1 <= g:
                    scores(kb + 1, n + 1)
                pf = pT[:].rearrange("p h t -> p (h t)")
                for cc in range(2):
                    k.mm(acc[cc][:, :], ckv_tok[:, kb, cc * 128:(cc + 1) * 128], pf, start=(kb == 0), stop=(kb == g),
                         r=[ckv_tok, pT], w=[acc[cc]])
                k.mm(den[:, :], c["ones_b"][:], pf, start=(kb == 0), stop=(kb == g), r=[c["ones_b"], pT], w=[den])
                n += 1
                yield
            k.op("dve", lambda: nc.vector.reciprocal(out=rden[:], in_=den[:, :]), r=[den], w=[rden])
            for cc in range(2):
                k.tt("dve", olat[:, cc, :], acc[cc][:, :], rden[:], ALU.mult, r=[acc[cc], rden], w=[olat])
            for hh in range(4):
                h = hg * 4 + hh
                psY = P["tr"][hh % 2]
                for cc in range(2):
                    k.mm(psY[:, 0:128], wuv[:, h, cc, :], olat[:, cc, hh * 128:(hh + 1) * 128], start=(cc == 0),
                         stop=(cc == 1), r=[wuv, olat], w=[psY])
                k.copy("act", ya[:, h, :], psY[:, 0:128], r=[psY], w=[ya])
            yield
        k.dma("sp", scr["yattT"][i], ya[:], r=[ya], w=[scr["yattT_b"][i]])

    def n_att(i):
        return 2 * (4 * i + jq + 2)

    for _ in front(0):
        pass
    for i in range(nblk):
        ga = att(i)
        gb = front(i + 1) if i + 1 < nblk else iter(())
        na = n_att(i)
        nb = n_front(i + 1) if i + 1 < nblk else 0
        done_a = done_b = 0
        a_alive = b_alive = True
        while a_alive or b_alive:
            fa = done_a / na
            fb = done_b / nb if nb else 2.0
            if a_alive and (fa <= fb or not b_alive):
                try:
                    next(ga)
                    done_a += 1
                except StopIteration:
                    a_alive = False
            elif b_alive:
                try:
                    next(gb)
                    done_b += 1
                except StopIteration:
                    b_alive = False
            else:
                break
    barrier(k)
    k.st = k_st
    st.close()


DSA_IMPL = [phase_dsa2]
```
